# Optimizing a Trainium2 kernel written in Bass

```python
import math
import jax
import jax.numpy as jnp
from jax import lax
import numpy as np

D_MODEL = 1024
BATCH = 32
SEQ = 2048
DEPTH = 2

GRID_W = 64
CTX_LEN = 256
HEAD_DIM = 64
N_MIXERS = 4
GROUP_WIDTH = D_MODEL // N_MIXERS

SWA_HEADS = GROUP_WIDTH // HEAD_DIM
SWA_KV_HEADS = 2
SWA_GQA = SWA_HEADS // SWA_KV_HEADS
SWA_WINDOW = 128
SWA_BLOCK = 128
ROPE_BASE = 10000.0

NA_HEADS = GROUP_WIDTH // HEAD_DIM
NA_ROWS = 8
NA_COLS = 16
NA_QB = 16
NA_KB = NA_QB + NA_COLS

HGRN_HEADS = GROUP_WIDTH // HEAD_DIM
HGRN_DK = HEAD_DIM
HGRN_DV = HEAD_DIM
HGRN_CHUNK = 64

S5_GROUP_CH = 16
S5_GROUPS = GROUP_WIDTH // S5_GROUP_CH
S5_STATE = 64

MOE_GROUPS = 4
MOE_PER_GROUP = 8
MOE_EXPERTS = MOE_GROUPS * MOE_PER_GROUP
MOE_TOPK = 2
MOE_HIDDEN = 512

NORM_EPS = 1e-6
NEG_INF = -1e30

PROJ_SIZES = (GROUP_WIDTH, SWA_KV_HEADS * HEAD_DIM, SWA_KV_HEADS * HEAD_DIM,
              GROUP_WIDTH, GROUP_WIDTH, GROUP_WIDTH,
              GROUP_WIDTH, GROUP_WIDTH, GROUP_WIDTH, GROUP_WIDTH, GROUP_WIDTH,
              GROUP_WIDTH)
PROJ_WIDTH = sum(PROJ_SIZES)

kernel_name = 'hybrid_dit_parallel_groups_hmoe'


def _rmsnorm(x, w):
    xf = x.astype(jnp.float32)
    y = xf * lax.rsqrt(jnp.mean(xf * xf, axis=-1, keepdims=True) + NORM_EPS)
    return (y * w.astype(jnp.float32)).astype(x.dtype)


def _modulate(h, shift, scale):
    return h * (1 + scale) + shift


def _rotate(x, ang):
    n = x.shape[-1] // 2
    cos = jnp.cos(ang)[None, :, None, :].astype(x.dtype)
    sin = jnp.sin(ang)[None, :, None, :].astype(x.dtype)
    x1, x2 = x[..., :n], x[..., n:]
    return jnp.concatenate([x1 * cos - x2 * sin, x1 * sin + x2 * cos], axis=-1)


def _rope_2d(x, row, col):
    half = HEAD_DIM // 2
    inv = 1.0 / (ROPE_BASE ** (jnp.arange(0, half, 2, dtype=jnp.float32) / half))
    ang_r = row.astype(jnp.float32)[:, None] * inv[None, :]
    ang_c = col.astype(jnp.float32)[:, None] * inv[None, :]
    return jnp.concatenate([_rotate(x[..., :half], ang_r), _rotate(x[..., half:], ang_c)], axis=-1)


def _sink_softmax(s, sink):
    m = jnp.maximum(jnp.max(s, axis=-1, keepdims=True), sink)
    e = jnp.exp(s - m)
    return e / (jnp.sum(e, axis=-1, keepdims=True) + jnp.exp(sink - m))


def _swa_mixer(q, k, v, qc, kc, vc, sink, row, col, need_ctx):
    bsz, n = q.shape[0], q.shape[1]
    scale = HEAD_DIM ** -0.5
    qg = (_rope_2d(q, row, col) * scale).reshape(bsz, n, SWA_KV_HEADS, SWA_GQA, HEAD_DIM)
    pad = ((0, 0), (SWA_WINDOW, SWA_WINDOW), (0, 0), (0, 0))
    kp = jnp.pad(_rope_2d(k, row, col), pad)
    vp = jnp.pad(v, pad)
    sink_b = sink.astype(jnp.float32).reshape(SWA_KV_HEADS, SWA_GQA, 1, 1)
    span = SWA_BLOCK + 2 * SWA_WINDOW

    def block(i):
        start = i * SWA_BLOCK
        qb = lax.dynamic_slice_in_dim(qg, start, SWA_BLOCK, axis=1)
        kb = lax.dynamic_slice_in_dim(kp, start, span, axis=1)
        vb = lax.dynamic_slice_in_dim(vp, start, span, axis=1)
        qpos = start + jnp.arange(SWA_BLOCK)
        kpos = start - SWA_WINDOW + jnp.arange(span)
        valid = ((jnp.abs(qpos[:, None] - kpos[None, :]) <= SWA_WINDOW)
                 & (kpos >= 0)[None, :] & (kpos < n)[None, :])
        s_lat = jnp.einsum('bqkgd,bskd->bkgqs', qb, kb).astype(jnp.float32)
        s_lat = jnp.where(valid, s_lat, NEG_INF)
        s_ctx = jnp.einsum('bqkgd,bskd->bkgqs', qb, kc).astype(jnp.float32)
        p = _sink_softmax(jnp.concatenate([s_lat, s_ctx], axis=-1), sink_b).astype(v.dtype)
        o = (jnp.einsum('bkgqs,bskd->bqkgd', p[..., :span], vb)
             + jnp.einsum('bkgqs,bskd->bqkgd', p[..., span:], vc))
        return o.reshape(bsz, SWA_BLOCK, SWA_HEADS * HEAD_DIM)

    out = lax.map(block, jnp.arange(n // SWA_BLOCK))
    out = jnp.moveaxis(out, 0, 1).reshape(bsz, n, SWA_HEADS * HEAD_DIM)
    out_c = None
    if need_ctx:
        lc = qc.shape[1]
        qcg = (qc * scale).reshape(bsz, lc, SWA_KV_HEADS, SWA_GQA, HEAD_DIM)
        s = jnp.einsum('bqkgd,bskd->bkgqs', qcg, kc).astype(jnp.float32)
        p = _sink_softmax(s, sink_b).astype(v.dtype)
        out_c = jnp.einsum('bkgqs,bskd->bqkgd', p, vc).reshape(bsz, lc, SWA_HEADS * HEAD_DIM)
    return out, out_c


def _na_mixer(q, k, v, qc, kc, vc, rpb, rows, need_ctx):
    bsz, n = q.shape[0], q.shape[1]
    wr = min(NA_ROWS, rows)
    ncb = GRID_W // NA_QB
    scale = HEAD_DIM ** -0.5
    qgrid = (q * scale).reshape(bsz, rows, GRID_W, NA_HEADS, HEAD_DIM)
    kgrid = k.reshape(bsz, rows, GRID_W, NA_HEADS, HEAD_DIM)
    vgrid = v.reshape(bsz, rows, GRID_W, NA_HEADS, HEAD_DIM)
    row_start = jnp.clip(jnp.arange(rows) - wr // 2, 0, rows - wr)
    qcols = np.arange(GRID_W).reshape(ncb, NA_QB)
    win_start = np.clip(qcols - NA_COLS // 2, 0, GRID_W - NA_COLS)
    kcols = (np.clip(np.arange(ncb) * NA_QB - NA_COLS // 2, 0, GRID_W - NA_KB)[:, None]
             + np.arange(NA_KB)[None, :])
    col_valid = ((kcols[:, None, :] >= win_start[:, :, None])
                 & (kcols[:, None, :] < win_start[:, :, None] + NA_COLS))
    col_idx = np.clip(kcols[:, None, :] - qcols[:, :, None] + NA_COLS - 1, 0, 2 * NA_COLS - 2)
    bias = jnp.transpose(rpb.astype(jnp.float32)[:, :, col_idx], (0, 2, 3, 1, 4))
    bias = jnp.where(col_valid[None, :, :, None, :], bias, NEG_INF)
    n_lat = wr * NA_KB

    def row_block(r):
        rs = row_start[r]
        qr = qgrid[:, r].reshape(bsz, ncb, NA_QB, NA_HEADS, HEAD_DIM)
        kb = lax.dynamic_slice_in_dim(kgrid, rs, wr, axis=1)[:, :, kcols]
        vb = lax.dynamic_slice_in_dim(vgrid, rs, wr, axis=1)[:, :, kcols]
        b_r = jnp.take(bias, rs + jnp.arange(wr) - r + NA_ROWS - 1, axis=3)
        s_lat = jnp.einsum('bjqhd,brjkhd->bhjqrk', qr, kb).astype(jnp.float32) + b_r[None]
        s_lat = s_lat.reshape(bsz, NA_HEADS, ncb, NA_QB, n_lat)
        s_ctx = jnp.einsum('bjqhd,bshd->bhjqs', qr, kc).astype(jnp.float32)
        p = jax.nn.softmax(jnp.concatenate([s_lat, s_ctx], axis=-1), axis=-1).astype(v.dtype)
        p_lat = p[..., :n_lat].reshape(bsz, NA_HEADS, ncb, NA_QB, wr, NA_KB)
        o = (jnp.einsum('bhjqrk,brjkhd->bjqhd', p_lat, vb)
             + jnp.einsum('bhjqs,bshd->bjqhd', p[..., n_lat:], vc))
        return o.reshape(bsz, GRID_W, NA_HEADS * HEAD_DIM)

    out = lax.map(row_block, jnp.arange(rows))
    out = jnp.moveaxis(out, 0, 1).reshape(bsz, n, NA_HEADS * HEAD_DIM)
    out_c = None
    if need_ctx:
        lc = qc.shape[1]
        s = jnp.einsum('bqhd,bshd->bhqs', qc * scale, kc).astype(jnp.float32)
        p = jax.nn.softmax(s, axis=-1).astype(v.dtype)
        out_c = jnp.einsum('bhqs,bshd->bqhd', p, vc).reshape(bsz, lc, NA_HEADS * HEAD_DIM)
    return out, out_c


def _hgrn_forget(f_raw, lb):
    f = lb + (1.0 - lb) * jax.nn.sigmoid(f_raw)
    return 1.0 - f, jnp.log(f)


def _gla_chunked(q, k, v, logf, s0):
    bsz, n, nh = q.shape[0], q.shape[1], q.shape[2]
    nc = n // HGRN_CHUNK

    def chunks(t):
        return jnp.transpose(t.reshape(bsz, nc, HGRN_CHUNK, nh, t.shape[-1]), (1, 0, 3, 2, 4))

    causal = jnp.tril(jnp.ones((HGRN_CHUNK, HGRN_CHUNK), dtype=bool))[:, :, None]

    def step(state, blk):
        qb, kb, vb, gb = blk
        b = jnp.cumsum(gb, axis=2)
        b_last = b[:, :, -1:, :]
        diff = jnp.where(causal, b[:, :, :, None, :] - b[:, :, None, :, :], -jnp.inf)
        attn = jnp.einsum('bhtk,bhsk,bhtsk->bhts', qb, kb, jnp.exp(diff))
        o = (jnp.einsum('bhtk,bhkv->bhtv', qb * jnp.exp(b), state)
             + jnp.einsum('bhts,bhsv->bhtv', attn, vb))
        state = (jnp.exp(b_last[:, :, 0, :])[..., None] * state
                 + jnp.einsum('bhsk,bhsv->bhkv', kb * jnp.exp(b_last - b), vb))
        return state, o

    state, o = lax.scan(step, s0, (chunks(q), chunks(k), chunks(v), chunks(logf)))
    o = jnp.transpose(o, (1, 0, 3, 2, 4)).reshape(bsz, n, nh, v.shape[-1])
    return o, state


def _gated_rmsnorm(o, g, w):
    o = o * lax.rsqrt(jnp.mean(o * o, axis=-1, keepdims=True) + NORM_EPS) * w.astype(jnp.float32)
    gate = jax.nn.silu(g.astype(jnp.float32)).reshape(o.shape)
    return (o * gate).reshape(o.shape[0], o.shape[1], -1)


def _flip(t, rev):
    return jnp.flip(t, axis=1) if rev else t


def _hgrn2_mixer(q, f_fw, f_bw, i, g, qc, fc_fw, fc_bw, ic, gc, lb_fw, lb_bw, norm_w, need_ctx):
    def heads(t):
        return t.astype(jnp.float32).reshape(t.shape[0], t.shape[1], HGRN_HEADS, -1)

    qh, vh = heads(jax.nn.silu(q)), heads(i)
    qch, vch = heads(jax.nn.silu(qc)), heads(ic)
    s0 = jnp.zeros((q.shape[0], HGRN_HEADS, HGRN_DK, HGRN_DV), jnp.float32)
    o = jnp.zeros_like(vh)
    oc = jnp.zeros_like(vch) if need_ctx else None
    for rev, f_raw, fc_raw, lb in ((False, f_fw, fc_fw, lb_fw), (True, f_bw, fc_bw, lb_bw)):
        lbh = lb.reshape(HGRN_HEADS, HGRN_DK)
        kh, logf = _hgrn_forget(heads(f_raw), lbh)
        kch, logfc = _hgrn_forget(heads(fc_raw), lbh)
        o_ctx, s_ctx = _gla_chunked(_flip(qch, rev), _flip(kch, rev), _flip(vch, rev), _flip(logfc, rev), s0)
        o_lat, _ = _gla_chunked(_flip(qh, rev), _flip(kh, rev), _flip(vh, rev), _flip(logf, rev), s_ctx)
        o = o + _flip(o_lat, rev)
        if need_ctx:
            oc = oc + _flip(o_ctx, rev)
    out = _gated_rmsnorm(o, g, norm_w).astype(q.dtype)
    out_c = _gated_rmsnorm(oc, gc, norm_w).astype(q.dtype) if need_ctx else None
    return out, out_c


def _s5_discretize(lam_re, lam_im, log_dt, b_re, b_im):
    dt = jnp.exp(log_dt)[:, None]
    mag = jnp.exp(lam_re * dt)
    a_re, a_im = mag * jnp.cos(lam_im * dt), mag * jnp.sin(lam_im * dt)
    den = lam_re * lam_re + lam_im * lam_im
    k_re = ((a_re - 1.0) * lam_re + a_im * lam_im) / den
    k_im = (a_im * lam_re - (a_re - 1.0) * lam_im) / den
    bb_re = k_re[..., None] * b_re - k_im[..., None] * b_im
    bb_im = k_re[..., None] * b_im + k_im[..., None] * b_re
    return a_re, a_im, bb_re, bb_im


def _s5_scan(u, a_re, a_im, bb_re, bb_im, h0=None):
    n = u.shape[1]
    x_re = jnp.einsum('blgh,gph->blgp', u, bb_re)
    x_im = jnp.einsum('blgh,gph->blgp', u, bb_im)
    if h0 is not None:
        h_re, h_im = h0
        x_re = x_re.at[:, 0].add(a_re * h_re - a_im * h_im)
        x_im = x_im.at[:, 0].add(a_re * h_im + a_im * h_re)
    shape = (1, n) + a_re.shape

    def combine(e1, e2):
        a1r, a1i, b1r, b1i = e1
        a2r, a2i, b2r, b2i = e2
        return (a2r * a1r - a2i * a1i, a2r * a1i + a2i * a1r,
                a2r * b1r - a2i * b1i + b2r, a2r * b1i + a2i * b1r + b2i)

    _, _, x_re, x_im = lax.associative_scan(
        combine, (jnp.broadcast_to(a_re, shape), jnp.broadcast_to(a_im, shape), x_re, x_im), axis=1)
    return x_re, x_im


def _s5_readout(x_re, x_im, c_re, c_im):
    y = jnp.einsum('blgp,ghp->blgh', x_re, c_re) - jnp.einsum('blgp,ghp->blgh', x_im, c_im)
    return y.reshape(y.shape[0], y.shape[1], -1)


def _s5_glu(y, w, b):
    z = jax.nn.gelu(y)
    return z * jax.nn.sigmoid(z @ w.astype(jnp.float32) + b.astype(jnp.float32))


def _s5_mixer(u, uc, lam_re, lam_im, log_dt, b_re, b_im, c_re, c_im, d, glu_w, glu_b, need_ctx):
    f32 = jnp.float32

    def groups(t):
        return t.astype(f32).reshape(t.shape[0], t.shape[1], S5_GROUPS, S5_GROUP_CH)

    ug, ucg = groups(u), groups(uc)
    y = d.astype(f32) * u.astype(f32)
    yc = d.astype(f32) * uc.astype(f32) if need_ctx else None
    for direc in range(2):
        rev = direc == 1
        a_re, a_im, bb_re, bb_im = _s5_discretize(
            lam_re[direc].astype(f32), lam_im[direc].astype(f32), log_dt[direc].astype(f32),
            b_re[direc].astype(f32), b_im[direc].astype(f32))
        cr, ci = c_re[direc].astype(f32), c_im[direc].astype(f32)
        xc_re, xc_im = _s5_scan(_flip(ucg, rev), a_re, a_im, bb_re, bb_im)
        x_re, x_im = _s5_scan(_flip(ug, rev), a_re, a_im, bb_re, bb_im, (xc_re[:, -1], xc_im[:, -1]))
        y = y + _flip(_s5_readout(x_re, x_im, cr, ci), rev)
        if need_ctx:
            yc = yc + _flip(_s5_readout(xc_re, xc_im, cr, ci), rev)
    out = _s5_glu(y, glu_w, glu_b).astype(u.dtype)
    out_c = _s5_glu(yc, glu_w, glu_b).astype(u.dtype) if need_ctx else None
    return out, out_c


def _token_mixers(h, hc, w_in, sink, rpb, lb_fw, lb_bw, hg_norm_w, s5_params, row, col, rows, need_ctx):
    cuts = [int(v) for v in np.cumsum(PROJ_SIZES)[:-1]]
    p = jnp.split(h @ w_in, cuts, axis=-1)
    pc = jnp.split(hc @ w_in, cuts, axis=-1)

    def hd(t):
        return t.reshape(t.shape[0], t.shape[1], -1, HEAD_DIM)

    a, a_c = _swa_mixer(hd(p[0]), hd(p[1]), hd(p[2]), hd(pc[0]), hd(pc[1]), hd(pc[2]),
                        sink, row, col, need_ctx)
    b, b_c = _na_mixer(hd(p[3]), hd(p[4]), hd(p[5]), hd(pc[3]), hd(pc[4]), hd(pc[5]),
                       rpb, rows, need_ctx)
    cm, c_c = _hgrn2_mixer(p[6], p[7], p[8], p[9], p[10], pc[6], pc[7], pc[8], pc[9], pc[10],
                           lb_fw, lb_bw, hg_norm_w, need_ctx)
    dm, d_c = _s5_mixer(p[11], pc[11], *s5_params, need_ctx)
    mix = jnp.concatenate([a, b, cm, dm], axis=-1)
    mix_c = jnp.concatenate([a_c, b_c, c_c, d_c], axis=-1) if need_ctx else None
    return mix, mix_c


def _hier_moe(t, gw, gb, ew, eb, w_gate, w_up, w_down):
    g_logits = (t @ gw + gb).astype(jnp.float32)
    g_prob = jax.nn.softmax(g_logits, axis=-1)
    g_idx = jnp.argmax(g_logits, axis=-1)
    g_w = jnp.take_along_axis(g_prob, g_idx[:, None], axis=-1)
    e_logits = (t @ ew + eb).astype(jnp.float32).reshape(-1, MOE_GROUPS, MOE_PER_GROUP)
    e_in = jnp.take_along_axis(e_logits, g_idx[:, None, None], axis=1)[:, 0]
    top_v, top_i = lax.top_k(e_in, MOE_TOPK)
    w = jax.nn.softmax(top_v, axis=-1) * g_w
    eid = g_idx[:, None] * MOE_PER_GROUP + top_i
    combine = jnp.sum(jax.nn.one_hot(eid, MOE_EXPERTS, dtype=jnp.float32) * w[..., None], axis=1).astype(t.dtype)
    y = jnp.zeros_like(t)
    for e in range(MOE_EXPERTS):
        hid = jax.nn.silu(t @ w_gate[e]) * (t @ w_up[e])
        y = y + combine[:, e:e + 1] * (hid @ w_down[e])
    return y


def setup_inputs(seed: int = 0) -> dict:
    key = jax.random.key(seed)
    ks = iter(jax.random.split(key, 40))
    f32 = jnp.float32

    def nrm(shape, scale):
        return jax.random.normal(next(ks), shape, f32) * scale

    D = D_MODEL
    G, P, Hs = S5_GROUPS, S5_STATE, S5_GROUP_CH
    lam_im_base = jnp.broadcast_to(math.pi * jnp.arange(P, dtype=f32), (DEPTH, 2, G, P))
    return {
        'x': nrm((BATCH, SEQ, D), 1.0),
        'c': nrm((BATCH, D), 1.0),
        'ctx': nrm((BATCH, CTX_LEN, D), 1.0),
        'c_ctx': nrm((D,), 1.0),
        'mod_w': nrm((DEPTH, D, 6 * D), 0.5 * D ** -0.5),
        'mod_b': nrm((DEPTH, 6 * D), 0.01),
        'norm1_w': 1.0 + nrm((DEPTH, D), 0.01),
        'norm2_w': 1.0 + nrm((DEPTH, D), 0.01),
        'w_in': nrm((DEPTH, D, PROJ_WIDTH), D ** -0.5),
        'w_out': nrm((DEPTH, D, D), D ** -0.5),
        'swa_sink': nrm((DEPTH, SWA_HEADS), 0.5),
        'na_rpb': nrm((DEPTH, NA_HEADS, 2 * NA_ROWS - 1, 2 * NA_COLS - 1), 0.1),
        'hgrn_lb': nrm((2, DEPTH, GROUP_WIDTH), 0.5),
        'hgrn_norm_w': 1.0 + nrm((DEPTH, HGRN_DV), 0.01),
        's5_lam_re': -0.5 + nrm((DEPTH, 2, G, P), 0.01),
        's5_lam_im': lam_im_base + nrm((DEPTH, 2, G, P), 0.01),
        's5_log_dt': jax.random.uniform(next(ks), (DEPTH, 2, G), f32, math.log(1e-3), math.log(1e-1)),
        's5_b_re': nrm((DEPTH, 2, G, P, Hs), (2 * Hs) ** -0.5),
        's5_b_im': nrm((DEPTH, 2, G, P, Hs), (2 * Hs) ** -0.5),
        's5_c_re': nrm((DEPTH, 2, G, Hs, P), P ** -0.5),
        's5_c_im': nrm((DEPTH, 2, G, Hs, P), P ** -0.5),
        's5_d': nrm((DEPTH, GROUP_WIDTH), 1.0),
        's5_glu_w': nrm((DEPTH, GROUP_WIDTH, GROUP_WIDTH), GROUP_WIDTH ** -0.5),
        's5_glu_b': nrm((DEPTH, GROUP_WIDTH), 0.01),
        'moe_group_w': nrm((DEPTH, D, MOE_GROUPS), D ** -0.5),
        'moe_group_b': nrm((DEPTH, MOE_GROUPS), 0.01),
        'moe_expert_w': nrm((DEPTH, D, MOE_EXPERTS), D ** -0.5),
        'moe_expert_b': nrm((DEPTH, MOE_EXPERTS), 0.01),
        'moe_w_gate': nrm((DEPTH, MOE_EXPERTS, D, MOE_HIDDEN), D ** -0.5),
        'moe_w_up': nrm((DEPTH, MOE_EXPERTS, D, MOE_HIDDEN), D ** -0.5),
        'moe_w_down': nrm((DEPTH, MOE_EXPERTS, MOE_HIDDEN, D), MOE_HIDDEN ** -0.5),
        'final_norm_w': 1.0 + nrm((D,), 0.01),
    }


def reference(x, c, ctx, c_ctx, mod_w, mod_b, norm1_w, norm2_w, w_in, w_out,
              swa_sink, na_rpb, hgrn_lb, hgrn_norm_w,
              s5_lam_re, s5_lam_im, s5_log_dt, s5_b_re, s5_b_im, s5_c_re, s5_c_im, s5_d, s5_glu_w, s5_glu_b,
              moe_group_w, moe_group_b, moe_expert_w, moe_expert_b, moe_w_gate, moe_w_up, moe_w_down,
              final_norm_w):
    bsz, n_tok, dm = x.shape
    rows = n_tok // GRID_W
    t = jnp.arange(n_tok)
    row, col = t // GRID_W, t % GRID_W
    lbp = jax.nn.softmax(hgrn_lb.astype(jnp.float32), axis=1)
    lower_bound = jnp.cumsum(lbp, axis=1) - lbp[:, :1]
    c_act = jax.nn.silu(c)
    cc_act = jax.nn.silu(c_ctx)
    n_lat = bsz * n_tok
    for l in range(DEPTH):
        last = l == DEPTH - 1
        mod = jnp.split(c_act @ mod_w[l] + mod_b[l], 6, axis=-1)
        modc = jnp.split(cc_act @ mod_w[l] + mod_b[l], 6, axis=-1)
        h = _modulate(_rmsnorm(x, norm1_w[l]), mod[0][:, None], mod[1][:, None])
        hc = _modulate(_rmsnorm(ctx, norm1_w[l]), modc[0], modc[1])
        s5_params = (s5_lam_re[l], s5_lam_im[l], s5_log_dt[l], s5_b_re[l], s5_b_im[l],
                     s5_c_re[l], s5_c_im[l], s5_d[l], s5_glu_w[l], s5_glu_b[l])
        mix, mix_c = _token_mixers(h, hc, w_in[l], swa_sink[l], na_rpb[l],
                                   lower_bound[0, l], lower_bound[1, l], hgrn_norm_w[l],
                                   s5_params, row, col, rows, not last)
        x = x + mod[2][:, None] * (mix @ w_out[l])
        moe_w = (moe_group_w[l], moe_group_b[l], moe_expert_w[l], moe_expert_b[l],
                 moe_w_gate[l], moe_w_up[l], moe_w_down[l])
        h2 = _modulate(_rmsnorm(x, norm2_w[l]), mod[3][:, None], mod[4][:, None])
        if last:
            x = x + mod[5][:, None] * _hier_moe(h2.reshape(-1, dm), *moe_w).reshape(x.shape)
        else:
            ctx = ctx + modc[2] * (mix_c @ w_out[l])
            h2c = _modulate(_rmsnorm(ctx, norm2_w[l]), modc[3], modc[4])
            y = _hier_moe(jnp.concatenate([h2.reshape(-1, dm), h2c.reshape(-1, dm)], axis=0), *moe_w)
            x = x + mod[5][:, None] * y[:n_lat].reshape(x.shape)
            ctx = ctx + modc[5] * y[n_lat:].reshape(ctx.shape)
    return _rmsnorm(x, final_norm_w)
```

```python
import numpy as np
from contextlib import ExitStack
import concourse.bass as bass
import concourse.mybir as mybir
from concourse.bass_utils import run_bass_kernel_spmd

F32 = mybir.dt.float32
BF16 = mybir.dt.bfloat16
I32 = mybir.dt.int32
U32 = mybir.dt.uint32
AF = mybir.ActivationFunctionType
ALU = mybir.AluOpType
AX = mybir.AxisListType

ENG = ('pe', 'act', 'dve', 'pool', 'sp')
NDMA = 24


class P:
    def __init__(self):
        self.nc = bass.Bass("TRN2", target_bir_lowering=False)
        self.es = ExitStack()
        nc = self.nc
        self.ops = {e: [] for e in ENG}
        self.cnt = {e: 0 for e in ENG}
        self.sem = {e: self.es.enter_context(nc.semaphore("s_" + e)) for e in ENG}
        self.dsem = [self.es.enter_context(nc.semaphore("d%d" % i)) for i in range(NDMA)]
        self.duse = [0] * NDMA
        self.dnext = 0
        self.known = {e: {} for e in ENG}
        self.res = {}
        self.ntens = 0
        self.nwait = 0

    def sb(self, shape, dt=F32, name=None):
        self.ntens += 1
        return self.es.enter_context(self.nc.sbuf_tensor((name or "t") + "_s%d" % self.ntens, list(shape), dt))

    def ps(self, shape, dt=F32, name=None):
        self.ntens += 1
        return self.es.enter_context(self.nc.psum_tensor((name or "p") + "_p%d" % self.ntens, list(shape), dt))

    def dram(self, name, shape, dt=F32, kind="Internal"):
        return self.nc.dram_tensor(name, list(shape), dt, kind=kind).ap()

    def _st(self, name):
        if name not in self.res:
            self.res[name] = {'whole': [None, []], 'subs': {}}
        return self.res[name]

    def _involved(self, key):
        if isinstance(key, tuple):
            name, idx = key
        else:
            name, idx = key, None
        st = self._st(name)
        if idx is None:
            return st, idx, [st['whole']] + list(st['subs'].values())
        if idx not in st['subs']:
            st['subs'][idx] = [None, []]
        return st, idx, [st['whole'], st['subs'][idx]]

    def _deps(self, reads, writes):
        deps = []
        for k in reads:
            _, _, inv = self._involved(k)
            for s in inv:
                if s[0] is not None:
                    deps.append(s[0])
        for k in writes:
            _, _, inv = self._involved(k)
            for s in inv:
                if s[0] is not None:
                    deps.append(s[0])
                deps.extend(s[1])
        return deps

    def _commit(self, reads, writes, tok):
        for k in reads:
            st, idx, inv = self._involved(k)
            (st['whole'] if idx is None else st['subs'][idx])[1].append(tok)
        for k in writes:
            st, idx, inv = self._involved(k)
            if idx is None:
                st['subs'] = {}
                st['whole'] = [tok, []]
            else:
                st['subs'][idx] = [tok, []]

    def _emit_waits(self, e, deps):
        kn = self.known[e]
        need = {}
        for (sk, v) in deps:
            if e == 'pe' and sk == 'pe':
                continue
            if kn.get(sk, 0) >= v:
                continue
            if need.get(sk, 0) < v:
                need[sk] = v
        for sk, v in need.items():
            kn[sk] = v
            sem = self.sem[sk] if isinstance(sk, str) else self.dsem[sk]
            self.ops[e].append(('wait', sem, v))
            self.nwait += 1

    def op(self, e, fn, reads=(), writes=(), sig=True):
        deps = self._deps(reads, writes)
        self._emit_waits(e, deps)
        if sig:
            self.cnt[e] += 1
            tok = (e, self.cnt[e])
        else:
            tok = (e, self.cnt[e] + 1)
        self.ops[e].append(('op', fn, sig))
        self._commit(reads, writes, tok)

    def i(self, e, method, *args, reads=(), writes=(), sig=True, **kwargs):
        self.op(e, lambda eng: getattr(eng, method)(*args, **kwargs), reads=reads, writes=writes, sig=sig)

    def dma(self, q, out, in_, reads=(), writes=(), **kw):
        deps = self._deps(reads, writes)
        j = self.dnext
        self.dnext = (self.dnext + 1) % NDMA
        if self.duse[j] > 0:
            deps.append((j, 16 * self.duse[j]))
        self._emit_waits(q, deps)
        self.duse[j] += 1
        tok = (j, 16 * self.duse[j])
        self.ops[q].append(('dma', out, in_, self.dsem[j], kw))
        self._commit(reads, writes, tok)

    def barrier(self):
        for e in ENG:
            deps = [(f, self.cnt[f]) for f in ENG if self.cnt[f] > 0]
            deps += [(j, 16 * self.duse[j]) for j in range(NDMA) if self.duse[j] > 0]
            self._emit_waits(e, deps)

    def phase(self):
        return _Phase(self)

    def flush(self):
        nc = self.nc
        if not any(self.ops[e] for e in ENG):
            return
        engs = {'pe': 'tensor', 'act': 'scalar', 'dve': 'vector', 'pool': 'gpsimd', 'sp': 'sync'}
        with nc.Block() as block:
            for e in ENG:
                lst = self.ops[e]
                semE = self.sem[e]

                def body(eng, lst=lst, semE=semE):
                    for it in lst:
                        if it[0] == 'wait':
                            eng.wait_ge(it[1], it[2])
                        elif it[0] == 'op':
                            inst = it[1](eng)
                            if it[2]:
                                inst.then_inc(semE, 1)
                        else:
                            eng.dma_start(out=it[1], in_=it[2], **it[4]).then_inc(it[3], 16)
                getattr(block, engs[e])(body)
        self.ninst = getattr(self, 'ninst', 0) + sum(len(self.ops[e]) for e in ENG)
        self.ops = {e: [] for e in ENG}

    def finish(self, final_keys):
        deps = self._deps(final_keys, [])
        self._emit_waits('sp', deps)
        self.flush()
        self.es.close()
        return self.nc


class _Phase:
    def __init__(self, p):
        self.p = p

    def __enter__(self):
        self.saved = self.p.es
        self.p.es = ExitStack()
        return self

    def __exit__(self, *a):
        self.p.barrier()
        self.p.flush()
        self.p.es.close()
        self.p.es = self.saved
        return False


import math

NB = 4
T = 2304
NT = 18
LC = 256
NL = 2048
D = 1024
NFM = 20
NTM = 896
BLKS = [(0, 256), (256, 512), (768, 512), (1280, 512), (1792, 512)]


def win_perm():
    A, B, C, Dd = 0, 512, 1280, 2560
    def hd(base, h): return list(range(base + 64 * h, base + 64 * h + 64))
    def sw(cols):
        c = np.array(cols)
        idx = np.concatenate([np.arange(16, 32), np.arange(0, 16), np.arange(48, 64), np.arange(32, 48)])
        return list(c[idx])
    fm = []
    fm += hd(A, 0) + hd(A, 2)
    fm += hd(A, 1) + hd(A, 3)
    fm += sw(hd(A, 0)) + sw(hd(A, 2))
    fm += sw(hd(A, 1)) + sw(hd(A, 3))
    fm += hd(A + 256, 0) + hd(A + 256, 1)
    fm += sw(hd(A + 256, 0)) + sw(hd(A + 256, 1))
    fm += list(range(B, B + 256))
    fm += list(range(B + 256, B + 512))
    fm += list(range(C, C + 256))
    fm += list(range(C + 256, C + 512))
    fm += list(range(C + 512, C + 768))
    fm += list(range(C + 1024, C + 1280))
    fm += list(range(Dd, Dd + 256))
    assert len(fm) == NFM * 128
    tm = list(range(A + 384, A + 512)) + list(range(B + 512, B + 768)) + list(range(C + 768, C + 1024)) + list(range(Dd, Dd + 256))
    assert len(tm) == NTM
    return np.array(fm + tm)


def rope_tables():
    half = 32
    inv = 1.0 / (10000.0 ** (np.arange(0, half, 2, dtype=np.float32) / half))
    t = np.arange(NL)
    row = (t // 64).astype(np.float32); col = (t % 64).astype(np.float32)
    ar = row[:, None] * inv[None, :]; ac = col[:, None] * inv[None, :]
    C = np.zeros((64, NL), np.float32); S = np.zeros((64, NL), np.float32)
    C[0:16] = np.cos(ar).T; C[16:32] = np.cos(ar).T; C[32:48] = np.cos(ac).T; C[48:64] = np.cos(ac).T
    S[0:16] = -np.sin(ar).T; S[16:32] = np.sin(ar).T; S[32:48] = -np.sin(ac).T; S[48:64] = np.sin(ac).T
    return np.concatenate([C, C], 0), np.concatenate([S, S], 0)


def host_prep(inp, core, nb=NB):
    f = np.float32
    b0 = core * nb
    m = {}
    x = inp['x'][b0:b0 + nb]; ctx = inp['ctx'][b0:b0 + nb]
    xt = np.concatenate([ctx, x], axis=1)
    m['xT'] = np.ascontiguousarray(xt.transpose(0, 2, 1)).astype(f)
    cc = np.concatenate([inp['c'][b0:b0 + nb], inp['c_ctx'][None, :]], 0)
    m['cT'] = np.ascontiguousarray(cc.T.reshape(8, 128, nb + 1).transpose(1, 0, 2)).astype(f)
    return m


def host_shared(inp):
    f = np.float32
    m = {}
    m['mod_w'] = inp['mod_w'].astype(f)
    m['mod_b2'] = np.ascontiguousarray(inp['mod_b'].reshape(2, 48, 128).transpose(0, 2, 1)).astype(f)
    for k in ('norm1_w', 'norm2_w'):
        m[k + '2'] = np.ascontiguousarray(inp[k].reshape(2, 8, 128).transpose(0, 2, 1)).astype(f)
    m['fnw2'] = np.ascontiguousarray(inp['final_norm_w'].reshape(8, 128).T).astype(f)
    m['w_in2'] = np.ascontiguousarray(inp['w_in'][:, :, win_perm()]).astype(f)
    m['w_out'] = inp['w_out'].astype(f)
    C, S = rope_tables()
    m['ropeC'] = C; m['ropeS'] = S
    import ml_dtypes
    bf = ml_dtypes.bfloat16
    i = np.arange(128)[:, None]; j = np.arange(128)[None, :]
    BIG = -240000.0
    mk = np.zeros((128, 384), f)
    mk[:, 0:128] = np.where(i <= j, 0.0, BIG)
    mk[:, 256:384] = np.where(j <= i, 0.0, BIG)
    m['swa_mask'] = mk.astype(bf)
    m['ident_bf'] = np.eye(128, dtype=f).astype(bf)
    m['ident_f'] = np.eye(128, dtype=f)
    m['swa_sink'] = inp['swa_sink'].astype(f)
    rp = np.zeros((2, 4, 15, 127), f); rp[:, :, :, 48:79] = inp['na_rpb']
    same = (i // 32) == (j // 32)
    m['hg_mask'] = np.stack([(same & (i <= j)), (same & (i >= j))]).astype(f).astype(bf)
    bo = np.zeros((128, 128), f); bo[:64, :64] = 1; bo[64:, 64:] = 1
    m['blockones'] = bo.astype(bf)
    m['hgrn_lb2'] = np.ascontiguousarray(inp['hgrn_lb'].reshape(2, 2, 2, 128).transpose(3, 0, 1, 2)).astype(f)
    G_, P_, H_ = 16, 64, 16
    def pdup(a):
        t = a.transpose(0, 3, 1, 2)
        return np.ascontiguousarray(np.concatenate([t, t], 1)).astype(f)
    m['s5_lre_p'] = pdup(inp['s5_lam_re']); m['s5_lim_p'] = pdup(inp['s5_lam_im'])
    m['s5_ldt_p'] = np.ascontiguousarray(np.broadcast_to(inp['s5_log_dt'][:, None, :, :], (2, 128, 2, 16))).astype(f)
    m['s5_lre_r'] = np.ascontiguousarray(inp['s5_lam_re'].reshape(2, 2, 1024)).astype(f)
    m['s5_lim_r'] = np.ascontiguousarray(inp['s5_lam_im'].reshape(2, 2, 1024)).astype(f)
    m['s5_ldt_r'] = np.ascontiguousarray(np.repeat(inp['s5_log_dt'], 64, axis=-1)).astype(f)
    def bemb(a):
        o = np.zeros((2, 2, 128, 16, 64), f)
        for g in range(16):
            o[:, :, (g % 8) * 16:(g % 8) * 16 + 16, g, :] = a[:, :, g].transpose(0, 1, 3, 2)
        return o
    m['s5_Br_emb'] = bemb(inp['s5_b_re']); m['s5_Bi_emb'] = bemb(inp['s5_b_im'])
    def cdup(a):
        t = a.transpose(0, 1, 4, 2, 3)
        return np.ascontiguousarray(np.concatenate([t, t], 2)).astype(f)
    m['s5_Cr2'] = cdup(inp['s5_c_re']); m['s5_Ci2'] = cdup(inp['s5_c_im'])
    m['s5_d2'] = np.ascontiguousarray(inp['s5_d'].reshape(2, 2, 128).transpose(0, 2, 1)).astype(f)
    m['s5_glu_w'] = inp['s5_glu_w'].astype(f)
    m['s5_glu_b2'] = np.ascontiguousarray(inp['s5_glu_b'].reshape(2, 2, 128).transpose(0, 2, 1)).astype(f)
    m['svec'] = np.ascontiguousarray(np.broadcast_to(np.arange(1, 129, dtype=f)[None, :], (128, 128)))
    m['Jrev128'] = np.ascontiguousarray(np.eye(128, dtype=f)[::-1]).astype(bf)
    msw = np.zeros((128, 128), f)
    for mm_ in range(64):
        msw[mm_ + 64, mm_] = -1.0
        msw[mm_, mm_ + 64] = 1.0
    m['MswT'] = msw
    es = np.zeros((32, 32, 128), f)
    for e_ in range(32):
        es[e_, e_, :] = 1.0
    m['esel'] = es.astype(bf)
    m['moe_rw'] = np.ascontiguousarray(np.concatenate([inp['moe_group_w'], inp['moe_expert_w']], -1)).astype(f)
    m['moe_rb'] = np.ascontiguousarray(np.concatenate([inp['moe_group_b'], inp['moe_expert_b']], -1)).astype(f)
    m['moe_w_gate'] = inp['moe_w_gate'].astype(f); m['moe_w_up'] = inp['moe_w_up'].astype(f); m['moe_w_down'] = inp['moe_w_down'].astype(f)
    m['hgrn_nw2'] = np.ascontiguousarray(np.concatenate([inp['hgrn_norm_w'], inp['hgrn_norm_w']], 1).T).astype(f)
    m['rpb_pad'] = rp
    m['Jrev'] = np.ascontiguousarray(np.eye(64, dtype=f)[::-1])
    kc = np.arange(64)[:, None]; qc = np.arange(64)[None, :]
    ws = np.clip(qc - 8, 0, 48)
    m['na_colmask'] = np.where((kc >= ws) & (kc < ws + 16), 0.0, BIG).astype(f)
    return m


def build(nb=NB, stages=('mod', 'norm1', 'proj'), dbg=(), nlayers=1):
    p = P(); nc = p.nc
    di = {}
    def din(name, shape, dt=F32):
        di[name] = p.dram(name, shape, dt, 'ExternalInput'); return di[name]
    xT_d = din('xT', [nb, D, T]); cT_d = din('cT', [128, 8, nb + 1])
    mod_w = din('mod_w', [2, D, 6 * D]); mod_b2 = din('mod_b2', [2, 128, 48])
    n1w = din('norm1_w2', [2, 128, 8]); n2w = din('norm2_w2', [2, 128, 8]); fnw = din('fnw2', [128, 8])
    w_in2 = din('w_in2', [2, D, NFM * 128 + NTM]); w_out = din('w_out', [2, D, D])
    ropeC_d = din('ropeC', [128, NL]); ropeS_d = din('ropeS', [128, NL])
    swa_mask_d = din('swa_mask', [128, 384], BF16); ident_bf_d = din('ident_bf', [128, 128], BF16)
    ident_f_d = din('ident_f', [128, 128]); swa_sink_d = din('swa_sink', [2, 4])
    rpb_pad_d = din('rpb_pad', [2, 4, 15, 127]); Jrev_d = din('Jrev', [64, 64]); na_cm_d = din('na_colmask', [64, 64])
    hg_mask_d = din('hg_mask', [2, 128, 128], BF16); blockones_d = din('blockones', [128, 128], BF16)
    hgrn_lb_d = din('hgrn_lb2', [128, 2, 2, 2]); hgrn_nw_d = din('hgrn_nw2', [128, 2])
    s5 = {}
    for nm, shp, dt_ in (('s5_lre_p', [2, 128, 2, 16], F32), ('s5_lim_p', [2, 128, 2, 16], F32), ('s5_ldt_p', [2, 128, 2, 16], F32),
                         ('s5_lre_r', [2, 2, 1024], F32), ('s5_lim_r', [2, 2, 1024], F32), ('s5_ldt_r', [2, 2, 1024], F32),
                         ('s5_Br_emb', [2, 2, 128, 16, 64], F32), ('s5_Bi_emb', [2, 2, 128, 16, 64], F32),
                         ('s5_Cr2', [2, 2, 128, 16, 16], F32), ('s5_Ci2', [2, 2, 128, 16, 16], F32),
                         ('s5_d2', [2, 128, 2], F32), ('s5_glu_w', [2, 256, 256], F32), ('s5_glu_b2', [2, 128, 2], F32),
                         ('svec', [128, 128], F32), ('Jrev128', [128, 128], BF16), ('MswT', [128, 128], F32)):
        s5[nm] = din(nm, shp, dt_)
    esel_d = din('esel', [32, 32, 128], BF16); moe_rw = din('moe_rw', [2, D, 36]); moe_rb = din('moe_rb', [2, 36])
    mwg = din('moe_w_gate', [2, 32, D, 512]); mwu = din('moe_w_up', [2, 32, D, 512]); mwd = din('moe_w_down', [2, 32, 512, D])
    mg_bf = p.dram('mg_bf', [2, 32, D, 512], BF16); mu_bf = p.dram('mu_bf', [2, 32, D, 512], BF16); md_bf = p.dram('md_bf', [2, 32, 512, D], BF16)
    wout_bf = p.dram('wout_bf', [2, D, D], BF16)
    outT = p.dram('outT', [nb, D, NL], F32, 'ExternalOutput')
    dbg_o = {}
    def dout(name, shape, dt=F32):
        dbg_o[name] = p.dram('dbg_' + name, shape, dt, 'ExternalOutput'); return dbg_o[name]
    win_bf = p.dram('win_bf', [2, D, NFM * 128 + NTM], BF16)
    Pfm = p.dram('Pfm', [nb, NFM * 128, T], BF16)
    Ptm = p.dram('Ptm', [nb, T, NTM], BF16)
    NJ = nb + 1

    modT = [p.sb([128, 48, NJ], F32, 'modT%d' % l) for l in range(2)]
    g1 = [p.sb([128, 8, NJ], F32, 'g1_%d' % l) for l in range(2)]
    g2 = [p.sb([128, 8, NJ], F32, 'g2_%d' % l) for l in range(2)]
    ones_f = p.sb([128, 128], F32, 'ones_f')
    p.i('dve', 'memset', ones_f[:], 1.0, writes=['ones_f'])
    ones_b = p.sb([128, 128], BF16, 'ones_b')
    p.i('dve', 'memset', ones_b[:], 1.0, writes=['ones_b'])
    ident_b = p.sb([128, 128], BF16, 'ident_b'); ident_f = p.sb([128, 128], F32, 'ident_f')
    p.dma('sp', ident_b[:], ident_bf_d[:, :], writes=['ident_b'])
    p.dma('sp', ident_f[:], ident_f_d[:, :], writes=['ident_f'])

    with p.phase():
        c_sb = p.sb([128, 8, NJ]); sc = p.sb([128, 8, NJ])
        mb = p.sb([128, 2, 48]); nw1 = p.sb([128, 2, 8]); nw2 = p.sb([128, 2, 8])
        p.dma('sp', c_sb[:], cT_d[:, :, :], writes=['c_sb'])
        p.dma('sp', mb[:], mod_b2.rearrange("l p n -> p l n"), writes=['mb'])
        p.dma('sp', nw1[:], n1w.rearrange("l p n -> p l n"), writes=['nw1'])
        p.dma('sp', nw2[:], n2w.rearrange("l p n -> p l n"), writes=['nw2'])
        p.i('act', 'activation', out=sc[:], in_=c_sb[:], func=AF.Silu, reads=['c_sb'], writes=['sc'])
        mwb = [p.sb([128, 8, 512], F32, 'mw%d' % i) for i in range(2)]
        mps = p.ps([128, 48, 8], F32, 'mps')
        for l in range(2):
            mwv = mod_w[l].rearrange("(c p) n -> p c n", p=128)
            for g in range(12):
                mw = mwb[g % 2]
                p.dma('sp' if g % 2 == 0 else 'pool', mw[:], mwv[:, :, g * 512:(g + 1) * 512], writes=[('mw', g % 2)])
                for j in range(4):
                    for kc in range(8):
                        p.i('pe', 'matmul', mps[:, g * 4 + j, 0:NJ], lhsT=mw[:, kc, j * 128:(j + 1) * 128], rhs=sc[:, kc, :],
                            start=(kc == 0), stop=(kc == 7), reads=[('mw', g % 2), 'sc'], writes=['mps'], sig=(kc == 7))
            p.i('dve', 'tensor_tensor', out=modT[l][:], in0=mps[:, :, 0:NJ],
                in1=mb[:, l, :].unsqueeze(2).to_broadcast([128, 48, NJ]), op=ALU.add,
                reads=['mps', 'mb'], writes=['modT%d' % l])
            for (gg, nw, mi, nm) in ((g1, nw1, 1, 'g1_'), (g2, nw2, 4, 'g2_')):
                p.i('dve', 'scalar_tensor_tensor', out=gg[l][:], in0=modT[l][:, mi * 8:(mi + 1) * 8, :], scalar=1.0,
                    in1=nw[:, l, :].unsqueeze(2).to_broadcast([128, 8, NJ]), op0=ALU.add, op1=ALU.mult,
                    reads=['modT%d' % l, 'nw1', 'nw2'], writes=[nm + str(l)])
        if 'mod' in dbg:
            d_ = dout('modT0', [128, 48, NJ])
            p.dma('sp', d_[:, :, :], modT[0][:], reads=['modT0'], writes=['dbg_modT0'])

    if 'proj' in stages:
        with p.phase():
            st = [p.sb([128, NFM * 128 + NTM], F32, 'wst%d' % i) for i in range(2)]
            sb_ = [p.sb([128, NFM * 128 + NTM], BF16, 'wsb%d' % i) for i in range(2)]
            k = 0
            for l in range(nlayers):
                for c in range(8):
                    i = k % 2; k += 1
                    p.dma('sp', st[i][:], w_in2[l, c * 128:(c + 1) * 128, :], writes=[('wst', i)])
                    p.i('dve' if c % 2 == 0 else 'pool', 'tensor_copy', out=sb_[i][:], in_=st[i][:],
                        reads=[('wst', i)], writes=[('wsb', i)])
                    p.dma('pool', win_bf[l, c * 128:(c + 1) * 128, :], sb_[i][:], reads=[('wsb', i)], writes=['win_bf'])

    XT = p.sb([128, 8, T], F32, 'XT')
    G = {}

    GTds = [p.sb([128, 4, 15, 64], BF16, 'GTd0')] * 2

    def na_prep(l):
        GTd = GTds[l]
        with p.phase():
            Hk = p.sb([64, 60, 64], F32, 'naH'); Jr = p.sb([64, 64], F32, 'naJ'); cm = p.sb([64, 64], F32, 'nacm')
            for hh in range(4):
                src = bass.AP(tensor=rpb_pad_d.tensor, offset=(l * 4 + hh) * 15 * 127, ap=[[1, 64], [127, 15], [1, 64]])
                p.dma('sp', Hk[:, hh * 15:(hh + 1) * 15, :], src, writes=['naH'])
            p.dma('sp', Jr[:], Jrev_d[:, :], writes=['naJ'])
            p.dma('sp', cm[:], na_cm_d[:, :], writes=['nacm'])
            tp = [p.ps([64, 8, 64], F32, 'natp%d' % i) for i in range(2)]
            k = 0
            for h in range(4):
                for d0 in (0, 8):
                    nd = min(8, 15 - d0)
                    ti = k % 2; k += 1
                    for j in range(nd):
                        dr = d0 + j
                        p.i('pe', 'matmul', tp[ti][:, j, :], lhsT=Hk[:, h * 15 + dr, :], rhs=Jr[:], start=True, stop=True,
                            reads=['naH', 'naJ'], writes=[('natp', ti)], sig=(j == nd - 1))
                    for j in range(nd):
                        dr = d0 + j
                        p.i('dve', 'scalar_tensor_tensor', out=GTd[(h % 2) * 64:(h % 2) * 64 + 64, h, 14 - dr, :], in0=tp[ti][:, j, :], scalar=8.0, in1=cm[:],
                            op0=ALU.mult, op1=ALU.add, reads=[('natp', ti), 'nacm'], writes=['GTd'])

    def na(l, b, need_ctx):
        rs_ = [min(max(qr - 4, 0), 24) for qr in range(32)]
        rng = []
        for kr in range(32):
            qs = [qr for qr in range(32) if rs_[qr] <= kr <= rs_[qr] + 7]
            assert qs == list(range(qs[0], qs[-1] + 1))
            rng.append((qs[0], qs[-1]))
        GTd = GTds[l]
        with p.phase():
            qg = [p.sb([128, T], BF16, 'naq%d' % i) for i in range(2)]
            kg = [p.sb([128, T], BF16, 'nak%d' % i) for i in range(2)]
            V = p.sb([128, 2, 256], BF16, 'nav')
            Vr = p.sb([128, 36, 256], BF16, 'navr')
            p.dma('sp', qg[0][:], Pfm[b, 768:896, :], reads=['Pfm'], writes=['naq0'])
            p.dma('pool', qg[1][:], Pfm[b, 896:1024, :], reads=['Pfm'], writes=['naq1'])
            p.dma('sp', kg[0][:], Pfm[b, 1024:1152, :], reads=['Pfm'], writes=['nak0'])
            p.dma('pool', kg[1][:], Pfm[b, 1152:1280, :], reads=['Pfm'], writes=['nak1'])
            p.dma('sp', V[:], Ptm[b, 0:256, :].rearrange("(n p) c -> p n c", p=128)[:, :, 128:384], reads=['Ptm'], writes=['nav'])
            p.dma('sp', Vr[0:64], Ptm[b].rearrange("(n p) c -> p n c", p=64)[:, :, 128:384], reads=['Ptm'], writes=['nav'])
            O = p.ps([64, 1024], F32, 'naO'); Dn = p.ps([64, 1024], F32, 'naD')
            st = [p.ps([128, 512], F32, 'nast%d' % i) for i in range(2)]
            pt = [p.sb([128, 512], BF16, 'napt%d' % i) for i in range(3)]
            rd = p.sb([64, 1024], F32, 'nard')
            cnt = [0]
            GTf = GTd[:].rearrange('p h d c -> p h (d c)')
            for h in range(4):
                qT = qg[h // 2]; kT = kg[h // 2]; base = (h % 2) * 64
                qsets = [(LC + 1024 * qh, 1024, True, qh) for qh in range(2)]
                if need_ctx:
                    qsets.append((0, LC, False, None))
                for (tq0, nqs, lat, qh) in qsets:
                    started = [False] * ((nqs + 511) // 512)

                    def pv(ptb, pti, vl, a, bnd, pb):
                        c = a
                        while c < bnd:
                            e_ = min(bnd, (c // 512 + 1) * 512)
                            bk = c // 512
                            stt = not started[bk]
                            if stt:
                                assert c % 512 == 0 and e_ - c == min(512, nqs - c)
                                started[bk] = True
                            K = vl.shape[0]
                            p.i('pe', 'matmul', O[:, c:e_], lhsT=vl, rhs=ptb[pb:pb + K, c - a:e_ - a],
                                start=stt, stop=False, reads=['nav', ('napt', pti)], writes=['naO'], sig=False, skip_group_check=True)
                            p.i('pe', 'matmul', Dn[:, c:e_], lhsT=ones_b[pb:pb + K, 0:64], rhs=ptb[pb:pb + K, c - a:e_ - a],
                                start=stt, stop=False, reads=['ones_b', ('napt', pti)], writes=['naD'], sig=True, skip_group_check=True)
                            c = e_

                    for ct in range(2):
                        for c0 in range(0, nqs, 512):
                            n = min(512, nqs - c0)
                            si = cnt[0] % 2; pi = cnt[0] % 3; cnt[0] += 1
                            p.i('pe', 'matmul', st[si][:, 0:n], lhsT=kT[base:base + 64, ct * 128:(ct + 1) * 128],
                                rhs=qT[base:base + 64, tq0 + c0:tq0 + c0 + n], start=True, stop=True,
                                reads=['nak%d' % (h // 2), 'naq%d' % (h // 2)], writes=[('nast', si)])
                            p.i('act', 'activation', out=pt[pi][:, 0:n], in_=st[si][:, 0:n], func=AF.Exp, scale=0.125,
                                reads=[('nast', si)], writes=[('napt', pi)])
                            pv(pt[pi], pi, V[:, ct, h * 64:(h + 1) * 64], c0, c0 + n, 0)
                    import os
                    NADBG = os.environ.get('NADBG', '')
                    if lat and 'nolat' not in NADBG:
                        for kr in range(0, 32, 2 if 'even' in NADBG else 1):
                            qlo, qhi = rng[kr]
                            for (ra, rb) in ((16 * qh, 16 * qh + 7), (16 * qh + 8, 16 * qh + 15)):
                                r0 = max(qlo, ra); r1 = min(qhi, rb)
                                if r0 > r1:
                                    continue
                                n = (r1 - r0 + 1) * 64
                                pb = 0
                                ktok = LC + kr * 64
                                qa = LC + r0 * 64
                                d0 = r0 - kr + 7
                                si = cnt[0] % 2; pi = cnt[0] % 3; cnt[0] += 1
                                p.i('pe', 'matmul', st[si][pb:pb + 64, 0:n], lhsT=kT[base:base + 64, ktok:ktok + 64],
                                    rhs=qT[base:base + 64, qa:qa + n], start=True, stop=False,
                                    reads=['nak%d' % (h // 2), 'naq%d' % (h // 2)], writes=[('nast', si)], sig=False)
                                if 'nobias' in NADBG:
                                    p.i('pe', 'matmul', st[si][pb:pb + 64, 0:n], lhsT=kT[base:base + 64, ktok:ktok + 64],
                                        rhs=qT[base:base + 64, qa:qa + n], start=False, stop=True,
                                        reads=['nak%d' % (h // 2), 'naq%d' % (h // 2)], writes=[('nast', si)])
                                else:
                                    p.i('pe', 'matmul', st[si][pb:pb + 64, 0:n], lhsT=ident_b[base:base + 64, base:base + 64],
                                        rhs=GTf[base:base + 64, h, d0 * 64:(d0 + r1 - r0 + 1) * 64], start=False, stop=True,
                                        reads=['ident_b', 'GTd'], writes=[('nast', si)])
                                p.i('act', 'activation', out=pt[pi][pb:pb + 64, 0:n], in_=st[si][pb:pb + 64, 0:n], func=AF.Exp, scale=0.125,
                                    reads=[('nast', si)], writes=[('napt', pi)])
                                a = (r0 - 16 * qh) * 64
                                pv(pt[pi], pi, Vr[0:64, 4 + kr, h * 64:(h + 1) * 64], a, a + n, pb)
                    p.i('dve', 'reciprocal', out=rd[:, 0:nqs], in_=Dn[:, 0:nqs], reads=['naD'], writes=['nard'])
                    ob = (h % 2) * 64
                    p.i('dve', 'tensor_tensor', out=G['MIXT'][ob:ob + 64, 2 + h // 2, tq0:tq0 + nqs], in0=O[:, 0:nqs], in1=rd[:, 0:nqs], op=ALU.mult,
                        reads=['naO', 'nard'], writes=[('MIXT', 2 + h // 2)])

    def hgrn(l, b):
        NCH = T // 32
        orders = [list(range(NCH)), list(range(7, -1, -1)) + list(range(NCH - 1, 7, -1))]
        import os
        HGDBG = os.environ.get('HGDBG', '')
        PL = 'dve' if 'nopool' in HGDBG else 'pool'
        with p.phase():
            msk = p.sb([128, T // 2], F32, 'hgmsk')
            p.i(PL, 'memset', msk[:], 1.0, writes=['hgmsk'])
            p.i(PL, 'memset', msk[:].rearrange("p (c k) -> p c k", k=32)[:, :, 0:1], 0.0, writes=['hgmsk'])
            hm = p.sb([128, 2, 128], BF16, 'hgm'); bo = p.sb([128, 128], BF16, 'hgbo')
            p.dma('sp', hm[:], hg_mask_d.rearrange("d s t -> s d t"), writes=['hgm'])
            p.dma('sp', bo[:], blockones_d[:, :], writes=['hgbo'])
            lbr = p.sb([128, 2, 2, 2], F32, 'hglbr'); lbv = p.sb([128, 2, 2], F32, 'hglb'); oml = p.sb([128, 2, 2], F32, 'hgoml')
            nw = p.sb([128, 2], F32, 'hgnw')
            p.dma('sp', lbr[:], hgrn_lb_d[:, :, :, :], writes=['hglbr'])
            p.dma('sp', nw[:], hgrn_nw_d[:, :], writes=['hgnw'])
            if l == 0:
                p.i('dve', 'memset', lbv[:], 0.0, writes=['hglb'])
            else:
                p.i('dve', 'tensor_tensor', out=lbv[:], in0=lbr[:, :, 1, :], in1=lbr[:, :, 0, :], op=ALU.subtract, reads=['hglbr'], writes=['hglb'])
                p.i('act', 'activation', out=lbv[:], in_=lbv[:], func=AF.Sigmoid, reads=['hglb'], writes=['hglb'])
            p.i('dve', 'tensor_scalar', out=oml[:], in0=lbv[:], scalar1=-1.0, scalar2=1.0, op0=ALU.mult, op1=ALU.add, reads=['hglb'], writes=['hgoml'])
            HN = T // 2
            tA = p.sb([128, HN], F32, 'hgA'); tB = p.sb([128, HN], F32, 'hgB'); tC = p.sb([128, HN], F32, 'hgC'); tD = p.sb([128, HN], F32, 'hgD')
            zb = p.sb([128, HN], BF16, 'hgz')
            sq_ = p.sb([128, T], BF16, 'hgsq')
            qt = [p.sb([128, T], BF16, 'hgqt%d' % d) for d in range(2)]
            kt = [p.sb([128, T], BF16, 'hgkt%d' % d) for d in range(2)]
            Sall = [p.sb([128, NCH, 64], BF16, 'hgS%d' % d) for d in range(2)]
            KhT = p.sb([128, T], BF16, 'hgKhT'); Khtm = p.sb([128, NT, 128], BF16, 'hgKhtm')
            Vc = p.sb([128, NT, 128], BF16, 'hgV')
            Dc = p.sb([128, NCH], F32, 'hgDc')
            Srun = [p.sb([128, 64], F32, 'hgSr%d' % i) for i in range(2)]
            gsb = p.sb([128, 512], BF16, 'hgg')
            tpa = p.ps([128, 128], F32, 'hgtpa'); tpb = p.ps([128, 128], F32, 'hgtpb'); tps = [tpa[:], tpb[:]]
            Ups = [p.ps([128, 512], F32, 'hgU%d' % i) for i in range(2)]
            Ops = [p.ps([128, 512], F32, 'hgO%d' % i) for i in range(2)]
            Aps = tps
            SSp = p.ps([128, 512], F32, 'hgSS')
            attn = [p.sb([128, 128], BF16, 'hgat%d' % i) for i in range(3)]
            n1 = p.sb([128, 512], F32, 'hgn1'); n2 = p.sb([128, 512], F32, 'hgn2'); n3 = p.sb([128, 512], BF16, 'hgn3')
            for ct in range(2):
                for hf in range(2):
                    t0 = hf * HN
                    p.dma('sp', zb[:], Pfm[b, (10 + ct) * 128:(11 + ct) * 128, t0:t0 + HN], reads=['Pfm'], writes=['hgz'])
                    p.i('act', 'activation', out=sq_[:, t0:t0 + HN], in_=zb[:], func=AF.Silu, reads=['hgz'], writes=['hgsq'])
                p.dma('pool', Vc[:], Ptm[b].rearrange("(n p) c -> p n c", p=128)[:, :, 384 + ct * 128:512 + ct * 128], reads=['Ptm'], writes=['hgV'])
                for d in range(2):
                    lb_ap = lbv[:, d, ct:ct + 1]; oml_ap = oml[:, d, ct:ct + 1]
                    for hf in range(2):
                        t0 = hf * HN; c0 = t0 // 32; ncq = HN // 32
                        p.dma('sp', zb[:], Pfm[b, (12 + 2 * d + ct) * 128:(13 + 2 * d + ct) * 128, t0:t0 + HN], reads=['Pfm'], writes=['hgz'])
                        p.i('act', 'activation', out=tA[:], in_=zb[:], func=AF.Sigmoid, reads=['hgz'], writes=['hgA'])
                        p.i('dve', 'tensor_scalar', out=tA[:], in0=tA[:], scalar1=oml_ap, scalar2=lb_ap, op0=ALU.mult, op1=ALU.add,
                            reads=['hgA', 'hglb', 'hgoml'], writes=['hgA'])
                        p.i('act', 'activation', out=tB[:], in_=tA[:], func=AF.Ln, reads=['hgA'], writes=['hgB'])
                        p.i('dve', 'tensor_tensor_scan', out=tC[:], data0=msk[:, 0:HN], data1=tB[:], initial=0.0, op0=ALU.mult, op1=ALU.add,
                            reads=['hgmsk', 'hgB'], writes=['hgC'])
                        p.i(PL, 'tensor_scalar', out=tA[:], in0=tA[:], scalar1=-1.0, scalar2=1.0, op0=ALU.mult, op1=ALU.add,
                            reads=['hgA'], writes=['hgA'])
                        C3 = tC[:].rearrange("p (c k) -> p c k", k=32)
                        totb = C3[:, :, 31:32].to_broadcast([128, ncq, 32])
                        p.i('act', 'activation', out=Dc[:, c0:c0 + ncq], in_=C3[:, :, 31], func=AF.Exp, reads=['hgC'], writes=['hgDc'])
                        B3 = tB[:].rearrange("p (c k) -> p c k", k=32); D3 = tD[:].rearrange("p (c k) -> p c k", k=32)
                        if d == 0:
                            p.i('dve', 'tensor_tensor', out=B3, in0=totb, in1=C3, op=ALU.subtract, reads=['hgC'], writes=['hgB'])
                            b_t, bl_t, bk, blk = tC, tB, 'hgC', 'hgB'
                        else:
                            p.i('dve', 'tensor_tensor', out=tB[:], in0=tC[:], in1=tB[:], op=ALU.subtract, reads=['hgC', 'hgB'], writes=['hgB'])
                            p.i('dve', 'tensor_tensor', out=D3, in0=totb, in1=B3, op=ALU.subtract, reads=['hgC', 'hgB'], writes=['hgD'])
                            b_t, bl_t, bk, blk = tD, tB, 'hgD', 'hgB'
                        p.i('act', 'activation', out=bl_t[:], in_=bl_t[:], func=AF.Exp, reads=[blk], writes=[blk])
                        p.i(PL, 'tensor_tensor', out=KhT[:, t0:t0 + HN], in0=tA[:], in1=bl_t[:], op=ALU.mult, reads=['hgA', blk], writes=['hgKhT'])
                        o_t, ok = (tD, 'hgD') if d == 0 else (tC, 'hgC')
                        p.i('act', 'activation', out=o_t[:], in_=b_t[:], func=AF.Exp, scale=-1.0, reads=[bk], writes=[ok])
                        p.i('dve', 'tensor_tensor', out=kt[d][:, t0:t0 + HN], in0=tA[:], in1=o_t[:], op=ALU.mult, reads=['hgA', ok], writes=['hgkt%d' % d])
                        p.i('act', 'activation', out=b_t[:], in_=b_t[:], func=AF.Exp, reads=[bk], writes=[bk])
                        p.i('dve', 'tensor_tensor', out=qt[d][:, t0:t0 + HN], in0=sq_[:, t0:t0 + HN], in1=b_t[:], op=ALU.mult, reads=['hgsq', bk], writes=['hgqt%d' % d])
                    for tt in range(0 if 'notr' in HGDBG else NT):
                        ti = tt % 2
                        p.i('pe', 'matmul', tps[ti], lhsT=KhT[:, tt * 128:(tt + 1) * 128], rhs=ident_b[:], start=True, stop=True,
                            reads=['hgKhT', 'ident_b'], writes=[('hgtp', ti)])
                        if tt % 2 == 0:
                            p.i('act', 'activation', out=Khtm[:, tt, :], in_=tps[ti], func=AF.Copy, reads=[('hgtp', ti)], writes=['hgKhtm'])
                        else:
                            p.i('dve', 'tensor_copy', out=Khtm[:, tt, :], in_=tps[ti], reads=[('hgtp', ti)], writes=['hgKhtm'])
                    order = orders[d]
                    p.i('dve', 'memset', Srun[0][:], 0.0, writes=[('hgSr', 0)])
                    p.i(PL, 'memset', Sall[d][:, order[0], :], 0.0, writes=['hgS%d' % d])
                    import os
                    HGDBG = os.environ.get('HGDBG', '')
                    for idx in range(0 if 'nostate' in HGDBG else NCH - 1):
                        c = order[idx]; tile_ = c // 4; j = c % 4
                        bank = (idx // 8) % 2; slot = idx % 8
                        for hl in range(2):
                            p.i('pe', 'matmul', Ups[bank][hl * 64:(hl + 1) * 64, slot * 64:(slot + 1) * 64], lhsT=Khtm[32 * j:32 * j + 32, tile_, hl * 64:(hl + 1) * 64],
                                rhs=Vc[32 * j:32 * j + 32, tile_, hl * 64:(hl + 1) * 64], start=True, stop=True, tile_position=(32 * j, 64 * hl),
                                reads=['hgKhtm', 'hgV'], writes=[('hgU', bank)], sig=(hl == 1))
                        so = Srun[idx % 2]; sn = Srun[(idx + 1) % 2]
                        p.i('dve', 'scalar_tensor_tensor', out=sn[:], in0=so[:], scalar=Dc[:, c:c + 1], in1=Ups[bank][:, slot * 64:(slot + 1) * 64], op0=ALU.mult, op1=ALU.add,
                            reads=[('hgSr', idx % 2), 'hgDc', ('hgU', bank)], writes=[('hgSr', (idx + 1) % 2)])
                        p.i('act', 'activation', out=Sall[d][:, order[idx + 1], :], in_=sn[:], func=AF.Copy,
                            reads=[('hgSr', (idx + 1) % 2)], writes=['hgS%d' % d])
                acnt = 0
                for gi, (t0, n) in enumerate([] if 'nopass2' in HGDBG else BLKS):
                    Op = Ops[gi % 2]; ok_ = ('hgO', gi % 2)
                    p.dma('sp', gsb[:, 0:n], Pfm[b, (16 + ct) * 128:(17 + ct) * 128, t0:t0 + n], reads=['Pfm'], writes=['hgg'])
                    for tl in range(n // 128):
                        tt = (t0 // 128) + tl
                        cs = slice(tl * 128, (tl + 1) * 128); ts_ = slice(tt * 128, (tt + 1) * 128)
                        for hl in range(2):
                            hb = hl * 64
                            for d in range(2):
                                ai = acnt % 2; ati = acnt % 3; acnt += 1
                                p.i('pe', 'matmul', Aps[ai], lhsT=kt[d][hb:hb + 64, ts_], rhs=qt[d][hb:hb + 64, ts_], start=True, stop=True,
                                    reads=['hgkt%d' % d, 'hgqt%d' % d], writes=[('hgtp', ai)])
                                p.i('dve', 'tensor_tensor', out=attn[ati][:], in0=Aps[ai], in1=hm[:, d, :], op=ALU.mult,
                                    reads=[('hgtp', ai), 'hgm'], writes=[('hgat', ati)])
                                p.i('pe', 'matmul', Op[hb:hb + 64, cs], lhsT=Vc[:, tt, hb:hb + 64], rhs=attn[ati][:], start=(d == 0), stop=False,
                                    reads=['hgV', ('hgat', ati)], writes=[ok_], sig=False, skip_group_check=True)
                                for j in range(4):
                                    c = tt * 4 + j
                                    p.i('pe', 'matmul', Op[hb:hb + 64, tl * 128 + 32 * j:tl * 128 + 32 * j + 32], lhsT=Sall[d][hb:hb + 64, c, :],
                                        rhs=qt[d][hb:hb + 64, c * 32:(c + 1) * 32], start=False, stop=(d == 1),
                                        reads=['hgS%d' % d, 'hgqt%d' % d], writes=[ok_], sig=(j == 3), skip_group_check=True)
                    p.i('act', 'activation', out=n3[:, 0:n], in_=Op[:, 0:n], func=AF.Square, reads=[ok_], writes=['hgn3'])
                    p.i('pe', 'matmul', SSp[:, 0:n], lhsT=bo[:], rhs=n3[:, 0:n], start=True, stop=True, reads=['hgbo', 'hgn3'], writes=['hgSS'])
                    p.i('dve', 'tensor_scalar', out=n1[:, 0:n], in0=SSp[:, 0:n], scalar1=1.0 / 64, scalar2=1e-6, op0=ALU.mult, op1=ALU.add,
                        reads=['hgSS'], writes=['hgn1'])
                    p.i('act', 'activation', out=n1[:, 0:n], in_=n1[:, 0:n], func=AF.Sqrt, reads=['hgn1'], writes=['hgn1'])
                    p.i('dve', 'reciprocal', out=n1[:, 0:n], in_=n1[:, 0:n], reads=['hgn1'], writes=['hgn1'])
                    p.i('act', 'activation', out=n2[:, 0:n], in_=gsb[:, 0:n], func=AF.Silu, reads=['hgg'], writes=['hgn2'])
                    p.i('dve', 'scalar_tensor_tensor', out=n1[:, 0:n], in0=n1[:, 0:n], scalar=nw[:, l:l + 1], in1=n2[:, 0:n], op0=ALU.mult, op1=ALU.mult,
                        reads=['hgn1', 'hgn2', 'hgnw'], writes=['hgn1'])
                    p.i('dve', 'tensor_tensor', out=G['MIXT'][:, 4 + ct, t0:t0 + n], in0=Op[:, 0:n], in1=n1[:, 0:n], op=ALU.mult,
                        reads=[ok_, 'hgn1'], writes=[('MIXT', 4 + ct)])

    TWO_PI = 6.283185307179586
    PI = 3.141592653589793

    def sincos(src, n, sin_out, cos_out, tmpf, tmpi, key_src, keys, mpi, eng='dve'):
        kf, ki, ks, kc = keys
        for (off, out_ap, ko) in ((0.0, sin_out, ks), (PI / 2, cos_out, kc)):
            p.i('dve', 'tensor_scalar', out=tmpf, in0=src, scalar1=off, scalar2=1.0 / TWO_PI, op0=ALU.add, op1=ALU.mult, reads=[key_src], writes=[kf])
            p.i('dve', 'tensor_copy', out=tmpi, in_=tmpf, reads=[kf], writes=[ki])
            p.i('dve', 'tensor_copy', out=tmpf, in_=tmpi, reads=[ki], writes=[kf])
            p.i('dve', 'scalar_tensor_tensor', out=tmpf, in0=tmpf, scalar=-TWO_PI, in1=src, op0=ALU.mult, op1=ALU.add, reads=[kf, key_src], writes=[kf])
            p.i('dve', 'tensor_scalar', out=tmpf, in0=tmpf, scalar1=off + PI, scalar2=None, op0=ALU.add, reads=[kf], writes=[kf])
            p.i('act', 'activation', out=out_ap, in_=tmpf, func=AF.Sin, bias=mpi[:], reads=[kf, 's5mpi'], writes=[ko])

    def s5mix(l, b):
        with p.phase():
            mpi = p.sb([128, 1], F32, 's5mpi')
            p.i('dve', 'memset', mpi[:], -PI, writes=['s5mpi'])
            Yacc = p.sb([128, 2, T], F32, 's5Y')
            d2 = p.sb([128, 2], F32, 's5d'); gb = p.sb([128, 2], F32, 's5gb')
            p.dma('sp', d2[:], s5['s5_d2'][l], writes=['s5d']); p.dma('sp', gb[:], s5['s5_glu_b2'][l], writes=['s5gb'])
            J128 = p.sb([128, 128], BF16, 's5J'); Msw = p.sb([128, 128], F32, 's5Msw'); sv = p.sb([128, 128], F32, 's5sv')
            p.dma('sp', J128[:], s5['Jrev128'][:, :], writes=['s5J']); p.dma('sp', Msw[:], s5['MswT'][:, :], writes=['s5Msw'])
            p.dma('sp', sv[:], s5['svec'][:, :], writes=['s5sv'])
            with p.phase():
                ub = p.sb([128, T], BF16, 's5ub')
                for ct in range(2):
                    p.dma('sp', ub[:], Pfm[b, (18 + ct) * 128:(19 + ct) * 128, :], reads=['Pfm'], writes=['s5ub'])
                    p.i('dve', 'tensor_scalar', out=Yacc[:, ct, :], in0=ub[:], scalar1=d2[:, ct:ct + 1], scalar2=None, op0=ALU.mult,
                        reads=['s5ub', 's5d'], writes=[('s5Y', ct)])
            cosT = p.sb([128, 16, 128], F32, 's5cos'); sinT = p.sb([128, 16, 128], F32, 's5sin')
            Bw1 = p.sb([128, 16, 128], BF16, 's5Bw1'); Bw2 = p.sb([128, 16, 128], BF16, 's5Bw2')
            C1 = p.sb([128, 16, 16], BF16, 's5C1'); C2 = p.sb([128, 16, 16], BF16, 's5C2')
            rho = p.sb([128, 16], F32, 's5rho')
            for d in range(2):
                Rm = ident_b if d == 0 else J128
                rkey = 'ident_b' if d == 0 else 's5J'
                with p.phase():
                    lre = p.sb([128, 16], F32, 'q_lre'); lim = p.sb([128, 16], F32, 'q_lim'); ldt = p.sb([128, 16], F32, 'q_ldt')
                    p.dma('sp', lre[:], s5['s5_lre_p'][l, :, d, :], writes=['q_lre']); p.dma('sp', lim[:], s5['s5_lim_p'][l, :, d, :], writes=['q_lim'])
                    p.dma('sp', ldt[:], s5['s5_ldt_p'][l, :, d, :], writes=['q_ldt'])
                    p.i('act', 'activation', out=ldt[:], in_=ldt[:], func=AF.Exp, reads=['q_ldt'], writes=['q_ldt'])
                    p.i('dve', 'tensor_tensor', out=lre[:], in0=lre[:], in1=ldt[:], op=ALU.mult, reads=['q_lre', 'q_ldt'], writes=['q_lre'])
                    p.i('act', 'activation', out=rho[:], in_=lre[:], func=AF.Exp, reads=['q_lre'], writes=['s5rho'])
                    p.i('dve', 'tensor_tensor', out=lim[:], in0=lim[:], in1=ldt[:], op=ALU.mult, reads=['q_lim', 'q_ldt'], writes=['q_lim'])
                    ang = p.sb([128, 16, 128], F32, 'q_ang'); tf = p.sb([128, 16, 128], F32, 'q_tf'); ti_ = p.sb([128, 16, 128], I32, 'q_ti')
                    p.i('dve', 'tensor_tensor', out=ang[:], in0=lim[:].unsqueeze(2).to_broadcast([128, 16, 128]),
                        in1=sv[:].unsqueeze(1).to_broadcast([128, 16, 128]), op=ALU.mult, reads=['q_lim', 's5sv'], writes=['q_ang'])
                    sincos(ang[:], None, sinT[:], cosT[:], tf[:], ti_[:], 'q_ang', ('q_tf', 'q_ti', 's5sin', 's5cos'), mpi)
                with p.phase():
                    R = lambda nm: p.sb([128, 1024], F32, nm)
                    rl, ri, rd_ = R('r_lre'), R('r_lim'), R('r_ldt')
                    p.dma('sp', rl[:], s5['s5_lre_r'][l, d].partition_broadcast(128), writes=['r_lre'])
                    p.dma('sp', ri[:], s5['s5_lim_r'][l, d].partition_broadcast(128), writes=['r_lim'])
                    p.dma('sp', rd_[:], s5['s5_ldt_r'][l, d].partition_broadcast(128), writes=['r_ldt'])
                    p.i('act', 'activation', out=rd_[:], in_=rd_[:], func=AF.Exp, reads=['r_ldt'], writes=['r_ldt'])
                    mag, th = R('r_mag'), R('r_th')
                    p.i('dve', 'tensor_tensor', out=mag[:], in0=rl[:], in1=rd_[:], op=ALU.mult, reads=['r_lre', 'r_ldt'], writes=['r_mag'])
                    p.i('act', 'activation', out=mag[:], in_=mag[:], func=AF.Exp, reads=['r_mag'], writes=['r_mag'])
                    p.i('dve', 'tensor_tensor', out=th[:], in0=ri[:], in1=rd_[:], op=ALU.mult, reads=['r_lim', 'r_ldt'], writes=['r_th'])
                    sn, cs = R('r_sn'), R('r_cs'); tf2 = R('r_tf'); ti2 = p.sb([128, 1024], I32, 'r_ti')
                    sincos(th[:], None, sn[:], cs[:], tf2[:], ti2[:], 'r_th', ('r_tf', 'r_ti', 'r_sn', 'r_cs'), mpi)
                    p.i('dve', 'tensor_tensor', out=cs[:], in0=cs[:], in1=mag[:], op=ALU.mult, reads=['r_cs', 'r_mag'], writes=['r_cs'])
                    p.i('dve', 'tensor_scalar', out=cs[:], in0=cs[:], scalar1=-1.0, scalar2=None, op0=ALU.add, reads=['r_cs'], writes=['r_cs'])
                    p.i('dve', 'tensor_tensor', out=sn[:], in0=sn[:], in1=mag[:], op=ALU.mult, reads=['r_sn', 'r_mag'], writes=['r_sn'])
                    p.i('dve', 'tensor_tensor', out=mag[:], in0=rl[:], in1=rl[:], op=ALU.mult, reads=['r_lre'], writes=['r_mag'])
                    p.i('dve', 'tensor_tensor', out=th[:], in0=ri[:], in1=ri[:], op=ALU.mult, reads=['r_lim'], writes=['r_th'])
                    p.i('dve', 'tensor_tensor', out=mag[:], in0=mag[:], in1=th[:], op=ALU.add, reads=['r_mag', 'r_th'], writes=['r_mag'])
                    p.i('dve', 'reciprocal', out=mag[:], in_=mag[:], reads=['r_mag'], writes=['r_mag'])
                    p.i('dve', 'tensor_tensor', out=th[:], in0=cs[:], in1=rl[:], op=ALU.mult, reads=['r_cs', 'r_lre'], writes=['r_th'])
                    p.i('dve', 'tensor_tensor', out=tf2[:], in0=sn[:], in1=ri[:], op=ALU.mult, reads=['r_sn', 'r_lim'], writes=['r_tf'])
                    p.i('dve', 'tensor_tensor', out=th[:], in0=th[:], in1=tf2[:], op=ALU.add, reads=['r_th', 'r_tf'], writes=['r_th'])
                    p.i('dve', 'tensor_tensor', out=th[:], in0=th[:], in1=mag[:], op=ALU.mult, reads=['r_th', 'r_mag'], writes=['r_th'])
                    p.i('dve', 'tensor_tensor', out=tf2[:], in0=sn[:], in1=rl[:], op=ALU.mult, reads=['r_sn', 'r_lre'], writes=['r_tf'])
                    p.i('dve', 'tensor_tensor', out=rd_[:], in0=cs[:], in1=ri[:], op=ALU.mult, reads=['r_cs', 'r_lim'], writes=['r_ldt'])
                    p.i('dve', 'tensor_tensor', out=tf2[:], in0=tf2[:], in1=rd_[:], op=ALU.subtract, reads=['r_tf', 'r_ldt'], writes=['r_tf'])
                    p.i('dve', 'tensor_tensor', out=tf2[:], in0=tf2[:], in1=mag[:], op=ALU.mult, reads=['r_tf', 'r_mag'], writes=['r_tf'])
                    p.dma('sp', rl[:], s5['s5_Br_emb'][l, d].rearrange("p g q -> p (g q)"), writes=['r_lre'])
                    p.dma('sp', ri[:], s5['s5_Bi_emb'][l, d].rearrange("p g q -> p (g q)"), writes=['r_lim'])
                    p.i('dve', 'tensor_tensor', out=cs[:], in0=th[:], in1=rl[:], op=ALU.mult, reads=['r_th', 'r_lre'], writes=['r_cs'])
                    p.i('dve', 'tensor_tensor', out=mag[:], in0=tf2[:], in1=ri[:], op=ALU.mult, reads=['r_tf', 'r_lim'], writes=['r_mag'])
                    p.i('dve', 'tensor_tensor', out=cs[:], in0=cs[:], in1=mag[:], op=ALU.subtract, reads=['r_cs', 'r_mag'], writes=['r_cs'])
                    p.i('dve', 'tensor_tensor', out=sn[:], in0=th[:], in1=ri[:], op=ALU.mult, reads=['r_th', 'r_lim'], writes=['r_sn'])
                    p.i('dve', 'tensor_tensor', out=mag[:], in0=tf2[:], in1=rl[:], op=ALU.mult, reads=['r_tf', 'r_lre'], writes=['r_mag'])
                    p.i('dve', 'tensor_tensor', out=sn[:], in0=sn[:], in1=mag[:], op=ALU.add, reads=['r_sn', 'r_mag'], writes=['r_sn'])
                    cs3 = cs[:].rearrange("p (g q) -> p g q", q=64); sn3 = sn[:].rearrange("p (g q) -> p g q", q=64)
                    p.i('dve', 'tensor_copy', out=Bw1[:, :, 0:64], in_=cs3, reads=['r_cs'], writes=['s5Bw1'])
                    p.i('dve', 'tensor_copy', out=Bw1[:, :, 64:128], in_=sn3, reads=['r_sn'], writes=['s5Bw1'])
                    p.i('dve', 'tensor_copy', out=Bw2[:, :, 0:64], in_=sn3, reads=['r_sn'], writes=['s5Bw2'])
                    p.i('dve', 'tensor_scalar', out=Bw2[:, :, 64:128], in0=cs3, scalar1=-1.0, scalar2=None, op0=ALU.mult, reads=['r_cs'], writes=['s5Bw2'])
                with p.phase():
                    cr = p.sb([128, 16, 16], F32, 'r_cr'); ci = p.sb([128, 16, 16], F32, 'r_ci')
                    p.dma('sp', cr[:], s5['s5_Cr2'][l, d], writes=['r_cr']); p.dma('sp', ci[:], s5['s5_Ci2'][l, d], writes=['r_ci'])
                    p.i('dve', 'tensor_copy', out=C1[0:64], in_=cr[0:64], reads=['r_cr'], writes=['s5C1'])
                    p.i('dve', 'tensor_scalar', out=C1[64:128], in0=ci[64:128], scalar1=-1.0, scalar2=None, op0=ALU.mult, reads=['r_ci'], writes=['s5C1'])
                    p.i('dve', 'tensor_scalar', out=C2[0:64], in0=ci[0:64], scalar1=-1.0, scalar2=None, op0=ALU.mult, reads=['r_ci'], writes=['s5C2'])
                    p.i('dve', 'tensor_scalar', out=C2[64:128], in0=cr[64:128], scalar1=-1.0, scalar2=None, op0=ALU.mult, reads=['r_cr'], writes=['s5C2'])
                with p.phase():
                    uTs = p.sb([128, 2, 128], BF16, 'm_uT')
                    Utm = p.sb([128, NT, 256], BF16, 's5U')
                    p.dma('pool', Utm[:], Ptm[b].rearrange("(n p) c -> p n c", p=128)[:, :, 640:896], reads=['Ptm'], writes=['s5U'])
                    t1 = p.sb([128, 1024], F32, 'm_t1'); t2 = p.sb([128, 1024], F32, 'm_t2')
                    inp_ = p.sb([128, 16, 128], F32, 'm_inp'); xt = p.sb([128, 16, 128], F32, 'm_xt')
                    P1 = p.sb([128, 16, 128], BF16, 'm_P1'); P2 = p.sb([128, 16, 128], BF16, 'm_P2')
                    carry = p.sb([128, 16], F32, 'm_carry'); pl1 = p.sb([128, 16], F32, 'm_pl1'); pl2 = p.sb([128, 16], F32, 'm_pl2')
                    Ytm = p.sb([128, 256], BF16, 'm_Ytm')
                    p.i('dve', 'memset', carry[:], 0.0, writes=['m_carry'])
                    ups = [p.ps([128, 128], F32, 'm_ups%d' % i) for i in range(2)]
                    bs = [p.ps([128, 1024], F32, 'm_bs%d' % i) for i in range(2)]
                    cps = p.ps([128, 16], F32, 'm_cps'); yps = p.ps([128, 256], F32, 'm_yps')
                    order = list(range(NT)) if d == 0 else [1, 0] + list(range(NT - 1, 1, -1))
                    for tt in order:
                        for ct in range(2):
                            p.i('pe', 'matmul', ups[ct][:], lhsT=Utm[:, tt, ct * 128:(ct + 1) * 128], rhs=Rm[:], start=True, stop=True,
                                reads=['s5U', rkey], writes=[('m_ups', ct)])
                            if ct == 0:
                                p.i('act', 'activation', out=uTs[:, ct, :], in_=ups[ct][:], func=AF.Copy, reads=[('m_ups', ct)], writes=[('m_uT', ct)])
                            else:
                                p.i('dve', 'tensor_copy', out=uTs[:, ct, :], in_=ups[ct][:], reads=[('m_ups', ct)], writes=[('m_uT', ct)])
                        for gh in range(2):
                            for gl in range(8):
                                g = gh * 8 + gl
                                p.i('pe', 'matmul', bs[0][:, gl * 128:(gl + 1) * 128], lhsT=Bw1[:, g, :], rhs=uTs[:, gh, :], start=True, stop=True,
                                    reads=['s5Bw1', ('m_uT', gh)], writes=[('m_bs', 0)], sig=(gl == 7))
                            for gl in range(8):
                                g = gh * 8 + gl
                                p.i('pe', 'matmul', bs[1][:, gl * 128:(gl + 1) * 128], lhsT=Bw2[:, g, :], rhs=uTs[:, gh, :], start=True, stop=True,
                                    reads=['s5Bw2', ('m_uT', gh)], writes=[('m_bs', 1)], sig=(gl == 7))
                            gs = slice(gh * 8, gh * 8 + 8)
                            c3 = cosT[:, gs, :].rearrange("p g s -> p (g s)"); s3 = sinT[:, gs, :].rearrange("p g s -> p (g s)")
                            p.i('dve', 'tensor_tensor', out=t1[:], in0=bs[0][:], in1=c3, op=ALU.mult, reads=[('m_bs', 0), 's5cos'], writes=['m_t1'])
                            p.i('dve', 'tensor_tensor', out=t2[:], in0=bs[1][:], in1=s3, op=ALU.mult, reads=[('m_bs', 1), 's5sin'], writes=['m_t2'])
                            p.i('pool', 'tensor_tensor', out=inp_[:, gs, :].rearrange("p g s -> p (g s)"), in0=t1[:], in1=t2[:], op=ALU.add,
                                reads=['m_t1', 'm_t2'], writes=[('m_inp', gh)])
                        for g in range(16):
                            p.i('dve', 'tensor_tensor_scan', out=xt[:, g, :], data0=rho[:, g:g + 1].to_broadcast([128, 128]), data1=inp_[:, g, :],
                                initial=carry[:, g:g + 1], op0=ALU.mult, op1=ALU.add,
                                reads=['s5rho', ('m_inp', g // 8), 'm_carry'], writes=[('m_xt', g)])
                        p.i('pool', 'tensor_tensor', out=P1[:], in0=xt[:], in1=cosT[:], op=ALU.mult, reads=['m_xt', 's5cos'], writes=['m_P1'])
                        p.i('dve', 'tensor_tensor', out=P2[:], in0=xt[:], in1=sinT[:], op=ALU.mult, reads=['m_xt', 's5sin'], writes=['m_P2'])
                        p.i('dve', 'tensor_tensor', out=pl1[:], in0=xt[:, :, 127], in1=cosT[:, :, 127], op=ALU.mult, reads=['m_xt', 's5cos'], writes=['m_pl1'])
                        p.i('dve', 'tensor_tensor', out=pl2[:], in0=xt[:, :, 127], in1=sinT[:, :, 127], op=ALU.mult, reads=['m_xt', 's5sin'], writes=['m_pl2'])
                        p.i('pe', 'matmul', cps[:], lhsT=ident_f[:], rhs=pl1[:], start=True, stop=False, reads=['ident_f', 'm_pl1'], writes=['m_cps'], sig=False)
                        p.i('pe', 'matmul', cps[:], lhsT=Msw[:], rhs=pl2[:], start=False, stop=True, reads=['s5Msw', 'm_pl2'], writes=['m_cps'])
                        p.i('dve', 'tensor_copy', out=carry[:], in_=cps[:], reads=['m_cps'], writes=['m_carry'])
                        for g in range(16):
                            p.i('pe', 'matmul', yps[:, g * 16:(g + 1) * 16], lhsT=P1[:, g, :], rhs=C1[:, g, :], start=True, stop=False,
                                reads=['m_P1', 's5C1'], writes=['m_yps'], sig=False)
                            p.i('pe', 'matmul', yps[:, g * 16:(g + 1) * 16], lhsT=P2[:, g, :], rhs=C2[:, g, :], start=False, stop=True,
                                reads=['m_P2', 's5C2'], writes=['m_yps'], sig=(g == 15))
                        p.i('act', 'activation', out=Ytm[:], in_=yps[:], func=AF.Copy, reads=['m_yps'], writes=['m_Ytm'])
                        for ct in range(2):
                            p.i('pe', 'matmul', ups[ct][:], lhsT=Ytm[:, ct * 128:(ct + 1) * 128], rhs=Rm[:], start=True, stop=True,
                                reads=['m_Ytm', rkey], writes=[('m_ups', ct)])
                            p.i('dve', 'tensor_tensor', out=Yacc[:, ct, tt * 128:(tt + 1) * 128], in0=ups[ct][:], in1=Yacc[:, ct, tt * 128:(tt + 1) * 128],
                                op=ALU.add, reads=[('m_ups', ct), ('s5Y', ct)], writes=[('s5Y', ct)])
            with p.phase():
                gw_f = p.sb([128, 2, 256], F32, 'g_wf'); gw = p.sb([128, 2, 256], BF16, 'g_w')
                p.dma('sp', gw_f[:], s5['s5_glu_w'][l].rearrange("(c p) n -> p c n", p=128), writes=['g_wf'])
                p.i('dve', 'tensor_copy', out=gw[:], in_=gw_f[:], reads=['g_wf'], writes=['g_w'])
                zT = p.sb([128, 2, T], BF16, 'g_z'); w1 = p.sb([128, T], F32, 'g_w1'); w2 = p.sb([128, T], F32, 'g_w2')
                for ct in range(2):
                    y = Yacc[:, ct, :]
                    p.i('dve', 'tensor_tensor', out=w1[:], in0=y, in1=y, op=ALU.mult, reads=[('s5Y', ct)], writes=['g_w1'])
                    p.i('dve', 'tensor_scalar', out=w1[:], in0=w1[:], scalar1=0.044715, scalar2=1.0, op0=ALU.mult, op1=ALU.add, reads=['g_w1'], writes=['g_w1'])
                    p.i('dve', 'tensor_tensor', out=w1[:], in0=w1[:], in1=y, op=ALU.mult, reads=['g_w1', ('s5Y', ct)], writes=['g_w1'])
                    p.i('act', 'activation', out=w2[:], in_=w1[:], func=AF.Sigmoid, scale=1.5957691216057308, reads=['g_w1'], writes=['g_w2'])
                    p.i('dve', 'tensor_tensor', out=zT[:, ct, :], in0=w2[:], in1=y, op=ALU.mult, reads=['g_w2', ('s5Y', ct)], writes=[('g_z', ct)])
                gps = [p.ps([128, 512], F32, 'g_ps%d' % i) for i in range(2)]
                sg_ = [p.sb([128, 512], F32, 'g_sg%d' % i) for i in range(2)]
                k = 0
                for oc in range(2):
                    for (t0, n) in BLKS:
                        i = k % 2; k += 1
                        for kc in range(2):
                            p.i('pe', 'matmul', gps[i][:, 0:n], lhsT=gw[:, kc, oc * 128:(oc + 1) * 128], rhs=zT[:, kc, t0:t0 + n], start=(kc == 0), stop=(kc == 1),
                                reads=['g_w', 'g_z'], writes=[('g_ps', i)], sig=(kc == 1))
                        p.i('act', 'activation', out=sg_[i][:, 0:n], in_=gps[i][:, 0:n], func=AF.Sigmoid, bias=gb[:, oc:oc + 1],
                            reads=[('g_ps', i), 's5gb'], writes=[('g_sg', i)])
                        p.i('dve', 'tensor_tensor', out=G['MIXT'][:, 6 + oc, t0:t0 + n], in0=sg_[i][:, 0:n], in1=zT[:, oc, t0:t0 + n], op=ALU.mult,
                            reads=[('g_sg', i), 'g_z'], writes=[('MIXT', 6 + oc)])

    def swa(l, b, need_ctx):
        with p.phase():
            qg = [p.sb([128, T], BF16, 'swq%d' % i) for i in range(2)]
            kT = p.sb([128, T], BF16, 'swk'); V = p.sb([128, NT, 128], BF16, 'swv')
            mk = p.sb([128, 384], BF16, 'swmask'); esk = p.sb([128, 4], F32, 'esk')
            p.dma('sp', qg[0][:], Pfm[b, 0:128, :], reads=['Pfm'], writes=['swq0'])
            p.dma('pool', qg[1][:], Pfm[b, 128:256, :], reads=['Pfm'], writes=['swq1'])
            p.dma('sp', kT[:], Pfm[b, 512:640, :], reads=['Pfm'], writes=['swk'])
            p.dma('pool', V[:], Ptm[b].rearrange("(n p) c -> p n c", p=128)[:, :, 0:128], reads=['Ptm'], writes=['swv'])
            p.dma('sp', mk[:], swa_mask_d[:, :], writes=['swmask'])
            p.dma('sp', esk[:], swa_sink_d[l].partition_broadcast(128), writes=['esk'])
            p.i('act', 'activation', out=esk[:], in_=esk[:], func=AF.Exp, reads=['esk'], writes=['esk'])
            O = p.ps([64, 1024], F32, 'swO'); Dn = p.ps([64, 1024], F32, 'swD')
            st = [p.ps([128, 512], F32, 'swst%d' % i) for i in range(2)]
            pt = [p.sb([128, 512], BF16, 'swpt%d' % i) for i in range(3)]
            rd = p.sb([64, 1024], F32, 'swrd')
            cnt = [0]
            for h in range(4):
                qT = qg[h % 2]; base = (h // 2) * 64; kh = h // 2
                qsets = [(LC + 1024 * qh, 1024, True, qh) for qh in range(2)]
                if need_ctx:
                    qsets.append((0, LC, False, None))
                for (tq0, nqs, lat, qh) in qsets:
                    started = [False] * ((nqs + 511) // 512)

                    def pv(ptb, pti, ktile, a, bnd):
                        c = a
                        while c < bnd:
                            e_ = min(bnd, (c // 512 + 1) * 512)
                            bk = c // 512
                            stt = not started[bk]
                            if stt:
                                assert c % 512 == 0 and e_ - c == min(512, nqs - c)
                                started[bk] = True
                            p.i('pe', 'matmul', O[:, c:e_], lhsT=V[:, ktile, kh * 64:(kh + 1) * 64], rhs=ptb[:, c - a:e_ - a],
                                start=stt, stop=False, reads=['swv', ('swpt', pti)], writes=['swO'], sig=False, skip_group_check=True)
                            p.i('pe', 'matmul', Dn[:, c:e_], lhsT=ones_b[:, 0:64], rhs=ptb[:, c - a:e_ - a],
                                start=stt, stop=False, reads=['ones_b', ('swpt', pti)], writes=['swD'], sig=True, skip_group_check=True)
                            c = e_

                    for ct in range(2):
                        for c0 in range(0, nqs, 512):
                            n = min(512, nqs - c0)
                            si = cnt[0] % 2; pi = cnt[0] % 3; cnt[0] += 1
                            p.i('pe', 'matmul', st[si][:, 0:n], lhsT=kT[base:base + 64, ct * 128:(ct + 1) * 128],
                                rhs=qT[base:base + 64, tq0 + c0:tq0 + c0 + n], start=True, stop=True,
                                reads=['swk', 'swq%d' % (h % 2)], writes=[('swst', si)])
                            p.i('act', 'activation', out=pt[pi][:, 0:n], in_=st[si][:, 0:n], func=AF.Exp, scale=0.125,
                                reads=[('swst', si)], writes=[('swpt', pi)])
                            pv(pt[pi], pi, ct, c0, c0 + n)
                    if lat:
                        for kt in range(16):
                            lo = max(kt - 1, 8 * qh); hi = min(kt + 1, 8 * qh + 7)
                            if lo > hi:
                                continue
                            n = (hi - lo + 1) * 128
                            m0 = (lo - (kt - 1)) * 128
                            qa = LC + lo * 128
                            si = cnt[0] % 2; pi = cnt[0] % 3; cnt[0] += 1
                            p.i('pe', 'matmul', st[si][:, 0:n], lhsT=kT[base:base + 64, LC + kt * 128:LC + (kt + 1) * 128],
                                rhs=qT[base:base + 64, qa:qa + n], start=True, stop=False,
                                reads=['swk', 'swq%d' % (h % 2)], writes=[('swst', si)], sig=False)
                            p.i('pe', 'matmul', st[si][:, 0:n], lhsT=ident_b[:], rhs=mk[:, m0:m0 + n], start=False, stop=True,
                                reads=['ident_b', 'swmask'], writes=[('swst', si)])
                            p.i('act', 'activation', out=pt[pi][:, 0:n], in_=st[si][:, 0:n], func=AF.Exp, scale=0.125,
                                reads=[('swst', si)], writes=[('swpt', pi)])
                            a = (lo - 8 * qh) * 128
                            pv(pt[pi], pi, 2 + kt, a, a + n)
                    p.i('dve', 'tensor_scalar', out=rd[:, 0:nqs], in0=Dn[:, 0:nqs], scalar1=esk[0:64, h:h + 1], scalar2=None, op0=ALU.add,
                        reads=['swD', 'esk'], writes=['swrd'])
                    p.i('dve', 'reciprocal', out=rd[:, 0:nqs], in_=rd[:, 0:nqs], reads=['swrd'], writes=['swrd'])
                    pb = (h % 2) * 64
                    p.i('dve', 'tensor_tensor', out=G['MIXT'][pb:pb + 64, h // 2, tq0:tq0 + nqs], in0=O[:, 0:nqs], in1=rd[:, 0:nqs], op=ALU.mult,
                        reads=['swO', 'swrd'], writes=[('MIXT', h // 2)])

    def norm_mod(l, b, gsb, gname, shift_idx):
        with p.phase():
            sq = p.sb([128, 8, 512], F32, 'sq')
            rs = p.sb([128, T], F32, 'rs')
            tmp = p.sb([128, 512], F32, 'ntmp')
            pss = [p.ps([128, 512], F32, 'nps%d' % i) for i in range(2)]
            for bi, (t0, n) in enumerate(BLKS):
                ps_ = pss[bi % 2]
                p.i('act', 'activation', out=sq[:, :, 0:n], in_=XT[:, :, t0:t0 + n], func=AF.Square, reads=['XT'], writes=['sq'])
                for c in range(8):
                    p.i('pe', 'matmul', ps_[:, 0:n], lhsT=ones_f[:], rhs=sq[:, c, 0:n], start=(c == 0), stop=(c == 7),
                        reads=['sq', 'ones_f'], writes=[('nps', bi % 2)], sig=(c == 7))
                p.i('dve', 'tensor_scalar', out=tmp[:, 0:n], in0=ps_[:, 0:n], scalar1=1.0 / D, scalar2=1e-6, op0=ALU.mult, op1=ALU.add,
                    reads=[('nps', bi % 2)], writes=['ntmp'])
                p.i('act', 'activation', out=tmp[:, 0:n], in_=tmp[:, 0:n], func=AF.Sqrt, reads=['ntmp'], writes=['ntmp'])
                p.i('dve', 'reciprocal', out=rs[:, t0:t0 + n], in_=tmp[:, 0:n], reads=['ntmp'], writes=[('rs', bi)])
            xn = [p.sb([128, T], F32, 'xn%d' % i) for i in range(2)]
            for c in range(8):
                xb = xn[c % 2]
                p.i('dve' if c % 2 == 0 else 'pool', 'tensor_tensor', out=xb[:], in0=XT[:, c, :], in1=rs[:], op=ALU.mult,
                    reads=['XT', 'rs'], writes=[('xn', c % 2)])
                for (t0, n, j) in ((0, LC, nb), (LC, NL, b)):
                    p.i('act', 'activation', out=G['HT'][:, c, t0:t0 + n], in_=xb[:, t0:t0 + n], func=AF.Identity,
                        scale=gsb[l][:, c, j:j + 1], bias=modT[l][:, shift_idx * 8 + c, j:j + 1],
                        reads=[('xn', c % 2), 'modT%d' % l, gname + str(l)], writes=[('HT', c)])

    def rstd_all(rs, blks):
        sq = p.sb([128, 8, 512], F32, 'sq')
        tmp = p.sb([128, 512], F32, 'ntmp')
        pss = [p.ps([128, 512], F32, 'nps%d' % i) for i in range(2)]
        for bi, (t0, n) in enumerate(blks):
            ps_ = pss[bi % 2]
            p.i('act', 'activation', out=sq[:, :, 0:n], in_=XT[:, :, t0:t0 + n], func=AF.Square, reads=['XT'], writes=['sq'])
            for c in range(8):
                p.i('pe', 'matmul', ps_[:, 0:n], lhsT=ones_f[:], rhs=sq[:, c, 0:n], start=(c == 0), stop=(c == 7),
                    reads=['sq', 'ones_f'], writes=[('nps', bi % 2)], sig=(c == 7))
            p.i('dve', 'tensor_scalar', out=tmp[:, 0:n], in0=ps_[:, 0:n], scalar1=1.0 / D, scalar2=1e-6, op0=ALU.mult, op1=ALU.add,
                reads=[('nps', bi % 2)], writes=['ntmp'])
            p.i('act', 'activation', out=tmp[:, 0:n], in_=tmp[:, 0:n], func=AF.Sqrt, reads=['ntmp'], writes=['ntmp'])
            p.i('dve', 'reciprocal', out=rs[:, t0:t0 + n], in_=tmp[:, 0:n], reads=['ntmp'], writes=[('rs', bi)])

    def wout(l, b, blks):
        with p.phase():
            Wo = p.sb([128, 8, D], BF16, 'Wo')
            p.dma('sp', Wo[:], wout_bf[l].rearrange("(c p) n -> p c n", p=128), reads=['wout_bf'], writes=['Wo'])
            ops_ = [p.ps([128, 512], F32, 'wops%d' % i) for i in range(2)]
            k = 0
            for (t0, n) in blks:
                j = b if t0 >= LC else nb
                for dc in range(8):
                    i = k % 2; k += 1
                    for kc in range(8):
                        p.i('pe', 'matmul', ops_[i][:, 0:n], lhsT=Wo[:, kc, dc * 128:(dc + 1) * 128], rhs=G['MIXT'][:, kc, t0:t0 + n],
                            start=(kc == 0), stop=(kc == 7), reads=['Wo', 'MIXT'], writes=[('wops', i)], sig=(kc == 7))
                    p.i('dve', 'scalar_tensor_tensor', out=XT[:, dc, t0:t0 + n], in0=ops_[i][:, 0:n], scalar=modT[l][:, 16 + dc, j:j + 1],
                        in1=XT[:, dc, t0:t0 + n], op0=ALU.mult, op1=ALU.add,
                        reads=[('wops', i), 'modT%d' % l, ('XT', dc)], writes=[('XT', dc)])

    def norm2_router(l, b, LG):
        with p.phase():
            rs = p.sb([128, T], F32, 'rs')
            rstd_all(rs, BLKS)
            Wr = p.sb([128, 8, 36], F32, 'Wr'); rb = p.sb([128, 36], F32, 'rb')
            p.dma('sp', Wr[:], moe_rw[l].rearrange("(c p) n -> p c n", p=128), writes=['Wr'])
            p.dma('sp', rb[:], moe_rb[l].partition_broadcast(128), writes=['rb'])
            xn = [p.sb([128, 8, 512], F32, 'xn%d' % i) for i in range(2)]
            lps = [p.ps([128, 512], F32, 'lgps%d' % i) for i in range(2)]
            for bi, (t0, n) in enumerate(BLKS):
                xb = xn[bi % 2]; j = b if t0 >= LC else nb
                p.i('dve', 'tensor_tensor', out=xb[:, :, 0:n], in0=XT[:, :, t0:t0 + n], in1=rs[:, t0:t0 + n].unsqueeze(1).to_broadcast([128, 8, n]),
                    op=ALU.mult, reads=['XT', 'rs'], writes=[('xn', bi % 2)])
                for c in range(8):
                    p.i('act', 'activation', out=xb[:, c, 0:n], in_=xb[:, c, 0:n], func=AF.Identity,
                        scale=g2[l][:, c, j:j + 1], bias=modT[l][:, 24 + c, j:j + 1],
                        reads=[('xn', bi % 2), 'modT%d' % l, 'g2_%d' % l], writes=[('xn', bi % 2)])
                p.i('pool', 'tensor_copy', out=G['HT'][:, :, t0:t0 + n], in_=xb[:, :, 0:n], reads=[('xn', bi % 2)], writes=['HT'])
                lp = lps[bi % 2]
                nt_ = n // 128
                for tl in range(nt_):
                    for c in range(8):
                        p.i('pe', 'matmul', lp[:, tl * 36:(tl + 1) * 36], lhsT=xb[:, c, tl * 128:(tl + 1) * 128], rhs=Wr[:, c, :],
                            start=(c == 0), stop=(c == 7), reads=[('xn', bi % 2), 'Wr'], writes=[('lgps', bi % 2)], sig=(c == 7))
                a0 = t0 // 128
                p.i('dve', 'tensor_tensor', out=LG[:, a0:a0 + nt_, :], in0=lp[:, 0:nt_ * 36].rearrange("p (a e) -> p a e", e=36),
                    in1=rb[:].unsqueeze(1).to_broadcast([128, nt_, 36]), op=ALU.add, reads=[('lgps', bi % 2), 'rb'], writes=['LG'])

    def router_math(LG, combT):
        BIGR = 1.0e4
        with p.phase():
            A_ = lambda shp, nm: p.sb(shp, F32, nm)
            gmax = A_([128, NT], 'rm_gmax'); oh = A_([128, NT, 4], 'rm_oh'); eg = A_([128, NT, 4], 'rm_eg'); gs_ = A_([128, NT], 'rm_gs')
            msk_ = A_([128, NT, 4, 8], 'rm_msk'); m8 = A_([128, NT, 8], 'rm_m8'); k1 = A_([128, NT, 32], 'rm_k1'); k2 = A_([128, NT, 32], 'rm_k2')
            m1 = A_([128, NT], 'rm_m1'); m2 = A_([128, NT], 'rm_m2'); w1 = A_([128, NT], 'rm_w1'); w2 = A_([128, NT], 'rm_w2')
            comb = A_([128, NT, 32], 'rm_comb'); ms2 = A_([128, NT, 32], 'rm_ms2')
            gl = LG[:, :, 0:4]
            p.i('dve', 'tensor_reduce', out=gmax[:], in_=gl, axis=AX.X, op=ALU.max, reads=['LG'], writes=['rm_gmax'])
            gmb = gmax[:].unsqueeze(2).to_broadcast([128, NT, 4])
            p.i('dve', 'tensor_tensor', out=oh[:], in0=gl, in1=gmb, op=ALU.is_equal, reads=['LG', 'rm_gmax'], writes=['rm_oh'])
            p.i('dve', 'tensor_tensor', out=eg[:], in0=gl, in1=gmb, op=ALU.subtract, reads=['LG', 'rm_gmax'], writes=['rm_eg'])
            p.i('act', 'activation', out=eg[:], in_=eg[:], func=AF.Exp, reads=['rm_eg'], writes=['rm_eg'])
            p.i('dve', 'tensor_reduce', out=gs_[:], in_=eg[:], axis=AX.X, op=ALU.add, reads=['rm_eg'], writes=['rm_gs'])
            p.i('dve', 'reciprocal', out=gs_[:], in_=gs_[:], reads=['rm_gs'], writes=['rm_gs'])
            p.i('dve', 'tensor_scalar', out=oh[:], in0=oh[:], scalar1=-1.0, scalar2=BIGR, op0=ALU.add, op1=ALU.mult, reads=['rm_oh'], writes=['rm_oh'])
            el = LG[:, :, 4:36].rearrange("p a (g e) -> p a g e", e=8)
            p.i('dve', 'tensor_tensor', out=msk_[:], in0=el, in1=oh[:].unsqueeze(3).to_broadcast([128, NT, 4, 8]), op=ALU.add,
                reads=['LG', 'rm_oh'], writes=['rm_msk'])
            mf = msk_[:].rearrange("p a g e -> p a (g e)")
            p.i('dve', 'tensor_reduce', out=m1[:], in_=mf, axis=AX.X, op=ALU.max, reads=['rm_msk'], writes=['rm_m1'])
            p.i('dve', 'tensor_tensor', out=k1[:], in0=mf, in1=m1[:].unsqueeze(2).to_broadcast([128, NT, 32]), op=ALU.is_equal,
                reads=['rm_msk', 'rm_m1'], writes=['rm_k1'])
            p.i('dve', 'scalar_tensor_tensor', out=ms2[:], in0=k1[:], scalar=-BIGR, in1=mf, op0=ALU.mult, op1=ALU.add,
                reads=['rm_k1', 'rm_msk'], writes=['rm_ms2'])
            p.i('dve', 'tensor_reduce', out=m2[:], in_=ms2[:], axis=AX.X, op=ALU.max, reads=['rm_ms2'], writes=['rm_m2'])
            p.i('dve', 'tensor_tensor', out=k2[:], in0=ms2[:], in1=m2[:].unsqueeze(2).to_broadcast([128, NT, 32]), op=ALU.is_equal,
                reads=['rm_ms2', 'rm_m2'], writes=['rm_k2'])
            p.i('dve', 'tensor_tensor', out=w1[:], in0=m2[:], in1=m1[:], op=ALU.subtract, reads=['rm_m1', 'rm_m2'], writes=['rm_w1'])
            p.i('act', 'activation', out=w1[:], in_=w1[:], func=AF.Exp, reads=['rm_w1'], writes=['rm_w1'])
            p.i('dve', 'tensor_scalar', out=w1[:], in0=w1[:], scalar1=1.0, scalar2=None, op0=ALU.add, reads=['rm_w1'], writes=['rm_w1'])
            p.i('dve', 'reciprocal', out=w1[:], in_=w1[:], reads=['rm_w1'], writes=['rm_w1'])
            p.i('dve', 'tensor_tensor', out=w1[:], in0=w1[:], in1=gs_[:], op=ALU.mult, reads=['rm_w1', 'rm_gs'], writes=['rm_w1'])
            p.i('dve', 'tensor_tensor', out=w2[:], in0=gs_[:], in1=w1[:], op=ALU.subtract, reads=['rm_w1', 'rm_gs'], writes=['rm_w2'])
            p.i('dve', 'tensor_tensor', out=k1[:], in0=k1[:], in1=w1[:].unsqueeze(2).to_broadcast([128, NT, 32]), op=ALU.mult,
                reads=['rm_k1', 'rm_w1'], writes=['rm_k1'])
            p.i('dve', 'tensor_tensor', out=k2[:], in0=k2[:], in1=w2[:].unsqueeze(2).to_broadcast([128, NT, 32]), op=ALU.mult,
                reads=['rm_k2', 'rm_w2'], writes=['rm_k2'])
            p.i('dve', 'tensor_tensor', out=comb[:], in0=k1[:], in1=k2[:], op=ALU.add, reads=['rm_k1', 'rm_k2'], writes=['rm_comb'])
            if 'comb' in dbg and 'comb' not in dbg_o:
                d_ = dout('comb', [128, NT, 32])
                p.dma('sp', d_[:, :, :], comb[:], reads=['rm_comb'], writes=['dbg_comb'])
            ctp = [p.ps([128, 128], F32, 'rm_ctp%d' % i) for i in range(2)]
            for tt in range(NT):
                i = tt % 2
                p.i('pe', 'matmul', ctp[i][0:32, :], lhsT=comb[:, tt, :], rhs=ident_f[:], start=True, stop=True,
                    reads=['rm_comb', 'ident_f'], writes=[('rm_ctp', i)])
                p.i('act', 'activation', out=combT[0:32, tt * 128:(tt + 1) * 128], in_=ctp[i][0:32, :], func=AF.Copy,
                    reads=[('rm_ctp', i)], writes=['combT'])

    def moe_ffn(l, b, blks, combT):
        with p.phase():
            Esel = p.sb([128, 32, 128], BF16, 'Esel')
            p.dma('sp', Esel[0:32], esel_d[:, :, :], writes=['Esel'])
            Wg = [p.sb([128, 8, 512], BF16, 'mWg%d' % i) for i in range(2)]
            Wu = [p.sb([128, 8, 512], BF16, 'mWu%d' % i) for i in range(2)]
            Wd = [p.sb([128, 4, D], BF16, 'mWd%d' % i) for i in range(2)]
            hid = p.sb([128, 4, 512], BF16, 'mhid')
            sg = [p.sb([128, 512], BF16, 'msg%d' % i) for i in range(2)]
            tu = [p.sb([128, 512], BF16, 'mtu%d' % i) for i in range(2)]
            cwb = p.sb([128, 512], F32, 'mcwb')
            gps = [p.ps([128, 512], F32, 'mgps%d' % i) for i in range(2)]
            ups = [p.ps([128, 512], F32, 'mups%d' % i) for i in range(2)]
            cwp = p.ps([128, 512], F32, 'mcwp')
            yps = [p.ps([128, 512], F32, 'myps%d' % i) for i in range(2)]
            ky = 0
            for e in range(32):
                wi = e % 2
                p.dma('sp', Wg[wi][:], mg_bf[l, e].rearrange("(c p) n -> p c n", p=128), reads=['mg_bf'], writes=[('mWg', wi)])
                p.dma('pool', Wu[wi][:], mu_bf[l, e].rearrange("(c p) n -> p c n", p=128), reads=['mu_bf'], writes=[('mWu', wi)])
                p.dma('sp', Wd[wi][:], md_bf[l, e].rearrange("(c p) n -> p c n", p=128), reads=['md_bf'], writes=[('mWd', wi)])
                for (t0, n) in blks:
                    j = b if t0 >= LC else nb
                    p.i('pe', 'matmul', cwp[:, 0:n], lhsT=Esel[0:32, e, :], rhs=combT[0:32, t0:t0 + n], start=True, stop=True,
                        reads=['Esel', 'combT'], writes=['mcwp'])
                    p.i('act', 'activation', out=cwb[:, 0:n], in_=cwp[:, 0:n], func=AF.Copy, reads=['mcwp'], writes=['mcwb'])
                    for hc in range(4):
                        i = hc % 2
                        for kc in range(8):
                            p.i('pe', 'matmul', gps[i][:, 0:n], lhsT=Wg[wi][:, kc, hc * 128:(hc + 1) * 128], rhs=G['HT'][:, kc, t0:t0 + n],
                                start=(kc == 0), stop=(kc == 7), reads=[('mWg', wi), 'HT'], writes=[('mgps', i)], sig=(kc == 7))
                        for kc in range(8):
                            p.i('pe', 'matmul', ups[i][:, 0:n], lhsT=Wu[wi][:, kc, hc * 128:(hc + 1) * 128], rhs=G['HT'][:, kc, t0:t0 + n],
                                start=(kc == 0), stop=(kc == 7), reads=[('mWu', wi), 'HT'], writes=[('mups', i)], sig=(kc == 7))
                        p.i('act', 'activation', out=sg[i][:, 0:n], in_=gps[i][:, 0:n], func=AF.Silu, reads=[('mgps', i)], writes=[('msg', i)])
                        p.i('dve', 'tensor_tensor', out=tu[i][:, 0:n], in0=ups[i][:, 0:n], in1=cwb[:, 0:n], op=ALU.mult,
                            reads=[('mups', i), 'mcwb'], writes=[('mtu', i)])
                        p.i('pool', 'tensor_tensor', out=hid[:, hc, 0:n], in0=sg[i][:, 0:n], in1=tu[i][:, 0:n], op=ALU.mult,
                            reads=[('msg', i), ('mtu', i)], writes=[('mhid', hc)])
                    for dc in range(8):
                        i = ky % 2; ky += 1
                        for hc in range(4):
                            p.i('pe', 'matmul', yps[i][:, 0:n], lhsT=Wd[wi][:, hc, dc * 128:(dc + 1) * 128], rhs=hid[:, hc, 0:n],
                                start=(hc == 0), stop=(hc == 3), reads=[('mWd', wi), ('mhid', hc)], writes=[('myps', i)], sig=(hc == 3))
                        p.i('dve', 'scalar_tensor_tensor', out=XT[:, dc, t0:t0 + n], in0=yps[i][:, 0:n], scalar=modT[l][:, 40 + dc, j:j + 1],
                            in1=XT[:, dc, t0:t0 + n], op0=ALU.mult, op1=ALU.add,
                            reads=[('myps', i), 'modT%d' % l, ('XT', dc)], writes=[('XT', dc)])

    def final_norm(b):
        with p.phase():
            rs = p.sb([128, T], F32, 'rs')
            rstd_all(rs, BLKS[1:])
            fw = p.sb([128, 8], F32, 'fnw'); p.dma('sp', fw[:], fnw[:, :], writes=['fnw'])
            xn = [p.sb([128, NL], F32, 'fxn%d' % i) for i in range(2)]
            for c in range(8):
                xb = xn[c % 2]
                p.i('dve' if c % 2 == 0 else 'pool', 'tensor_tensor', out=xb[:], in0=XT[:, c, LC:T], in1=rs[:, LC:T], op=ALU.mult,
                    reads=['XT', 'rs'], writes=[('fxn', c % 2)])
                p.i('act', 'activation', out=xb[:], in_=xb[:], func=AF.Identity, scale=fw[:, c:c + 1], reads=[('fxn', c % 2), 'fnw'], writes=[('fxn', c % 2)])
                p.dma('sp', outT[b, c * 128:(c + 1) * 128, :], xb[:], reads=[('fxn', c % 2)], writes=['outT'])

    def moe_prep(layers):
        with p.phase():
            stf = [p.sb([128, 4096], F32, 'mpf%d' % i) for i in range(3)]
            stb = [p.sb([128, 4096], BF16, 'mpb%d' % i) for i in range(3)]
            engs = ['dve', 'pool', 'act']
            k = 0
            def cast(src_t, dst_t, dkey, ncol):
                nonlocal k
                i = k % 3; k += 1
                fv = stf[i][:].rearrange("p (c n) -> p c n", n=ncol); bv = stb[i][:].rearrange("p (c n) -> p c n", n=ncol)
                p.dma('sp', fv, src_t.rearrange("(c p) n -> p c n", p=128), writes=[('mpf', i)])
                if engs[i] == 'act':
                    p.i('act', 'activation', out=stb[i][:], in_=stf[i][:], func=AF.Copy, reads=[('mpf', i)], writes=[('mpb', i)])
                else:
                    p.i(engs[i], 'tensor_copy', out=stb[i][:], in_=stf[i][:], reads=[('mpf', i)], writes=[('mpb', i)])
                p.dma('pool', dst_t.rearrange("(c p) n -> p c n", p=128), bv, reads=[('mpb', i)], writes=[dkey])
            for l in layers:
                for c2 in range(2):
                    cast(w_out[l, c2 * 512:(c2 + 1) * 512, :], wout_bf[l, c2 * 512:(c2 + 1) * 512, :], 'wout_bf', 1024)
                for e in range(32):
                    cast(mwg[l, e], mg_bf[l, e], 'mg_bf', 512)
                    cast(mwu[l, e], mu_bf[l, e], 'mu_bf', 512)
                    cast(mwd[l, e], md_bf[l, e], 'md_bf', 1024)

    def proj(l, b):
        with p.phase():
            W = p.sb([128, 8, 1536], BF16, 'Wp')
            rc = p.sb([128, NL], F32, 'ropeC'); rsn = p.sb([128, NL], F32, 'ropeS')
            p.dma('pool', rc[:], ropeC_d[:, :], writes=['ropeC'])
            p.dma('pool', rsn[:], ropeS_d[:, :], writes=['ropeS'])
            pps = [p.ps([128, 512], F32, 'pps%d' % i) for i in range(4)]
            ev = [p.sb([128, 512], BF16, 'pev%d' % i) for i in range(4)]
            t1 = p.sb([128, 512], F32, 'pt1'); t2 = p.sb([128, 512], F32, 'pt2')
            wv = win_bf[l].rearrange("(c p) n -> p c n", p=128)
            cnt = [0]

            def evac(pi, n, dst_ap):
                i = cnt[0] % 4; cnt[0] += 1
                if i % 2 == 0:
                    p.i('act', 'activation', out=ev[i][:, 0:n], in_=pps[pi][:, 0:n], func=AF.Copy, reads=[('pps', pi)], writes=[('pev', i)])
                else:
                    p.i('dve', 'tensor_copy', out=ev[i][:, 0:n], in_=pps[pi][:, 0:n], reads=[('pps', pi)], writes=[('pev', i)])
                p.dma('sp', dst_ap, ev[i][:, 0:n], reads=[('pev', i)], writes=['Pfm'])

            def mm(wcol, pi, t0, n):
                for c in range(8):
                    p.i('pe', 'matmul', pps[pi][:, 0:n], lhsT=W[:, c, wcol:wcol + 128], rhs=G['HT'][:, c, t0:t0 + n],
                        start=(c == 0), stop=(c == 7), reads=['Wp', 'HT'], writes=[('pps', pi)], sig=(c == 7))

            p.dma('sp', W[:], wv[:, :, 0:1536], writes=['Wp'])
            for (t0, n) in BLKS:
                lat = t0 >= LC
                for (ga, gs, go) in ((0, 2, 0), (1, 3, 1), (4, 5, 4)):
                    mm(ga * 128, 0, t0, n)
                    if lat:
                        mm(gs * 128, 1, t0, n)
                        l0 = t0 - LC
                        p.i('dve', 'tensor_tensor', out=t1[:, 0:n], in0=pps[0][:, 0:n], in1=rc[:, l0:l0 + n], op=ALU.mult,
                            reads=[('pps', 0), 'ropeC'], writes=['pt1'])
                        p.i('dve', 'tensor_tensor', out=t2[:, 0:n], in0=pps[1][:, 0:n], in1=rsn[:, l0:l0 + n], op=ALU.mult,
                            reads=[('pps', 1), 'ropeS'], writes=['pt2'])
                        i = cnt[0] % 4; cnt[0] += 1
                        p.i('pool', 'tensor_tensor', out=ev[i][:, 0:n], in0=t1[:, 0:n], in1=t2[:, 0:n], op=ALU.add,
                            reads=['pt1', 'pt2'], writes=[('pev', i)])
                        p.dma('sp', Pfm[b, go * 128:(go + 1) * 128, t0:t0 + n], ev[i][:, 0:n], reads=[('pev', i)], writes=['Pfm'])
                    else:
                        evac(0, n, Pfm[b, go * 128:(go + 1) * 128, t0:t0 + n])
                for gi, g in enumerate(range(6, 12)):
                    pi = 2 + gi % 2
                    mm(g * 128, pi, t0, n)
                    evac(pi, n, Pfm[b, g * 128:(g + 1) * 128, t0:t0 + n])
            p.dma('sp', W[:, :, 0:1024], wv[:, :, 1536:2560], writes=['Wp'])
            for (t0, n) in BLKS:
                for gi, g in enumerate(range(12, 20)):
                    pi = gi % 4
                    mm(gi * 128, pi, t0, n)
                    evac(pi, n, Pfm[b, g * 128:(g + 1) * 128, t0:t0 + n])
            p.dma('sp', W[:, :, 0:NTM], wv[:, :, 2560:2560 + NTM], writes=['Wp'])
            evt = [p.sb([128, NTM], BF16, 'pevt%d' % i) for i in range(2)]
            for tt in range(NT):
                for (c0, cn, pi) in ((0, 512, 0), (512, 384, 1)):
                    for c in range(8):
                        p.i('pe', 'matmul', pps[pi][:, 0:cn], lhsT=G['HT'][:, c, tt * 128:(tt + 1) * 128], rhs=W[:, c, c0:c0 + cn],
                            start=(c == 0), stop=(c == 7), reads=['Wp', 'HT'], writes=[('pps', pi)], sig=(c == 7))
                i = tt % 2
                p.i('act', 'activation', out=evt[i][:, 0:512], in_=pps[0][:, 0:512], func=AF.Copy, reads=[('pps', 0)], writes=[('pevt', i)])
                p.i('dve', 'tensor_copy', out=evt[i][:, 512:896], in_=pps[1][:, 0:384], reads=[('pps', 1)], writes=[('pevt', i)])
                p.dma('sp', Ptm[b, tt * 128:(tt + 1) * 128, :], evt[i][:], reads=[('pevt', i)], writes=['Ptm'])

    layers = list(range(nlayers))
    if 'moe' in stages or 'wout' in stages:
        moe_prep(layers)
    for b in range(nb):
        p.dma('sp', XT[:], xT_d[b].rearrange("(c p) t -> p c t", p=128), writes=['XT'])
        for l in layers:
            last = (l == 1)
            blks = BLKS[1:] if last else BLKS
            with p.phase():
                G['HT'] = p.sb([128, 8, T], BF16, 'HT')
                if 'norm1' in stages:
                    norm_mod(l, b, g1, 'g1_', 0)
                if 'proj' in stages:
                    proj(l, b)
                if 'HT' in dbg and 'HT' not in dbg_o:
                    d_ = dout('HT', [128, 8, T], BF16)
                    p.dma('sp', d_[:, :, :], G['HT'][:], reads=['HT'], writes=['dbg_HT'])
            with p.phase():
                G['MIXT'] = p.sb([128, 8, T], BF16, 'MIXT')
                if 'swa' in stages:
                    swa(l, b, not last)
                if 'na' in stages:
                    na_prep(l)
                    na(l, b, not last)
                if 'hgrn' in stages:
                    hgrn(l, b)
                if 's5' in stages:
                    s5mix(l, b)
                if 'MIXT' in dbg and 'MIXT' not in dbg_o:
                    d_ = dout('MIXT', [128, 8, T], BF16)
                    p.dma('sp', d_[:, :, :], G['MIXT'][:], reads=['MIXT'], writes=['dbg_MIXT'])
                if 'wout' in stages:
                    wout(l, b, blks)
            if 'x1' in dbg and 'x1' not in dbg_o:
                d_ = dout('x1', [128, 8, T])
                p.dma('sp', d_[:, :, :], XT[:], reads=['XT'], writes=['dbg_x1'])
            if 'moe' in stages:
                with p.phase():
                    G['HT'] = p.sb([128, 8, T], BF16, 'HT')
                    LG = p.sb([128, NT, 36], F32, 'LG'); combT = p.sb([128, T], BF16, 'combT')
                    norm2_router(l, b, LG)
                    if 'LG' in dbg and 'LG' not in dbg_o:
                        d_ = dout('LG', [128, NT, 36])
                        p.dma('sp', d_[:, :, :], LG[:], reads=['LG'], writes=['dbg_LG'])
                    router_math(LG, combT)
                    if 'nomoeffn' not in stages:
                        moe_ffn(l, b, blks, combT)
            if 'x2' in dbg and ('x2_%d' % l) not in dbg_o and b == 0:
                d_ = dout('x2_%d' % l, [128, 8, T])
                p.dma('sp', d_[:, :, :], XT[:], reads=['XT'], writes=['dbg_x2_%d' % l])
        if 'final' in stages:
            final_norm(b)
    if 'P' in dbg:
        d1 = dout('Pfm', [NFM * 128, T], BF16); d2 = dout('Ptm', [T, NTM], BF16)
        p.dma('sp', d1[:, :], Pfm[nb - 1], reads=['Pfm'], writes=['dbg_Pfm'])
        p.dma('sp', d2[:, :], Ptm[nb - 1], reads=['Ptm'], writes=['dbg_Ptm'])
    nc = p.finish(['dbg_' + k for k in dbg_o] + (['outT'] if 'final' in stages else []))
    print('ninst', p.ninst, 'nwait', p.nwait, 'cnt', p.cnt, 'duse', p.duse)
    return nc


ALL_STAGES = ('mod', 'norm1', 'proj', 'swa', 'na', 'hgrn', 's5', 'wout', 'moe', 'final')


def kernel(**inputs):
    inputs = {k: np.asarray(v) for k, v in inputs.items()}
    shared = host_shared(inputs)
    nc = build(nb=NB, stages=ALL_STAGES, dbg=(), nlayers=2)
    in_maps = []
    for core in range(8):
        m = host_prep(inputs, core, nb=NB)
        m.update(shared)
        in_maps.append(m)
    res = run_bass_kernel_spmd(nc, in_maps, core_ids=list(range(8)))
    outs = [np.asarray(r['outT']) for r in res.results]
    out = np.concatenate(outs, 0).transpose(0, 2, 1)
    return np.ascontiguousarray(out).astype(np.float32)
```

```python
import numpy as np
from contextlib import ExitStack
import concourse.bass as bass
import concourse.mybir as mybir
from concourse.bass_utils import run_bass_kernel_spmd

F32 = mybir.dt.float32
BF16 = mybir.dt.bfloat16
I32 = mybir.dt.int32
U32 = mybir.dt.uint32
AF = mybir.ActivationFunctionType
ALU = mybir.AluOpType
AX = mybir.AxisListType

ENG = ('pe', 'act', 'dve', 'pool', 'sp')
NDMA = 24


class P:
    def __init__(self):
        self.nc = bass.Bass("TRN2", target_bir_lowering=False)
        self.es = ExitStack()
        nc = self.nc
        self.ops = {e: [] for e in ENG}
        self.cnt = {e: 0 for e in ENG}
        self.sem = {e: self.es.enter_context(nc.semaphore("s_" + e)) for e in ENG}
        self.dsem = [self.es.enter_context(nc.semaphore("d%d" % i)) for i in range(NDMA)]
        self.duse = [0] * NDMA
        self.dnext = 0
        self.known = {e: {} for e in ENG}
        self.res = {}
        self.ntens = 0
        self.nwait = 0

    def sb(self, shape, dt=F32, name=None):
        self.ntens += 1
        return self.es.enter_context(self.nc.sbuf_tensor((name or "t") + "_s%d" % self.ntens, list(shape), dt))

    def ps(self, shape, dt=F32, name=None):
        self.ntens += 1
        return self.es.enter_context(self.nc.psum_tensor((name or "p") + "_p%d" % self.ntens, list(shape), dt))

    def dram(self, name, shape, dt=F32, kind="Internal"):
        return self.nc.dram_tensor(name, list(shape), dt, kind=kind).ap()

    def _st(self, name):
        if name not in self.res:
            self.res[name] = {'whole': [None, []], 'subs': {}}
        return self.res[name]

    def _involved(self, key):
        if isinstance(key, tuple):
            name, idx = key
        else:
            name, idx = key, None
        st = self._st(name)
        if idx is None:
            return st, idx, [st['whole']] + list(st['subs'].values())
        if idx not in st['subs']:
            st['subs'][idx] = [None, []]
        return st, idx, [st['whole'], st['subs'][idx]]

    def _deps(self, reads, writes):
        deps = []
        for k in reads:
            _, _, inv = self._involved(k)
            for s in inv:
                if s[0] is not None:
                    deps.append(s[0])
        for k in writes:
            _, _, inv = self._involved(k)
            for s in inv:
                if s[0] is not None:
                    deps.append(s[0])
                deps.extend(s[1])
        return deps

    def _commit(self, reads, writes, tok):
        for k in reads:
            st, idx, inv = self._involved(k)
            (st['whole'] if idx is None else st['subs'][idx])[1].append(tok)
        for k in writes:
            st, idx, inv = self._involved(k)
            if idx is None:
                st['subs'] = {}
                st['whole'] = [tok, []]
            else:
                st['subs'][idx] = [tok, []]

    def _emit_waits(self, e, deps):
        kn = self.known[e]
        need = {}
        for (sk, v) in deps:
            if e == 'pe' and sk == 'pe':
                continue
            if kn.get(sk, 0) >= v:
                continue
            if need.get(sk, 0) < v:
                need[sk] = v
        for sk, v in need.items():
            kn[sk] = v
            sem = self.sem[sk] if isinstance(sk, str) else self.dsem[sk]
            self.ops[e].append(('wait', sem, v))
            self.nwait += 1

    def op(self, e, fn, reads=(), writes=(), sig=True):
        deps = self._deps(reads, writes)
        self._emit_waits(e, deps)
        if sig:
            self.cnt[e] += 1
            tok = (e, self.cnt[e])
        else:
            tok = (e, self.cnt[e] + 1)
        self.ops[e].append(('op', fn, sig))
        self._commit(reads, writes, tok)

    def i(self, e, method, *args, reads=(), writes=(), sig=True, **kwargs):
        self.op(e, lambda eng: getattr(eng, method)(*args, **kwargs), reads=reads, writes=writes, sig=sig)

    def dma(self, q, out, in_, reads=(), writes=(), **kw):
        deps = self._deps(reads, writes)
        j = self.dnext
        self.dnext = (self.dnext + 1) % NDMA
        if self.duse[j] > 0:
            deps.append((j, 16 * self.duse[j]))
        self._emit_waits(q, deps)
        self.duse[j] += 1
        tok = (j, 16 * self.duse[j])
        self.ops[q].append(('dma', out, in_, self.dsem[j], kw))
        self._commit(reads, writes, tok)

    def barrier(self):
        for e in ENG:
            deps = [(f, self.cnt[f]) for f in ENG if self.cnt[f] > 0]
            deps += [(j, 16 * self.duse[j]) for j in range(NDMA) if self.duse[j] > 0]
            self._emit_waits(e, deps)

    def phase(self):
        return _Phase(self)

    def flush(self):
        nc = self.nc
        if not any(self.ops[e] for e in ENG):
            return
        engs = {'pe': 'tensor', 'act': 'scalar', 'dve': 'vector', 'pool': 'gpsimd', 'sp': 'sync'}
        with nc.Block() as block:
            for e in ENG:
                lst = self.ops[e]
                semE = self.sem[e]

                def body(eng, lst=lst, semE=semE):
                    for it in lst:
                        if it[0] == 'wait':
                            eng.wait_ge(it[1], it[2])
                        elif it[0] == 'op':
                            inst = it[1](eng)
                            if it[2]:
                                inst.then_inc(semE, 1)
                        else:
                            eng.dma_start(out=it[1], in_=it[2], **it[4]).then_inc(it[3], 16)
                getattr(block, engs[e])(body)
        self.ninst = getattr(self, 'ninst', 0) + sum(len(self.ops[e]) for e in ENG)
        self.ops = {e: [] for e in ENG}

    def finish(self, final_keys):
        deps = self._deps(final_keys, [])
        self._emit_waits('sp', deps)
        self.flush()
        self.es.close()
        return self.nc


class _Phase:
    def __init__(self, p):
        self.p = p

    def __enter__(self):
        self.saved = self.p.es
        self.p.es = ExitStack()
        return self

    def __exit__(self, *a):
        self.p.barrier()
        self.p.flush()
        self.p.es.close()
        self.p.es = self.saved
        return False


import math

NB = 4
T = 2304
NT = 18
LC = 256
NL = 2048
D = 1024
NFM = 20
NTM = 896
BLKS = [(0, 256), (256, 512), (768, 512), (1280, 512), (1792, 512)]


def win_perm():
    A, B, C, Dd = 0, 512, 1280, 2560
    def hd(base, h): return list(range(base + 64 * h, base + 64 * h + 64))
    def sw(cols):
        c = np.array(cols)
        idx = np.concatenate([np.arange(16, 32), np.arange(0, 16), np.arange(48, 64), np.arange(32, 48)])
        return list(c[idx])
    fm = []
    fm += hd(A, 0) + hd(A, 2)
    fm += hd(A, 1) + hd(A, 3)
    fm += sw(hd(A, 0)) + sw(hd(A, 2))
    fm += sw(hd(A, 1)) + sw(hd(A, 3))
    fm += hd(A + 256, 0) + hd(A + 256, 1)
    fm += sw(hd(A + 256, 0)) + sw(hd(A + 256, 1))
    fm += list(range(B, B + 256))
    fm += list(range(B + 256, B + 512))
    fm += list(range(C, C + 256))
    fm += list(range(C + 256, C + 512))
    fm += list(range(C + 512, C + 768))
    fm += list(range(C + 1024, C + 1280))
    fm += list(range(Dd, Dd + 256))
    assert len(fm) == NFM * 128
    tm = list(range(A + 384, A + 512)) + list(range(B + 512, B + 768)) + list(range(C + 768, C + 1024)) + list(range(Dd, Dd + 256))
    assert len(tm) == NTM
    return np.array(fm + tm)


def rope_tables():
    half = 32
    inv = 1.0 / (10000.0 ** (np.arange(0, half, 2, dtype=np.float32) / half))
    t = np.arange(NL)
    row = (t // 64).astype(np.float32); col = (t % 64).astype(np.float32)
    ar = row[:, None] * inv[None, :]; ac = col[:, None] * inv[None, :]
    C = np.zeros((64, NL), np.float32); S = np.zeros((64, NL), np.float32)
    C[0:16] = np.cos(ar).T; C[16:32] = np.cos(ar).T; C[32:48] = np.cos(ac).T; C[48:64] = np.cos(ac).T
    S[0:16] = -np.sin(ar).T; S[16:32] = np.sin(ar).T; S[32:48] = -np.sin(ac).T; S[48:64] = np.sin(ac).T
    return np.concatenate([C, C], 0), np.concatenate([S, S], 0)


def host_prep(inp, core, nb=NB):
    f = np.float32
    b0 = core * nb
    m = {}
    x = inp['x'][b0:b0 + nb]; ctx = inp['ctx'][b0:b0 + nb]
    xt = np.concatenate([ctx, x], axis=1)
    m['xT'] = np.ascontiguousarray(xt.transpose(0, 2, 1)).astype(f)
    cc = np.concatenate([inp['c'][b0:b0 + nb], inp['c_ctx'][None, :]], 0)
    m['cT'] = np.ascontiguousarray(cc.T.reshape(8, 128, nb + 1).transpose(1, 0, 2)).astype(f)
    return m


def host_shared(inp):
    f = np.float32
    m = {}
    m['mod_w'] = inp['mod_w'].astype(f)
    m['mod_b2'] = np.ascontiguousarray(inp['mod_b'].reshape(2, 48, 128).transpose(0, 2, 1)).astype(f)
    for k in ('norm1_w', 'norm2_w'):
        m[k + '2'] = np.ascontiguousarray(inp[k].reshape(2, 8, 128).transpose(0, 2, 1)).astype(f)
    m['fnw2'] = np.ascontiguousarray(inp['final_norm_w'].reshape(8, 128).T).astype(f)
    m['w_in2'] = np.ascontiguousarray(inp['w_in'][:, :, win_perm()]).astype(f)
    m['w_out'] = inp['w_out'].astype(f)
    C, S = rope_tables()
    m['ropeC'] = C; m['ropeS'] = S
    import ml_dtypes
    bf = ml_dtypes.bfloat16
    i = np.arange(128)[:, None]; j = np.arange(128)[None, :]
    BIG = -240000.0
    mk = np.zeros((128, 384), f)
    mk[:, 0:128] = np.where(i <= j, 0.0, BIG)
    mk[:, 256:384] = np.where(j <= i, 0.0, BIG)
    m['swa_mask'] = mk.astype(bf)
    m['ident_bf'] = np.eye(128, dtype=f).astype(bf)
    m['ident_f'] = np.eye(128, dtype=f)
    m['swa_sink'] = inp['swa_sink'].astype(f)
    rp = np.zeros((2, 4, 15, 127), f); rp[:, :, :, 48:79] = inp['na_rpb']
    same = (i // 32) == (j // 32)
    m['hg_mask'] = np.stack([(same & (i <= j)), (same & (i >= j))]).astype(f).astype(bf)
    bo = np.zeros((128, 128), f); bo[:64, :64] = 1; bo[64:, 64:] = 1
    m['blockones'] = bo.astype(bf)
    m['hgrn_lb2'] = np.ascontiguousarray(inp['hgrn_lb'].reshape(2, 2, 2, 128).transpose(3, 0, 1, 2)).astype(f)
    G_, P_, H_ = 16, 64, 16
    def pdup(a):
        t = a.transpose(0, 3, 1, 2)
        return np.ascontiguousarray(np.concatenate([t, t], 1)).astype(f)
    m['s5_lre_p'] = pdup(inp['s5_lam_re']); m['s5_lim_p'] = pdup(inp['s5_lam_im'])
    m['s5_ldt_p'] = np.ascontiguousarray(np.broadcast_to(inp['s5_log_dt'][:, None, :, :], (2, 128, 2, 16))).astype(f)
    m['s5_lre_r'] = np.ascontiguousarray(inp['s5_lam_re'].reshape(2, 2, 1024)).astype(f)
    m['s5_lim_r'] = np.ascontiguousarray(inp['s5_lam_im'].reshape(2, 2, 1024)).astype(f)
    m['s5_ldt_r'] = np.ascontiguousarray(np.repeat(inp['s5_log_dt'], 64, axis=-1)).astype(f)
    def bemb(a):
        o = np.zeros((2, 2, 128, 16, 64), f)
        for g in range(16):
            o[:, :, (g % 8) * 16:(g % 8) * 16 + 16, g, :] = a[:, :, g].transpose(0, 1, 3, 2)
        return o
    m['s5_Br_emb'] = bemb(inp['s5_b_re']); m['s5_Bi_emb'] = bemb(inp['s5_b_im'])
    def cdup(a):
        t = a.transpose(0, 1, 4, 2, 3)
        return np.ascontiguousarray(np.concatenate([t, t], 2)).astype(f)
    m['s5_Cr2'] = cdup(inp['s5_c_re']); m['s5_Ci2'] = cdup(inp['s5_c_im'])
    m['s5_d2'] = np.ascontiguousarray(inp['s5_d'].reshape(2, 2, 128).transpose(0, 2, 1)).astype(f)
    m['s5_glu_w'] = inp['s5_glu_w'].astype(f)
    m['s5_glu_b2'] = np.ascontiguousarray(inp['s5_glu_b'].reshape(2, 2, 128).transpose(0, 2, 1)).astype(f)
    m['svec'] = np.ascontiguousarray(np.broadcast_to(np.arange(1, 129, dtype=f)[None, :], (128, 128)))
    m['Jrev128'] = np.ascontiguousarray(np.eye(128, dtype=f)[::-1]).astype(bf)
    msw = np.zeros((128, 128), f)
    for mm_ in range(64):
        msw[mm_ + 64, mm_] = -1.0
        msw[mm_, mm_ + 64] = 1.0
    m['MswT'] = msw
    es = np.zeros((32, 32, 128), f)
    for e_ in range(32):
        es[e_, e_, :] = 1.0
    m['esel'] = es.astype(bf)
    m['moe_rw'] = np.ascontiguousarray(np.concatenate([inp['moe_group_w'], inp['moe_expert_w']], -1)).astype(f)
    m['moe_rb'] = np.ascontiguousarray(np.concatenate([inp['moe_group_b'], inp['moe_expert_b']], -1)).astype(f)
    m['moe_w_gate'] = inp['moe_w_gate'].astype(f); m['moe_w_up'] = inp['moe_w_up'].astype(f); m['moe_w_down'] = inp['moe_w_down'].astype(f)
    m['hgrn_nw2'] = np.ascontiguousarray(np.concatenate([inp['hgrn_norm_w'], inp['hgrn_norm_w']], 1).T).astype(f)
    m['rpb_pad'] = rp
    m['Jrev'] = np.ascontiguousarray(np.eye(64, dtype=f)[::-1])
    kc = np.arange(64)[:, None]; qc = np.arange(64)[None, :]
    ws = np.clip(qc - 8, 0, 48)
    m['na_colmask'] = np.where((kc >= ws) & (kc < ws + 16), 0.0, BIG).astype(f)
    return m


def build(nb=NB, stages=('mod', 'norm1', 'proj'), dbg=(), nlayers=1):
    p = P(); nc = p.nc
    di = {}
    def din(name, shape, dt=F32):
        di[name] = p.dram(name, shape, dt, 'ExternalInput'); return di[name]
    xT_d = din('xT', [nb, D, T]); cT_d = din('cT', [128, 8, nb + 1])
    mod_w = din('mod_w', [2, D, 6 * D]); mod_b2 = din('mod_b2', [2, 128, 48])
    n1w = din('norm1_w2', [2, 128, 8]); n2w = din('norm2_w2', [2, 128, 8]); fnw = din('fnw2', [128, 8])
    w_in2 = din('w_in2', [2, D, NFM * 128 + NTM]); w_out = din('w_out', [2, D, D])
    ropeC_d = din('ropeC', [128, NL]); ropeS_d = din('ropeS', [128, NL])
    swa_mask_d = din('swa_mask', [128, 384], BF16); ident_bf_d = din('ident_bf', [128, 128], BF16)
    ident_f_d = din('ident_f', [128, 128]); swa_sink_d = din('swa_sink', [2, 4])
    rpb_pad_d = din('rpb_pad', [2, 4, 15, 127]); Jrev_d = din('Jrev', [64, 64]); na_cm_d = din('na_colmask', [64, 64])
    hg_mask_d = din('hg_mask', [2, 128, 128], BF16); blockones_d = din('blockones', [128, 128], BF16)
    hgrn_lb_d = din('hgrn_lb2', [128, 2, 2, 2]); hgrn_nw_d = din('hgrn_nw2', [128, 2])
    s5 = {}
    for nm, shp, dt_ in (('s5_lre_p', [2, 128, 2, 16], F32), ('s5_lim_p', [2, 128, 2, 16], F32), ('s5_ldt_p', [2, 128, 2, 16], F32),
                         ('s5_lre_r', [2, 2, 1024], F32), ('s5_lim_r', [2, 2, 1024], F32), ('s5_ldt_r', [2, 2, 1024], F32),
                         ('s5_Br_emb', [2, 2, 128, 16, 64], F32), ('s5_Bi_emb', [2, 2, 128, 16, 64], F32),
                         ('s5_Cr2', [2, 2, 128, 16, 16], F32), ('s5_Ci2', [2, 2, 128, 16, 16], F32),
                         ('s5_d2', [2, 128, 2], F32), ('s5_glu_w', [2, 256, 256], F32), ('s5_glu_b2', [2, 128, 2], F32),
                         ('svec', [128, 128], F32), ('Jrev128', [128, 128], BF16), ('MswT', [128, 128], F32)):
        s5[nm] = din(nm, shp, dt_)
    esel_d = din('esel', [32, 32, 128], BF16); moe_rw = din('moe_rw', [2, D, 36]); moe_rb = din('moe_rb', [2, 36])
    mwg = din('moe_w_gate', [2, 32, D, 512]); mwu = din('moe_w_up', [2, 32, D, 512]); mwd = din('moe_w_down', [2, 32, 512, D])
    mg_bf = p.dram('mg_bf', [2, 32, D, 512], BF16); mu_bf = p.dram('mu_bf', [2, 32, D, 512], BF16); md_bf = p.dram('md_bf', [2, 32, 512, D], BF16)
    wout_bf = p.dram('wout_bf', [2, D, D], BF16)
    outT = p.dram('outT', [nb, D, NL], F32, 'ExternalOutput')
    dbg_o = {}
    def dout(name, shape, dt=F32):
        dbg_o[name] = p.dram('dbg_' + name, shape, dt, 'ExternalOutput'); return dbg_o[name]
    win_bf = p.dram('win_bf', [2, D, NFM * 128 + NTM], BF16)
    Pfm = p.dram('Pfm', [nb, NFM * 128, T], BF16)
    Ptm = p.dram('Ptm', [nb, T, NTM], BF16)
    NJ = nb + 1

    modT = [p.sb([128, 48, NJ], F32, 'modT%d' % l) for l in range(2)]
    g1 = [p.sb([128, 8, NJ], F32, 'g1_%d' % l) for l in range(2)]
    g2 = [p.sb([128, 8, NJ], F32, 'g2_%d' % l) for l in range(2)]
    ones_f = p.sb([128, 128], F32, 'ones_f')
    p.i('dve', 'memset', ones_f[:], 1.0, writes=['ones_f'])
    ones_b = p.sb([128, 128], BF16, 'ones_b')
    p.i('dve', 'memset', ones_b[:], 1.0, writes=['ones_b'])
    ident_b = p.sb([128, 128], BF16, 'ident_b'); ident_f = p.sb([128, 128], F32, 'ident_f')
    p.dma('sp', ident_b[:], ident_bf_d[:, :], writes=['ident_b'])
    p.dma('sp', ident_f[:], ident_f_d[:, :], writes=['ident_f'])

    with p.phase():
        c_sb = p.sb([128, 8, NJ]); sc = p.sb([128, 8, NJ])
        mb = p.sb([128, 2, 48]); nw1 = p.sb([128, 2, 8]); nw2 = p.sb([128, 2, 8])
        p.dma('sp', c_sb[:], cT_d[:, :, :], writes=['c_sb'])
        p.dma('sp', mb[:], mod_b2.rearrange("l p n -> p l n"), writes=['mb'])
        p.dma('sp', nw1[:], n1w.rearrange("l p n -> p l n"), writes=['nw1'])
        p.dma('sp', nw2[:], n2w.rearrange("l p n -> p l n"), writes=['nw2'])
        p.i('act', 'activation', out=sc[:], in_=c_sb[:], func=AF.Silu, reads=['c_sb'], writes=['sc'])
        mwb = [p.sb([128, 8, 512], F32, 'mw%d' % i) for i in range(2)]
        mps = p.ps([128, 48, 8], F32, 'mps')
        for l in range(2):
            mwv = mod_w[l].rearrange("(c p) n -> p c n", p=128)
            for g in range(12):
                mw = mwb[g % 2]
                p.dma('sp' if g % 2 == 0 else 'pool', mw[:], mwv[:, :, g * 512:(g + 1) * 512], writes=[('mw', g % 2)])
                for j in range(4):
                    for kc in range(8):
                        p.i('pe', 'matmul', mps[:, g * 4 + j, 0:NJ], lhsT=mw[:, kc, j * 128:(j + 1) * 128], rhs=sc[:, kc, :],
                            start=(kc == 0), stop=(kc == 7), reads=[('mw', g % 2), 'sc'], writes=['mps'], sig=(kc == 7))
            p.i('dve', 'tensor_tensor', out=modT[l][:], in0=mps[:, :, 0:NJ],
                in1=mb[:, l, :].unsqueeze(2).to_broadcast([128, 48, NJ]), op=ALU.add,
                reads=['mps', 'mb'], writes=['modT%d' % l])
            for (gg, nw, mi, nm) in ((g1, nw1, 1, 'g1_'), (g2, nw2, 4, 'g2_')):
                p.i('dve', 'scalar_tensor_tensor', out=gg[l][:], in0=modT[l][:, mi * 8:(mi + 1) * 8, :], scalar=1.0,
                    in1=nw[:, l, :].unsqueeze(2).to_broadcast([128, 8, NJ]), op0=ALU.add, op1=ALU.mult,
                    reads=['modT%d' % l, 'nw1', 'nw2'], writes=[nm + str(l)])
        if 'mod' in dbg:
            d_ = dout('modT0', [128, 48, NJ])
            p.dma('sp', d_[:, :, :], modT[0][:], reads=['modT0'], writes=['dbg_modT0'])

    if 'proj' in stages:
        with p.phase():
            st = [p.sb([128, NFM * 128 + NTM], F32, 'wst%d' % i) for i in range(2)]
            sb_ = [p.sb([128, NFM * 128 + NTM], BF16, 'wsb%d' % i) for i in range(2)]
            k = 0
            for l in range(nlayers):
                for c in range(8):
                    i = k % 2; k += 1
                    p.dma('sp', st[i][:], w_in2[l, c * 128:(c + 1) * 128, :], writes=[('wst', i)])
                    p.i('dve' if c % 2 == 0 else 'pool', 'tensor_copy', out=sb_[i][:], in_=st[i][:],
                        reads=[('wst', i)], writes=[('wsb', i)])
                    p.dma('pool', win_bf[l, c * 128:(c + 1) * 128, :], sb_[i][:], reads=[('wsb', i)], writes=['win_bf'])

    XT = p.sb([128, 8, T], F32, 'XT')
    G = {}

    GTds = [p.sb([128, 4, 15, 64], BF16, 'GTd0')] * 2

    def na_prep(l):
        GTd = GTds[l]
        with p.phase():
            Hk = p.sb([64, 60, 64], F32, 'naH'); Jr = p.sb([64, 64], F32, 'naJ'); cm = p.sb([64, 64], F32, 'nacm')
            for hh in range(4):
                src = bass.AP(tensor=rpb_pad_d.tensor, offset=(l * 4 + hh) * 15 * 127, ap=[[1, 64], [127, 15], [1, 64]])
                p.dma('sp', Hk[:, hh * 15:(hh + 1) * 15, :], src, writes=['naH'])
            p.dma('sp', Jr[:], Jrev_d[:, :], writes=['naJ'])
            p.dma('sp', cm[:], na_cm_d[:, :], writes=['nacm'])
            tp = [p.ps([64, 8, 64], F32, 'natp%d' % i) for i in range(2)]
            k = 0
            for h in range(4):
                for d0 in (0, 8):
                    nd = min(8, 15 - d0)
                    ti = k % 2; k += 1
                    for j in range(nd):
                        dr = d0 + j
                        p.i('pe', 'matmul', tp[ti][:, j, :], lhsT=Hk[:, h * 15 + dr, :], rhs=Jr[:], start=True, stop=True,
                            reads=['naH', 'naJ'], writes=[('natp', ti)], sig=(j == nd - 1))
                    for j in range(nd):
                        dr = d0 + j
                        p.i('dve', 'scalar_tensor_tensor', out=GTd[(h % 2) * 64:(h % 2) * 64 + 64, h, 14 - dr, :], in0=tp[ti][:, j, :], scalar=8.0, in1=cm[:],
                            op0=ALU.mult, op1=ALU.add, reads=[('natp', ti), 'nacm'], writes=['GTd'])

    def na(l, b, need_ctx):
        rs_ = [min(max(qr - 4, 0), 24) for qr in range(32)]
        rng = []
        for kr in range(32):
            qs = [qr for qr in range(32) if rs_[qr] <= kr <= rs_[qr] + 7]
            assert qs == list(range(qs[0], qs[-1] + 1))
            rng.append((qs[0], qs[-1]))
        GTd = GTds[l]
        with p.phase():
            qg = [p.sb([128, T], BF16, 'naq%d' % i) for i in range(2)]
            kg = [p.sb([128, T], BF16, 'nak%d' % i) for i in range(2)]
            V = p.sb([128, 2, 256], BF16, 'nav')
            Vr = p.sb([128, 36, 256], BF16, 'navr')
            p.dma('sp', qg[0][:], Pfm[b, 768:896, :], reads=['Pfm'], writes=['naq0'])
            p.dma('pool', qg[1][:], Pfm[b, 896:1024, :], reads=['Pfm'], writes=['naq1'])
            p.dma('sp', kg[0][:], Pfm[b, 1024:1152, :], reads=['Pfm'], writes=['nak0'])
            p.dma('pool', kg[1][:], Pfm[b, 1152:1280, :], reads=['Pfm'], writes=['nak1'])
            p.dma('sp', V[:], Ptm[b, 0:256, :].rearrange("(n p) c -> p n c", p=128)[:, :, 128:384], reads=['Ptm'], writes=['nav'])
            p.dma('sp', Vr[0:64], Ptm[b].rearrange("(n p) c -> p n c", p=64)[:, :, 128:384], reads=['Ptm'], writes=['nav'])
            O = p.ps([64, 1024], F32, 'naO'); Dn = p.ps([64, 1024], F32, 'naD')
            st = [p.ps([128, 512], F32, 'nast%d' % i) for i in range(2)]
            pt = [p.sb([128, 512], BF16, 'napt%d' % i) for i in range(3)]
            rd = p.sb([64, 1024], F32, 'nard')
            cnt = [0]
            GTf = GTd[:].rearrange('p h d c -> p h (d c)')
            for h in range(4):
                qT = qg[h // 2]; kT = kg[h // 2]; base = (h % 2) * 64
                qsets = [(LC + 1024 * qh, 1024, True, qh) for qh in range(2)]
                if need_ctx:
                    qsets.append((0, LC, False, None))
                for (tq0, nqs, lat, qh) in qsets:
                    started = [False] * ((nqs + 511) // 512)

                    def pv(ptb, pti, vl, a, bnd, pb):
                        c = a
                        while c < bnd:
                            e_ = min(bnd, (c // 512 + 1) * 512)
                            bk = c // 512
                            stt = not started[bk]
                            if stt:
                                assert c % 512 == 0 and e_ - c == min(512, nqs - c)
                                started[bk] = True
                            K = vl.shape[0]
                            p.i('pe', 'matmul', O[:, c:e_], lhsT=vl, rhs=ptb[pb:pb + K, c - a:e_ - a],
                                start=stt, stop=False, reads=['nav', ('napt', pti)], writes=['naO'], sig=False, skip_group_check=True)
                            p.i('pe', 'matmul', Dn[:, c:e_], lhsT=ones_b[pb:pb + K, 0:64], rhs=ptb[pb:pb + K, c - a:e_ - a],
                                start=stt, stop=False, reads=['ones_b', ('napt', pti)], writes=['naD'], sig=True, skip_group_check=True)
                            c = e_

                    for ct in range(2):
                        for c0 in range(0, nqs, 512):
                            n = min(512, nqs - c0)
                            si = cnt[0] % 2; pi = cnt[0] % 3; cnt[0] += 1
                            p.i('pe', 'matmul', st[si][:, 0:n], lhsT=kT[base:base + 64, ct * 128:(ct + 1) * 128],
                                rhs=qT[base:base + 64, tq0 + c0:tq0 + c0 + n], start=True, stop=True,
                                reads=['nak%d' % (h // 2), 'naq%d' % (h // 2)], writes=[('nast', si)])
                            p.i('act', 'activation', out=pt[pi][:, 0:n], in_=st[si][:, 0:n], func=AF.Exp, scale=0.125,
                                reads=[('nast', si)], writes=[('napt', pi)])
                            pv(pt[pi], pi, V[:, ct, h * 64:(h + 1) * 64], c0, c0 + n, 0)
                    import os
                    NADBG = os.environ.get('NADBG', '')
                    if lat and 'nolat' not in NADBG:
                        for kr in range(0, 32, 2 if 'even' in NADBG else 1):
                            qlo, qhi = rng[kr]
                            for (ra, rb) in ((16 * qh, 16 * qh + 7), (16 * qh + 8, 16 * qh + 15)):
                                r0 = max(qlo, ra); r1 = min(qhi, rb)
                                if r0 > r1:
                                    continue
                                n = (r1 - r0 + 1) * 64
                                pb = 0
                                ktok = LC + kr * 64
                                qa = LC + r0 * 64
                                d0 = r0 - kr + 7
                                si = cnt[0] % 2; pi = cnt[0] % 3; cnt[0] += 1
                                p.i('pe', 'matmul', st[si][pb:pb + 64, 0:n], lhsT=kT[base:base + 64, ktok:ktok + 64],
                                    rhs=qT[base:base + 64, qa:qa + n], start=True, stop=False,
                                    reads=['nak%d' % (h // 2), 'naq%d' % (h // 2)], writes=[('nast', si)], sig=False)
                                if 'nobias' in NADBG:
                                    p.i('pe', 'matmul', st[si][pb:pb + 64, 0:n], lhsT=kT[base:base + 64, ktok:ktok + 64],
                                        rhs=qT[base:base + 64, qa:qa + n], start=False, stop=True,
                                        reads=['nak%d' % (h // 2), 'naq%d' % (h // 2)], writes=[('nast', si)])
                                else:
                                    p.i('pe', 'matmul', st[si][pb:pb + 64, 0:n], lhsT=ident_b[base:base + 64, base:base + 64],
                                        rhs=GTf[base:base + 64, h, d0 * 64:(d0 + r1 - r0 + 1) * 64], start=False, stop=True,
                                        reads=['ident_b', 'GTd'], writes=[('nast', si)])
                                p.i('act', 'activation', out=pt[pi][pb:pb + 64, 0:n], in_=st[si][pb:pb + 64, 0:n], func=AF.Exp, scale=0.125,
                                    reads=[('nast', si)], writes=[('napt', pi)])
                                a = (r0 - 16 * qh) * 64
                                pv(pt[pi], pi, Vr[0:64, 4 + kr, h * 64:(h + 1) * 64], a, a + n, pb)
                    p.i('dve', 'reciprocal', out=rd[:, 0:nqs], in_=Dn[:, 0:nqs], reads=['naD'], writes=['nard'])
                    ob = (h % 2) * 64
                    p.i('dve', 'tensor_tensor', out=G['MIXT'][ob:ob + 64, 2 + h // 2, tq0:tq0 + nqs], in0=O[:, 0:nqs], in1=rd[:, 0:nqs], op=ALU.mult,
                        reads=['naO', 'nard'], writes=[('MIXT', 2 + h // 2)])

    def hgrn(l, b):
        NCH = T // 32
        orders = [list(range(NCH)), list(range(7, -1, -1)) + list(range(NCH - 1, 7, -1))]
        import os
        HGDBG = os.environ.get('HGDBG', '')
        PL = 'dve' if 'nopool' in HGDBG else 'pool'
        with p.phase():
            msk = p.sb([128, T // 2], F32, 'hgmsk')
            p.i(PL, 'memset', msk[:], 1.0, writes=['hgmsk'])
            p.i(PL, 'memset', msk[:].rearrange("p (c k) -> p c k", k=32)[:, :, 0:1], 0.0, writes=['hgmsk'])
            hm = p.sb([128, 2, 128], BF16, 'hgm'); bo = p.sb([128, 128], BF16, 'hgbo')
            p.dma('sp', hm[:], hg_mask_d.rearrange("d s t -> s d t"), writes=['hgm'])
            p.dma('sp', bo[:], blockones_d[:, :], writes=['hgbo'])
            lbr = p.sb([128, 2, 2, 2], F32, 'hglbr'); lbv = p.sb([128, 2, 2], F32, 'hglb'); oml = p.sb([128, 2, 2], F32, 'hgoml')
            nw = p.sb([128, 2], F32, 'hgnw')
            p.dma('sp', lbr[:], hgrn_lb_d[:, :, :, :], writes=['hglbr'])
            p.dma('sp', nw[:], hgrn_nw_d[:, :], writes=['hgnw'])
            if l == 0:
                p.i('dve', 'memset', lbv[:], 0.0, writes=['hglb'])
            else:
                p.i('dve', 'tensor_tensor', out=lbv[:], in0=lbr[:, :, 1, :], in1=lbr[:, :, 0, :], op=ALU.subtract, reads=['hglbr'], writes=['hglb'])
                p.i('act', 'activation', out=lbv[:], in_=lbv[:], func=AF.Sigmoid, reads=['hglb'], writes=['hglb'])
            p.i('dve', 'tensor_scalar', out=oml[:], in0=lbv[:], scalar1=-1.0, scalar2=1.0, op0=ALU.mult, op1=ALU.add, reads=['hglb'], writes=['hgoml'])
            HN = T // 2
            tA = p.sb([128, HN], F32, 'hgA'); tB = p.sb([128, HN], F32, 'hgB'); tC = p.sb([128, HN], F32, 'hgC'); tD = p.sb([128, HN], F32, 'hgD')
            zb = p.sb([128, HN], BF16, 'hgz')
            sq_ = p.sb([128, T], BF16, 'hgsq')
            qt = [p.sb([128, T], BF16, 'hgqt%d' % d) for d in range(2)]
            kt = [p.sb([128, T], BF16, 'hgkt%d' % d) for d in range(2)]
            Sall = [p.sb([128, NCH, 64], BF16, 'hgS%d' % d) for d in range(2)]
            KhT = p.sb([128, T], BF16, 'hgKhT'); Khtm = p.sb([128, NT, 128], BF16, 'hgKhtm')
            Vc = p.sb([128, NT, 128], BF16, 'hgV')
            Dc = p.sb([128, NCH], F32, 'hgDc')
            Srun = [p.sb([128, 64], F32, 'hgSr%d' % i) for i in range(2)]
            gsb = p.sb([128, 512], BF16, 'hgg')
            tpa = p.ps([128, 128], F32, 'hgtpa'); tpb = p.ps([128, 128], F32, 'hgtpb'); tps = [tpa[:], tpb[:]]
            Ups = [p.ps([128, 512], F32, 'hgU%d' % i) for i in range(2)]
            Ops = [p.ps([128, 512], F32, 'hgO%d' % i) for i in range(2)]
            Aps = tps
            SSp = p.ps([128, 512], F32, 'hgSS')
            attn = [p.sb([128, 128], BF16, 'hgat%d' % i) for i in range(3)]
            n1 = p.sb([128, 512], F32, 'hgn1'); n2 = p.sb([128, 512], F32, 'hgn2'); n3 = p.sb([128, 512], BF16, 'hgn3')
            for ct in range(2):
                for hf in range(2):
                    t0 = hf * HN
                    p.dma('sp', zb[:], Pfm[b, (10 + ct) * 128:(11 + ct) * 128, t0:t0 + HN], reads=['Pfm'], writes=['hgz'])
                    p.i('act', 'activation', out=sq_[:, t0:t0 + HN], in_=zb[:], func=AF.Silu, reads=['hgz'], writes=['hgsq'])
                p.dma('pool', Vc[:], Ptm[b].rearrange("(n p) c -> p n c", p=128)[:, :, 384 + ct * 128:512 + ct * 128], reads=['Ptm'], writes=['hgV'])
                for d in range(2):
                    lb_ap = lbv[:, d, ct:ct + 1]; oml_ap = oml[:, d, ct:ct + 1]
                    for hf in range(2):
                        t0 = hf * HN; c0 = t0 // 32; ncq = HN // 32
                        p.dma('sp', zb[:], Pfm[b, (12 + 2 * d + ct) * 128:(13 + 2 * d + ct) * 128, t0:t0 + HN], reads=['Pfm'], writes=['hgz'])
                        p.i('act', 'activation', out=tA[:], in_=zb[:], func=AF.Sigmoid, reads=['hgz'], writes=['hgA'])
                        p.i('dve', 'tensor_scalar', out=tA[:], in0=tA[:], scalar1=oml_ap, scalar2=lb_ap, op0=ALU.mult, op1=ALU.add,
                            reads=['hgA', 'hglb', 'hgoml'], writes=['hgA'])
                        p.i('act', 'activation', out=tB[:], in_=tA[:], func=AF.Ln, reads=['hgA'], writes=['hgB'])
                        p.i('dve', 'tensor_tensor_scan', out=tC[:], data0=msk[:, 0:HN], data1=tB[:], initial=0.0, op0=ALU.mult, op1=ALU.add,
                            reads=['hgmsk', 'hgB'], writes=['hgC'])
                        p.i(PL, 'tensor_scalar', out=tA[:], in0=tA[:], scalar1=-1.0, scalar2=1.0, op0=ALU.mult, op1=ALU.add,
                            reads=['hgA'], writes=['hgA'])
                        C3 = tC[:].rearrange("p (c k) -> p c k", k=32)
                        totb = C3[:, :, 31:32].to_broadcast([128, ncq, 32])
                        p.i('act', 'activation', out=Dc[:, c0:c0 + ncq], in_=C3[:, :, 31], func=AF.Exp, reads=['hgC'], writes=['hgDc'])
                        B3 = tB[:].rearrange("p (c k) -> p c k", k=32); D3 = tD[:].rearrange("p (c k) -> p c k", k=32)
                        if d == 0:
                            p.i('dve', 'tensor_tensor', out=B3, in0=totb, in1=C3, op=ALU.subtract, reads=['hgC'], writes=['hgB'])
                            b_t, bl_t, bk, blk = tC, tB, 'hgC', 'hgB'
                        else:
                            p.i('dve', 'tensor_tensor', out=tB[:], in0=tC[:], in1=tB[:], op=ALU.subtract, reads=['hgC', 'hgB'], writes=['hgB'])
                            p.i('dve', 'tensor_tensor', out=D3, in0=totb, in1=B3, op=ALU.subtract, reads=['hgC', 'hgB'], writes=['hgD'])
                            b_t, bl_t, bk, blk = tD, tB, 'hgD', 'hgB'
                        p.i('act', 'activation', out=bl_t[:], in_=bl_t[:], func=AF.Exp, reads=[blk], writes=[blk])
                        p.i(PL, 'tensor_tensor', out=KhT[:, t0:t0 + HN], in0=tA[:], in1=bl_t[:], op=ALU.mult, reads=['hgA', blk], writes=['hgKhT'])
                        o_t, ok = (tD, 'hgD') if d == 0 else (tC, 'hgC')
                        p.i('act', 'activation', out=o_t[:], in_=b_t[:], func=AF.Exp, scale=-1.0, reads=[bk], writes=[ok])
                        p.i('dve', 'tensor_tensor', out=kt[d][:, t0:t0 + HN], in0=tA[:], in1=o_t[:], op=ALU.mult, reads=['hgA', ok], writes=['hgkt%d' % d])
                        p.i('act', 'activation', out=b_t[:], in_=b_t[:], func=AF.Exp, reads=[bk], writes=[bk])
                        p.i('dve', 'tensor_tensor', out=qt[d][:, t0:t0 + HN], in0=sq_[:, t0:t0 + HN], in1=b_t[:], op=ALU.mult, reads=['hgsq', bk], writes=['hgqt%d' % d])
                    for tt in range(0 if 'notr' in HGDBG else NT):
                        ti = tt % 2
                        p.i('pe', 'matmul', tps[ti], lhsT=KhT[:, tt * 128:(tt + 1) * 128], rhs=ident_b[:], start=True, stop=True,
                            reads=['hgKhT', 'ident_b'], writes=[('hgtp', ti)])
                        if tt % 2 == 0:
                            p.i('act', 'activation', out=Khtm[:, tt, :], in_=tps[ti], func=AF.Copy, reads=[('hgtp', ti)], writes=['hgKhtm'])
                        else:
                            p.i('dve', 'tensor_copy', out=Khtm[:, tt, :], in_=tps[ti], reads=[('hgtp', ti)], writes=['hgKhtm'])
                    order = orders[d]
                    p.i('dve', 'memset', Srun[0][:], 0.0, writes=[('hgSr', 0)])
                    p.i(PL, 'memset', Sall[d][:, order[0], :], 0.0, writes=['hgS%d' % d])
                    import os
                    HGDBG = os.environ.get('HGDBG', '')
                    for idx in range(0 if 'nostate' in HGDBG else NCH - 1):
                        c = order[idx]; tile_ = c // 4; j = c % 4
                        bank = (idx // 8) % 2; slot = idx % 8
                        for hl in range(2):
                            p.i('pe', 'matmul', Ups[bank][hl * 64:(hl + 1) * 64, slot * 64:(slot + 1) * 64], lhsT=Khtm[32 * j:32 * j + 32, tile_, hl * 64:(hl + 1) * 64],
                                rhs=Vc[32 * j:32 * j + 32, tile_, hl * 64:(hl + 1) * 64], start=True, stop=True, tile_position=(32 * j, 64 * hl),
                                reads=['hgKhtm', 'hgV'], writes=[('hgU', bank)], sig=(hl == 1))
                        so = Srun[idx % 2]; sn = Srun[(idx + 1) % 2]
                        p.i('dve', 'scalar_tensor_tensor', out=sn[:], in0=so[:], scalar=Dc[:, c:c + 1], in1=Ups[bank][:, slot * 64:(slot + 1) * 64], op0=ALU.mult, op1=ALU.add,
                            reads=[('hgSr', idx % 2), 'hgDc', ('hgU', bank)], writes=[('hgSr', (idx + 1) % 2)])
                        p.i('act', 'activation', out=Sall[d][:, order[idx + 1], :], in_=sn[:], func=AF.Copy,
                            reads=[('hgSr', (idx + 1) % 2)], writes=['hgS%d' % d])
                acnt = 0
                for gi, (t0, n) in enumerate([] if 'nopass2' in HGDBG else BLKS):
                    Op = Ops[gi % 2]; ok_ = ('hgO', gi % 2)
                    p.dma('sp', gsb[:, 0:n], Pfm[b, (16 + ct) * 128:(17 + ct) * 128, t0:t0 + n], reads=['Pfm'], writes=['hgg'])
                    for tl in range(n // 128):
                        tt = (t0 // 128) + tl
                        cs = slice(tl * 128, (tl + 1) * 128); ts_ = slice(tt * 128, (tt + 1) * 128)
                        for hl in range(2):
                            hb = hl * 64
                            for d in range(2):
                                ai = acnt % 2; ati = acnt % 3; acnt += 1
                                p.i('pe', 'matmul', Aps[ai], lhsT=kt[d][hb:hb + 64, ts_], rhs=qt[d][hb:hb + 64, ts_], start=True, stop=True,
                                    reads=['hgkt%d' % d, 'hgqt%d' % d], writes=[('hgtp', ai)])
                                p.i('dve', 'tensor_tensor', out=attn[ati][:], in0=Aps[ai], in1=hm[:, d, :], op=ALU.mult,
                                    reads=[('hgtp', ai), 'hgm'], writes=[('hgat', ati)])
                                p.i('pe', 'matmul', Op[hb:hb + 64, cs], lhsT=Vc[:, tt, hb:hb + 64], rhs=attn[ati][:], start=(d == 0), stop=False,
                                    reads=['hgV', ('hgat', ati)], writes=[ok_], sig=False, skip_group_check=True)
                                for j in range(4):
                                    c = tt * 4 + j
                                    p.i('pe', 'matmul', Op[hb:hb + 64, tl * 128 + 32 * j:tl * 128 + 32 * j + 32], lhsT=Sall[d][hb:hb + 64, c, :],
                                        rhs=qt[d][hb:hb + 64, c * 32:(c + 1) * 32], start=False, stop=(d == 1),
                                        reads=['hgS%d' % d, 'hgqt%d' % d], writes=[ok_], sig=(j == 3), skip_group_check=True)
                    p.i('act', 'activation', out=n3[:, 0:n], in_=Op[:, 0:n], func=AF.Square, reads=[ok_], writes=['hgn3'])
                    p.i('pe', 'matmul', SSp[:, 0:n], lhsT=bo[:], rhs=n3[:, 0:n], start=True, stop=True, reads=['hgbo', 'hgn3'], writes=['hgSS'])
                    p.i('dve', 'tensor_scalar', out=n1[:, 0:n], in0=SSp[:, 0:n], scalar1=1.0 / 64, scalar2=1e-6, op0=ALU.mult, op1=ALU.add,
                        reads=['hgSS'], writes=['hgn1'])
                    p.i('act', 'activation', out=n1[:, 0:n], in_=n1[:, 0:n], func=AF.Sqrt, reads=['hgn1'], writes=['hgn1'])
                    p.i('dve', 'reciprocal', out=n1[:, 0:n], in_=n1[:, 0:n], reads=['hgn1'], writes=['hgn1'])
                    p.i('act', 'activation', out=n2[:, 0:n], in_=gsb[:, 0:n], func=AF.Silu, reads=['hgg'], writes=['hgn2'])
                    p.i('dve', 'scalar_tensor_tensor', out=n1[:, 0:n], in0=n1[:, 0:n], scalar=nw[:, l:l + 1], in1=n2[:, 0:n], op0=ALU.mult, op1=ALU.mult,
                        reads=['hgn1', 'hgn2', 'hgnw'], writes=['hgn1'])
                    p.i('dve', 'tensor_tensor', out=G['MIXT'][:, 4 + ct, t0:t0 + n], in0=Op[:, 0:n], in1=n1[:, 0:n], op=ALU.mult,
                        reads=[ok_, 'hgn1'], writes=[('MIXT', 4 + ct)])

    TWO_PI = 6.283185307179586
    PI = 3.141592653589793

    def sincos(src, n, sin_out, cos_out, tmpf, tmpi, key_src, keys, mpi, eng='dve'):
        kf, ki, ks, kc = keys
        for (off, out_ap, ko) in ((0.0, sin_out, ks), (PI / 2, cos_out, kc)):
            p.i('dve', 'tensor_scalar', out=tmpf, in0=src, scalar1=off, scalar2=1.0 / TWO_PI, op0=ALU.add, op1=ALU.mult, reads=[key_src], writes=[kf])
            p.i('dve', 'tensor_copy', out=tmpi, in_=tmpf, reads=[kf], writes=[ki])
            p.i('dve', 'tensor_copy', out=tmpf, in_=tmpi, reads=[ki], writes=[kf])
            p.i('dve', 'scalar_tensor_tensor', out=tmpf, in0=tmpf, scalar=-TWO_PI, in1=src, op0=ALU.mult, op1=ALU.add, reads=[kf, key_src], writes=[kf])
            p.i('dve', 'tensor_scalar', out=tmpf, in0=tmpf, scalar1=off + PI, scalar2=None, op0=ALU.add, reads=[kf], writes=[kf])
            p.i('act', 'activation', out=out_ap, in_=tmpf, func=AF.Sin, bias=mpi[:], reads=[kf, 's5mpi'], writes=[ko])

    def s5mix(l, b):
        with p.phase():
            mpi = p.sb([128, 1], F32, 's5mpi')
            p.i('dve', 'memset', mpi[:], -PI, writes=['s5mpi'])
            Yacc = p.sb([128, 2, T], F32, 's5Y')
            d2 = p.sb([128, 2], F32, 's5d'); gb = p.sb([128, 2], F32, 's5gb')
            p.dma('sp', d2[:], s5['s5_d2'][l], writes=['s5d']); p.dma('sp', gb[:], s5['s5_glu_b2'][l], writes=['s5gb'])
            J128 = p.sb([128, 128], BF16, 's5J'); Msw = p.sb([128, 128], F32, 's5Msw'); sv = p.sb([128, 128], F32, 's5sv')
            p.dma('sp', J128[:], s5['Jrev128'][:, :], writes=['s5J']); p.dma('sp', Msw[:], s5['MswT'][:, :], writes=['s5Msw'])
            p.dma('sp', sv[:], s5['svec'][:, :], writes=['s5sv'])
            with p.phase():
                ub = p.sb([128, T], BF16, 's5ub')
                for ct in range(2):
                    p.dma('sp', ub[:], Pfm[b, (18 + ct) * 128:(19 + ct) * 128, :], reads=['Pfm'], writes=['s5ub'])
                    p.i('dve', 'tensor_scalar', out=Yacc[:, ct, :], in0=ub[:], scalar1=d2[:, ct:ct + 1], scalar2=None, op0=ALU.mult,
                        reads=['s5ub', 's5d'], writes=[('s5Y', ct)])
            cosT = p.sb([128, 16, 128], F32, 's5cos'); sinT = p.sb([128, 16, 128], F32, 's5sin')
            Bw1 = p.sb([128, 16, 128], BF16, 's5Bw1'); Bw2 = p.sb([128, 16, 128], BF16, 's5Bw2')
            C1 = p.sb([128, 16, 16], BF16, 's5C1'); C2 = p.sb([128, 16, 16], BF16, 's5C2')
            rho = p.sb([128, 16], F32, 's5rho')
            for d in range(2):
                Rm = ident_b if d == 0 else J128
                rkey = 'ident_b' if d == 0 else 's5J'
                with p.phase():
                    lre = p.sb([128, 16], F32, 'q_lre'); lim = p.sb([128, 16], F32, 'q_lim'); ldt = p.sb([128, 16], F32, 'q_ldt')
                    p.dma('sp', lre[:], s5['s5_lre_p'][l, :, d, :], writes=['q_lre']); p.dma('sp', lim[:], s5['s5_lim_p'][l, :, d, :], writes=['q_lim'])
                    p.dma('sp', ldt[:], s5['s5_ldt_p'][l, :, d, :], writes=['q_ldt'])
                    p.i('act', 'activation', out=ldt[:], in_=ldt[:], func=AF.Exp, reads=['q_ldt'], writes=['q_ldt'])
                    p.i('dve', 'tensor_tensor', out=lre[:], in0=lre[:], in1=ldt[:], op=ALU.mult, reads=['q_lre', 'q_ldt'], writes=['q_lre'])
                    p.i('act', 'activation', out=rho[:], in_=lre[:], func=AF.Exp, reads=['q_lre'], writes=['s5rho'])
                    p.i('dve', 'tensor_tensor', out=lim[:], in0=lim[:], in1=ldt[:], op=ALU.mult, reads=['q_lim', 'q_ldt'], writes=['q_lim'])
                    ang = p.sb([128, 16, 128], F32, 'q_ang'); tf = p.sb([128, 16, 128], F32, 'q_tf'); ti_ = p.sb([128, 16, 128], I32, 'q_ti')
                    p.i('dve', 'tensor_tensor', out=ang[:], in0=lim[:].unsqueeze(2).to_broadcast([128, 16, 128]),
                        in1=sv[:].unsqueeze(1).to_broadcast([128, 16, 128]), op=ALU.mult, reads=['q_lim', 's5sv'], writes=['q_ang'])
                    sincos(ang[:], None, sinT[:], cosT[:], tf[:], ti_[:], 'q_ang', ('q_tf', 'q_ti', 's5sin', 's5cos'), mpi)
                with p.phase():
                    R = lambda nm: p.sb([128, 1024], F32, nm)
                    rl, ri, rd_ = R('r_lre'), R('r_lim'), R('r_ldt')
                    p.dma('sp', rl[:], s5['s5_lre_r'][l, d].partition_broadcast(128), writes=['r_lre'])
                    p.dma('sp', ri[:], s5['s5_lim_r'][l, d].partition_broadcast(128), writes=['r_lim'])
                    p.dma('sp', rd_[:], s5['s5_ldt_r'][l, d].partition_broadcast(128), writes=['r_ldt'])
                    p.i('act', 'activation', out=rd_[:], in_=rd_[:], func=AF.Exp, reads=['r_ldt'], writes=['r_ldt'])
                    mag, th = R('r_mag'), R('r_th')
                    p.i('dve', 'tensor_tensor', out=mag[:], in0=rl[:], in1=rd_[:], op=ALU.mult, reads=['r_lre', 'r_ldt'], writes=['r_mag'])
                    p.i('act', 'activation', out=mag[:], in_=mag[:], func=AF.Exp, reads=['r_mag'], writes=['r_mag'])
                    p.i('dve', 'tensor_tensor', out=th[:], in0=ri[:], in1=rd_[:], op=ALU.mult, reads=['r_lim', 'r_ldt'], writes=['r_th'])
                    sn, cs = R('r_sn'), R('r_cs'); tf2 = R('r_tf'); ti2 = p.sb([128, 1024], I32, 'r_ti')
                    sincos(th[:], None, sn[:], cs[:], tf2[:], ti2[:], 'r_th', ('r_tf', 'r_ti', 'r_sn', 'r_cs'), mpi)
                    p.i('dve', 'tensor_tensor', out=cs[:], in0=cs[:], in1=mag[:], op=ALU.mult, reads=['r_cs', 'r_mag'], writes=['r_cs'])
                    p.i('dve', 'tensor_scalar', out=cs[:], in0=cs[:], scalar1=-1.0, scalar2=None, op0=ALU.add, reads=['r_cs'], writes=['r_cs'])
                    p.i('dve', 'tensor_tensor', out=sn[:], in0=sn[:], in1=mag[:], op=ALU.mult, reads=['r_sn', 'r_mag'], writes=['r_sn'])
                    p.i('dve', 'tensor_tensor', out=mag[:], in0=rl[:], in1=rl[:], op=ALU.mult, reads=['r_lre'], writes=['r_mag'])
                    p.i('dve', 'tensor_tensor', out=th[:], in0=ri[:], in1=ri[:], op=ALU.mult, reads=['r_lim'], writes=['r_th'])
                    p.i('dve', 'tensor_tensor', out=mag[:], in0=mag[:], in1=th[:], op=ALU.add, reads=['r_mag', 'r_th'], writes=['r_mag'])
                    p.i('dve', 'reciprocal', out=mag[:], in_=mag[:], reads=['r_mag'], writes=['r_mag'])
                    p.i('dve', 'tensor_tensor', out=th[:], in0=cs[:], in1=rl[:], op=ALU.mult, reads=['r_cs', 'r_lre'], writes=['r_th'])
                    p.i('dve', 'tensor_tensor', out=tf2[:], in0=sn[:], in1=ri[:], op=ALU.mult, reads=['r_sn', 'r_lim'], writes=['r_tf'])
                    p.i('dve', 'tensor_tensor', out=th[:], in0=th[:], in1=tf2[:], op=ALU.add, reads=['r_th', 'r_tf'], writes=['r_th'])
                    p.i('dve', 'tensor_tensor', out=th[:], in0=th[:], in1=mag[:], op=ALU.mult, reads=['r_th', 'r_mag'], writes=['r_th'])
                    p.i('dve', 'tensor_tensor', out=tf2[:], in0=sn[:], in1=rl[:], op=ALU.mult, reads=['r_sn', 'r_lre'], writes=['r_tf'])
                    p.i('dve', 'tensor_tensor', out=rd_[:], in0=cs[:], in1=ri[:], op=ALU.mult, reads=['r_cs', 'r_lim'], writes=['r_ldt'])
                    p.i('dve', 'tensor_tensor', out=tf2[:], in0=tf2[:], in1=rd_[:], op=ALU.subtract, reads=['r_tf', 'r_ldt'], writes=['r_tf'])
                    p.i('dve', 'tensor_tensor', out=tf2[:], in0=tf2[:], in1=mag[:], op=ALU.mult, reads=['r_tf', 'r_mag'], writes=['r_tf'])
                    p.dma('sp', rl[:], s5['s5_Br_emb'][l, d].rearrange("p g q -> p (g q)"), writes=['r_lre'])
                    p.dma('sp', ri[:], s5['s5_Bi_emb'][l, d].rearrange("p g q -> p (g q)"), writes=['r_lim'])
                    p.i('dve', 'tensor_tensor', out=cs[:], in0=th[:], in1=rl[:], op=ALU.mult, reads=['r_th', 'r_lre'], writes=['r_cs'])
                    p.i('dve', 'tensor_tensor', out=mag[:], in0=tf2[:], in1=ri[:], op=ALU.mult, reads=['r_tf', 'r_lim'], writes=['r_mag'])
                    p.i('dve', 'tensor_tensor', out=cs[:], in0=cs[:], in1=mag[:], op=ALU.subtract, reads=['r_cs', 'r_mag'], writes=['r_cs'])
                    p.i('dve', 'tensor_tensor', out=sn[:], in0=th[:], in1=ri[:], op=ALU.mult, reads=['r_th', 'r_lim'], writes=['r_sn'])
                    p.i('dve', 'tensor_tensor', out=mag[:], in0=tf2[:], in1=rl[:], op=ALU.mult, reads=['r_tf', 'r_lre'], writes=['r_mag'])
                    p.i('dve', 'tensor_tensor', out=sn[:], in0=sn[:], in1=mag[:], op=ALU.add, reads=['r_sn', 'r_mag'], writes=['r_sn'])
                    cs3 = cs[:].rearrange("p (g q) -> p g q", q=64); sn3 = sn[:].rearrange("p (g q) -> p g q", q=64)
                    p.i('dve', 'tensor_copy', out=Bw1[:, :, 0:64], in_=cs3, reads=['r_cs'], writes=['s5Bw1'])
                    p.i('dve', 'tensor_copy', out=Bw1[:, :, 64:128], in_=sn3, reads=['r_sn'], writes=['s5Bw1'])
                    p.i('dve', 'tensor_copy', out=Bw2[:, :, 0:64], in_=sn3, reads=['r_sn'], writes=['s5Bw2'])
                    p.i('dve', 'tensor_scalar', out=Bw2[:, :, 64:128], in0=cs3, scalar1=-1.0, scalar2=None, op0=ALU.mult, reads=['r_cs'], writes=['s5Bw2'])
                with p.phase():
                    cr = p.sb([128, 16, 16], F32, 'r_cr'); ci = p.sb([128, 16, 16], F32, 'r_ci')
                    p.dma('sp', cr[:], s5['s5_Cr2'][l, d], writes=['r_cr']); p.dma('sp', ci[:], s5['s5_Ci2'][l, d], writes=['r_ci'])
                    p.i('dve', 'tensor_copy', out=C1[0:64], in_=cr[0:64], reads=['r_cr'], writes=['s5C1'])
                    p.i('dve', 'tensor_scalar', out=C1[64:128], in0=ci[64:128], scalar1=-1.0, scalar2=None, op0=ALU.mult, reads=['r_ci'], writes=['s5C1'])
                    p.i('dve', 'tensor_scalar', out=C2[0:64], in0=ci[0:64], scalar1=-1.0, scalar2=None, op0=ALU.mult, reads=['r_ci'], writes=['s5C2'])
                    p.i('dve', 'tensor_scalar', out=C2[64:128], in0=cr[64:128], scalar1=-1.0, scalar2=None, op0=ALU.mult, reads=['r_cr'], writes=['s5C2'])
                with p.phase():
                    uTs = p.sb([128, 2, 128], BF16, 'm_uT')
                    utl = [p.sb([128, 256], BF16, 's5U%d' % i) for i in range(2)]
                    t2 = p.sb([128, 1024], BF16, 'm_t2')
                    inp_ = p.sb([128, 16, 128], F32, 'm_inp'); xt = p.sb([128, 16, 128], F32, 'm_xt')
                    P1 = p.sb([128, 16, 128], BF16, 'm_P1'); P2 = p.sb([128, 16, 128], BF16, 'm_P2')
                    carry = p.sb([128, 16], F32, 'm_carry'); pl1 = p.sb([128, 16], F32, 'm_pl1'); pl2 = p.sb([128, 16], F32, 'm_pl2')
                    Ytm = p.sb([128, 256], BF16, 'm_Ytm')
                    p.i('dve', 'memset', carry[:], 0.0, writes=['m_carry'])
                    ups = [p.ps([128, 128], F32, 'm_ups%d' % i) for i in range(2)]
                    bs = [p.ps([128, 1024], F32, 'm_bs%d' % i) for i in range(2)]
                    cps = p.ps([128, 16], F32, 'm_cps'); yps = p.ps([128, 256], F32, 'm_yps')
                    order = list(range(NT)) if d == 0 else [1, 0] + list(range(NT - 1, 1, -1))
                    inp2 = p.sb([128, 16, 128], F32, 'm_inpB'); uTs2 = p.sb([128, 2, 128], BF16, 'm_uTB')
                    inps = [inp_, inp2]; uTl = [uTs, uTs2]

                    def stage_in(tt, bf_):
                        ib = inps[bf_]; ub_ = uTl[bf_]
                        p.dma('sp', utl[bf_][:], Ptm[b, tt * 128:(tt + 1) * 128, 640:896], reads=['Ptm'], writes=[('s5U', bf_)])
                        for ct in range(2):
                            p.i('pe', 'matmul', ups[ct][:], lhsT=utl[bf_][:, ct * 128:(ct + 1) * 128], rhs=Rm[:], start=True, stop=True,
                                reads=[('s5U', bf_), rkey], writes=[('m_ups', ct)])
                            p.i('act', 'activation', out=ub_[:, ct, :], in_=ups[ct][:], func=AF.Copy, reads=[('m_ups', ct)], writes=[('m_uT%d' % bf_, ct)])
                        for gh in range(2):
                            for gl in range(8):
                                g = gh * 8 + gl
                                p.i('pe', 'matmul', bs[0][:, gl * 128:(gl + 1) * 128], lhsT=Bw1[:, g, :], rhs=ub_[:, gh, :], start=True, stop=True,
                                    reads=['s5Bw1', ('m_uT%d' % bf_, gh)], writes=[('m_bs', 0)], sig=(gl == 7))
                            for gl in range(8):
                                g = gh * 8 + gl
                                p.i('pe', 'matmul', bs[1][:, gl * 128:(gl + 1) * 128], lhsT=Bw2[:, g, :], rhs=ub_[:, gh, :], start=True, stop=True,
                                    reads=['s5Bw2', ('m_uT%d' % bf_, gh)], writes=[('m_bs', 1)], sig=(gl == 7))
                            gs = slice(gh * 8, gh * 8 + 8)
                            c3 = cosT[:, gs, :].rearrange("p g s -> p (g s)"); s3 = sinT[:, gs, :].rearrange("p g s -> p (g s)")
                            ibv = ib[:, gs, :].rearrange("p g s -> p (g s)")
                            p.i('dve', 'tensor_tensor', out=ibv, in0=bs[0][:], in1=c3, op=ALU.mult, reads=[('m_bs', 0), 's5cos'], writes=[('m_inp%d' % bf_, gh)])
                            p.i('dve', 'tensor_tensor', out=t2[:], in0=bs[1][:], in1=s3, op=ALU.mult, reads=[('m_bs', 1), 's5sin'], writes=['m_t2'])
                            p.i('pool', 'tensor_tensor', out=ibv, in0=ibv, in1=t2[:], op=ALU.add,
                                reads=['m_t2', ('m_inp%d' % bf_, gh)], writes=[('m_inp%d' % bf_, gh)])

                    stage_in(order[0], 0)
                    for k_, tt in enumerate(order):
                        bf_ = k_ % 2
                        ib = inps[bf_]
                        for g in range(16):
                            p.i('dve', 'tensor_tensor_scan', out=xt[:, g, :], data0=rho[:, g:g + 1].to_broadcast([128, 128]), data1=ib[:, g, :],
                                initial=carry[:, g:g + 1], op0=ALU.mult, op1=ALU.add,
                                reads=['s5rho', ('m_inp%d' % bf_, g // 8), 'm_carry'], writes=[('m_xt', g)])
                        p.i('dve', 'tensor_tensor', out=pl1[:], in0=xt[:, :, 127], in1=cosT[:, :, 127], op=ALU.mult, reads=['m_xt', 's5cos'], writes=['m_pl1'])
                        p.i('dve', 'tensor_tensor', out=pl2[:], in0=xt[:, :, 127], in1=sinT[:, :, 127], op=ALU.mult, reads=['m_xt', 's5sin'], writes=['m_pl2'])
                        p.i('pe', 'matmul', cps[:], lhsT=ident_f[:], rhs=pl1[:], start=True, stop=False, reads=['ident_f', 'm_pl1'], writes=['m_cps'], sig=False)
                        p.i('pe', 'matmul', cps[:], lhsT=Msw[:], rhs=pl2[:], start=False, stop=True, reads=['s5Msw', 'm_pl2'], writes=['m_cps'])
                        p.i('pool', 'tensor_tensor', out=P1[:], in0=xt[:], in1=cosT[:], op=ALU.mult, reads=['m_xt', 's5cos'], writes=['m_P1'])
                        p.i('dve', 'tensor_tensor', out=P2[:], in0=xt[:], in1=sinT[:], op=ALU.mult, reads=['m_xt', 's5sin'], writes=['m_P2'])
                        if k_ + 1 < len(order):
                            stage_in(order[k_ + 1], 1 - bf_)
                        p.i('dve', 'tensor_copy', out=carry[:], in_=cps[:], reads=['m_cps'], writes=['m_carry'])
                        for g in range(16):
                            p.i('pe', 'matmul', yps[:, g * 16:(g + 1) * 16], lhsT=P1[:, g, :], rhs=C1[:, g, :], start=True, stop=False,
                                reads=['m_P1', 's5C1'], writes=['m_yps'], sig=False)
                            p.i('pe', 'matmul', yps[:, g * 16:(g + 1) * 16], lhsT=P2[:, g, :], rhs=C2[:, g, :], start=False, stop=True,
                                reads=['m_P2', 's5C2'], writes=['m_yps'], sig=(g == 15))
                        p.i('act', 'activation', out=Ytm[:], in_=yps[:], func=AF.Copy, reads=['m_yps'], writes=['m_Ytm'])
                        for ct in range(2):
                            p.i('pe', 'matmul', ups[ct][:], lhsT=Ytm[:, ct * 128:(ct + 1) * 128], rhs=Rm[:], start=True, stop=True,
                                reads=['m_Ytm', rkey], writes=[('m_ups', ct)])
                            p.i('dve', 'tensor_tensor', out=Yacc[:, ct, tt * 128:(tt + 1) * 128], in0=ups[ct][:], in1=Yacc[:, ct, tt * 128:(tt + 1) * 128],
                                op=ALU.add, reads=[('m_ups', ct), ('s5Y', ct)], writes=[('s5Y', ct)])
            with p.phase():
                gw_f = p.sb([128, 2, 256], F32, 'g_wf'); gw = p.sb([128, 2, 256], BF16, 'g_w')
                p.dma('sp', gw_f[:], s5['s5_glu_w'][l].rearrange("(c p) n -> p c n", p=128), writes=['g_wf'])
                p.i('dve', 'tensor_copy', out=gw[:], in_=gw_f[:], reads=['g_wf'], writes=['g_w'])
                zT = p.sb([128, 2, T], BF16, 'g_z'); w1 = p.sb([128, T], F32, 'g_w1'); w2 = p.sb([128, T], F32, 'g_w2')
                for ct in range(2):
                    y = Yacc[:, ct, :]
                    p.i('dve', 'tensor_tensor', out=w1[:], in0=y, in1=y, op=ALU.mult, reads=[('s5Y', ct)], writes=['g_w1'])
                    p.i('dve', 'tensor_scalar', out=w1[:], in0=w1[:], scalar1=0.044715, scalar2=1.0, op0=ALU.mult, op1=ALU.add, reads=['g_w1'], writes=['g_w1'])
                    p.i('dve', 'tensor_tensor', out=w1[:], in0=w1[:], in1=y, op=ALU.mult, reads=['g_w1', ('s5Y', ct)], writes=['g_w1'])
                    p.i('act', 'activation', out=w2[:], in_=w1[:], func=AF.Sigmoid, scale=1.5957691216057308, reads=['g_w1'], writes=['g_w2'])
                    p.i('dve', 'tensor_tensor', out=zT[:, ct, :], in0=w2[:], in1=y, op=ALU.mult, reads=['g_w2', ('s5Y', ct)], writes=[('g_z', ct)])
                gps = [p.ps([128, 512], F32, 'g_ps%d' % i) for i in range(2)]
                sg_ = [p.sb([128, 512], F32, 'g_sg%d' % i) for i in range(2)]
                k = 0
                for oc in range(2):
                    for (t0, n) in BLKS:
                        i = k % 2; k += 1
                        for kc in range(2):
                            p.i('pe', 'matmul', gps[i][:, 0:n], lhsT=gw[:, kc, oc * 128:(oc + 1) * 128], rhs=zT[:, kc, t0:t0 + n], start=(kc == 0), stop=(kc == 1),
                                reads=['g_w', 'g_z'], writes=[('g_ps', i)], sig=(kc == 1))
                        p.i('act', 'activation', out=sg_[i][:, 0:n], in_=gps[i][:, 0:n], func=AF.Sigmoid, bias=gb[:, oc:oc + 1],
                            reads=[('g_ps', i), 's5gb'], writes=[('g_sg', i)])
                        p.i('dve', 'tensor_tensor', out=G['MIXT'][:, 6 + oc, t0:t0 + n], in0=sg_[i][:, 0:n], in1=zT[:, oc, t0:t0 + n], op=ALU.mult,
                            reads=[('g_sg', i), 'g_z'], writes=[('MIXT', 6 + oc)])

    def swa(l, b, need_ctx):
        with p.phase():
            qg = [p.sb([128, T], BF16, 'swq%d' % i) for i in range(2)]
            kT = p.sb([128, T], BF16, 'swk'); V = p.sb([128, NT, 128], BF16, 'swv')
            mk = p.sb([128, 384], BF16, 'swmask'); esk = p.sb([128, 4], F32, 'esk')
            p.dma('sp', qg[0][:], Pfm[b, 0:128, :], reads=['Pfm'], writes=['swq0'])
            p.dma('pool', qg[1][:], Pfm[b, 128:256, :], reads=['Pfm'], writes=['swq1'])
            p.dma('sp', kT[:], Pfm[b, 512:640, :], reads=['Pfm'], writes=['swk'])
            p.dma('pool', V[:], Ptm[b].rearrange("(n p) c -> p n c", p=128)[:, :, 0:128], reads=['Ptm'], writes=['swv'])
            p.dma('sp', mk[:], swa_mask_d[:, :], writes=['swmask'])
            p.dma('sp', esk[:], swa_sink_d[l].partition_broadcast(128), writes=['esk'])
            p.i('act', 'activation', out=esk[:], in_=esk[:], func=AF.Exp, reads=['esk'], writes=['esk'])
            O = p.ps([64, 1024], F32, 'swO'); Dn = p.ps([64, 1024], F32, 'swD')
            st = [p.ps([128, 512], F32, 'swst%d' % i) for i in range(2)]
            pt = [p.sb([128, 512], BF16, 'swpt%d' % i) for i in range(3)]
            rd = p.sb([64, 1024], F32, 'swrd')
            cnt = [0]
            for h in range(4):
                qT = qg[h % 2]; base = (h // 2) * 64; kh = h // 2
                qsets = [(LC + 1024 * qh, 1024, True, qh) for qh in range(2)]
                if need_ctx:
                    qsets.append((0, LC, False, None))
                for (tq0, nqs, lat, qh) in qsets:
                    started = [False] * ((nqs + 511) // 512)

                    def pv(ptb, pti, ktile, a, bnd):
                        c = a
                        while c < bnd:
                            e_ = min(bnd, (c // 512 + 1) * 512)
                            bk = c // 512
                            stt = not started[bk]
                            if stt:
                                assert c % 512 == 0 and e_ - c == min(512, nqs - c)
                                started[bk] = True
                            p.i('pe', 'matmul', O[:, c:e_], lhsT=V[:, ktile, kh * 64:(kh + 1) * 64], rhs=ptb[:, c - a:e_ - a],
                                start=stt, stop=False, reads=['swv', ('swpt', pti)], writes=['swO'], sig=False, skip_group_check=True)
                            p.i('pe', 'matmul', Dn[:, c:e_], lhsT=ones_b[:, 0:64], rhs=ptb[:, c - a:e_ - a],
                                start=stt, stop=False, reads=['ones_b', ('swpt', pti)], writes=['swD'], sig=True, skip_group_check=True)
                            c = e_

                    for ct in range(2):
                        for c0 in range(0, nqs, 512):
                            n = min(512, nqs - c0)
                            si = cnt[0] % 2; pi = cnt[0] % 3; cnt[0] += 1
                            p.i('pe', 'matmul', st[si][:, 0:n], lhsT=kT[base:base + 64, ct * 128:(ct + 1) * 128],
                                rhs=qT[base:base + 64, tq0 + c0:tq0 + c0 + n], start=True, stop=True,
                                reads=['swk', 'swq%d' % (h % 2)], writes=[('swst', si)])
                            p.i('act', 'activation', out=pt[pi][:, 0:n], in_=st[si][:, 0:n], func=AF.Exp, scale=0.125,
                                reads=[('swst', si)], writes=[('swpt', pi)])
                            pv(pt[pi], pi, ct, c0, c0 + n)
                    if lat:
                        for kt in range(16):
                            lo = max(kt - 1, 8 * qh); hi = min(kt + 1, 8 * qh + 7)
                            if lo > hi:
                                continue
                            n = (hi - lo + 1) * 128
                            m0 = (lo - (kt - 1)) * 128
                            qa = LC + lo * 128
                            si = cnt[0] % 2; pi = cnt[0] % 3; cnt[0] += 1
                            p.i('pe', 'matmul', st[si][:, 0:n], lhsT=kT[base:base + 64, LC + kt * 128:LC + (kt + 1) * 128],
                                rhs=qT[base:base + 64, qa:qa + n], start=True, stop=False,
                                reads=['swk', 'swq%d' % (h % 2)], writes=[('swst', si)], sig=False)
                            p.i('pe', 'matmul', st[si][:, 0:n], lhsT=ident_b[:], rhs=mk[:, m0:m0 + n], start=False, stop=True,
                                reads=['ident_b', 'swmask'], writes=[('swst', si)])
                            p.i('act', 'activation', out=pt[pi][:, 0:n], in_=st[si][:, 0:n], func=AF.Exp, scale=0.125,
                                reads=[('swst', si)], writes=[('swpt', pi)])
                            a = (lo - 8 * qh) * 128
                            pv(pt[pi], pi, 2 + kt, a, a + n)
                    p.i('dve', 'tensor_scalar', out=rd[:, 0:nqs], in0=Dn[:, 0:nqs], scalar1=esk[0:64, h:h + 1], scalar2=None, op0=ALU.add,
                        reads=['swD', 'esk'], writes=['swrd'])
                    p.i('dve', 'reciprocal', out=rd[:, 0:nqs], in_=rd[:, 0:nqs], reads=['swrd'], writes=['swrd'])
                    pb = (h % 2) * 64
                    p.i('dve', 'tensor_tensor', out=G['MIXT'][pb:pb + 64, h // 2, tq0:tq0 + nqs], in0=O[:, 0:nqs], in1=rd[:, 0:nqs], op=ALU.mult,
                        reads=['swO', 'swrd'], writes=[('MIXT', h // 2)])

    def norm_mod(l, b, gsb, gname, shift_idx):
        with p.phase():
            sq = p.sb([128, 8, 512], F32, 'sq')
            rs = p.sb([128, T], F32, 'rs')
            tmp = p.sb([128, 512], F32, 'ntmp')
            pss = [p.ps([128, 512], F32, 'nps%d' % i) for i in range(2)]
            for bi, (t0, n) in enumerate(BLKS):
                ps_ = pss[bi % 2]
                p.i('act', 'activation', out=sq[:, :, 0:n], in_=XT[:, :, t0:t0 + n], func=AF.Square, reads=['XT'], writes=['sq'])
                for c in range(8):
                    p.i('pe', 'matmul', ps_[:, 0:n], lhsT=ones_f[:], rhs=sq[:, c, 0:n], start=(c == 0), stop=(c == 7),
                        reads=['sq', 'ones_f'], writes=[('nps', bi % 2)], sig=(c == 7))
                p.i('dve', 'tensor_scalar', out=tmp[:, 0:n], in0=ps_[:, 0:n], scalar1=1.0 / D, scalar2=1e-6, op0=ALU.mult, op1=ALU.add,
                    reads=[('nps', bi % 2)], writes=['ntmp'])
                p.i('act', 'activation', out=tmp[:, 0:n], in_=tmp[:, 0:n], func=AF.Sqrt, reads=['ntmp'], writes=['ntmp'])
                p.i('dve', 'reciprocal', out=rs[:, t0:t0 + n], in_=tmp[:, 0:n], reads=['ntmp'], writes=[('rs', bi)])
            xn = [p.sb([128, T], F32, 'xn%d' % i) for i in range(2)]
            for c in range(8):
                xb = xn[c % 2]
                p.i('dve' if c % 2 == 0 else 'pool', 'tensor_tensor', out=xb[:], in0=XT[:, c, :], in1=rs[:], op=ALU.mult,
                    reads=['XT', 'rs'], writes=[('xn', c % 2)])
                for (t0, n, j) in ((0, LC, nb), (LC, NL, b)):
                    p.i('act', 'activation', out=G['HT'][:, c, t0:t0 + n], in_=xb[:, t0:t0 + n], func=AF.Identity,
                        scale=gsb[l][:, c, j:j + 1], bias=modT[l][:, shift_idx * 8 + c, j:j + 1],
                        reads=[('xn', c % 2), 'modT%d' % l, gname + str(l)], writes=[('HT', c)])

    def rstd_all(rs, blks):
        sq = p.sb([128, 8, 512], F32, 'sq')
        tmp = p.sb([128, 512], F32, 'ntmp')
        pss = [p.ps([128, 512], F32, 'nps%d' % i) for i in range(2)]
        for bi, (t0, n) in enumerate(blks):
            ps_ = pss[bi % 2]
            p.i('act', 'activation', out=sq[:, :, 0:n], in_=XT[:, :, t0:t0 + n], func=AF.Square, reads=['XT'], writes=['sq'])
            for c in range(8):
                p.i('pe', 'matmul', ps_[:, 0:n], lhsT=ones_f[:], rhs=sq[:, c, 0:n], start=(c == 0), stop=(c == 7),
                    reads=['sq', 'ones_f'], writes=[('nps', bi % 2)], sig=(c == 7))
            p.i('dve', 'tensor_scalar', out=tmp[:, 0:n], in0=ps_[:, 0:n], scalar1=1.0 / D, scalar2=1e-6, op0=ALU.mult, op1=ALU.add,
                reads=[('nps', bi % 2)], writes=['ntmp'])
            p.i('act', 'activation', out=tmp[:, 0:n], in_=tmp[:, 0:n], func=AF.Sqrt, reads=['ntmp'], writes=['ntmp'])
            p.i('dve', 'reciprocal', out=rs[:, t0:t0 + n], in_=tmp[:, 0:n], reads=['ntmp'], writes=[('rs', bi)])

    def wout(l, b, blks):
        with p.phase():
            Wo = p.sb([128, 8, D], BF16, 'Wo')
            p.dma('sp', Wo[:], wout_bf[l].rearrange("(c p) n -> p c n", p=128), reads=['wout_bf'], writes=['Wo'])
            ops_ = [p.ps([128, 512], F32, 'wops%d' % i) for i in range(2)]
            k = 0
            for (t0, n) in blks:
                j = b if t0 >= LC else nb
                for dc in range(8):
                    i = k % 2; k += 1
                    for kc in range(8):
                        p.i('pe', 'matmul', ops_[i][:, 0:n], lhsT=Wo[:, kc, dc * 128:(dc + 1) * 128], rhs=G['MIXT'][:, kc, t0:t0 + n],
                            start=(kc == 0), stop=(kc == 7), reads=['Wo', 'MIXT'], writes=[('wops', i)], sig=(kc == 7))
                    p.i('dve', 'scalar_tensor_tensor', out=XT[:, dc, t0:t0 + n], in0=ops_[i][:, 0:n], scalar=modT[l][:, 16 + dc, j:j + 1],
                        in1=XT[:, dc, t0:t0 + n], op0=ALU.mult, op1=ALU.add,
                        reads=[('wops', i), 'modT%d' % l, ('XT', dc)], writes=[('XT', dc)])

    def norm2_router(l, b, LG):
        with p.phase():
            rs = p.sb([128, T], F32, 'rs')
            rstd_all(rs, BLKS)
            Wr = p.sb([128, 8, 36], F32, 'Wr'); rb = p.sb([128, 36], F32, 'rb')
            p.dma('sp', Wr[:], moe_rw[l].rearrange("(c p) n -> p c n", p=128), writes=['Wr'])
            p.dma('sp', rb[:], moe_rb[l].partition_broadcast(128), writes=['rb'])
            xn = [p.sb([128, 8, 512], F32, 'xn%d' % i) for i in range(2)]
            lps = [p.ps([128, 512], F32, 'lgps%d' % i) for i in range(2)]
            for bi, (t0, n) in enumerate(BLKS):
                xb = xn[bi % 2]; j = b if t0 >= LC else nb
                p.i('dve', 'tensor_tensor', out=xb[:, :, 0:n], in0=XT[:, :, t0:t0 + n], in1=rs[:, t0:t0 + n].unsqueeze(1).to_broadcast([128, 8, n]),
                    op=ALU.mult, reads=['XT', 'rs'], writes=[('xn', bi % 2)])
                for c in range(8):
                    p.i('act', 'activation', out=xb[:, c, 0:n], in_=xb[:, c, 0:n], func=AF.Identity,
                        scale=g2[l][:, c, j:j + 1], bias=modT[l][:, 24 + c, j:j + 1],
                        reads=[('xn', bi % 2), 'modT%d' % l, 'g2_%d' % l], writes=[('xn', bi % 2)])
                p.i('pool', 'tensor_copy', out=G['HT'][:, :, t0:t0 + n], in_=xb[:, :, 0:n], reads=[('xn', bi % 2)], writes=['HT'])
                lp = lps[bi % 2]
                nt_ = n // 128
                for tl in range(nt_):
                    for c in range(8):
                        p.i('pe', 'matmul', lp[:, tl * 36:(tl + 1) * 36], lhsT=xb[:, c, tl * 128:(tl + 1) * 128], rhs=Wr[:, c, :],
                            start=(c == 0), stop=(c == 7), reads=[('xn', bi % 2), 'Wr'], writes=[('lgps', bi % 2)], sig=(c == 7))
                a0 = t0 // 128
                p.i('dve', 'tensor_tensor', out=LG[:, a0:a0 + nt_, :], in0=lp[:, 0:nt_ * 36].rearrange("p (a e) -> p a e", e=36),
                    in1=rb[:].unsqueeze(1).to_broadcast([128, nt_, 36]), op=ALU.add, reads=[('lgps', bi % 2), 'rb'], writes=['LG'])

    def router_math(LG, combT):
        BIGR = 1.0e4
        with p.phase():
            A_ = lambda shp, nm: p.sb(shp, F32, nm)
            gmax = A_([128, NT], 'rm_gmax'); oh = A_([128, NT, 4], 'rm_oh'); eg = A_([128, NT, 4], 'rm_eg'); gs_ = A_([128, NT], 'rm_gs')
            msk_ = A_([128, NT, 4, 8], 'rm_msk'); m8 = A_([128, NT, 8], 'rm_m8'); k1 = A_([128, NT, 32], 'rm_k1'); k2 = A_([128, NT, 32], 'rm_k2')
            m1 = A_([128, NT], 'rm_m1'); m2 = A_([128, NT], 'rm_m2'); w1 = A_([128, NT], 'rm_w1'); w2 = A_([128, NT], 'rm_w2')
            comb = A_([128, NT, 32], 'rm_comb'); ms2 = A_([128, NT, 32], 'rm_ms2')
            gl = LG[:, :, 0:4]
            p.i('dve', 'tensor_reduce', out=gmax[:], in_=gl, axis=AX.X, op=ALU.max, reads=['LG'], writes=['rm_gmax'])
            gmb = gmax[:].unsqueeze(2).to_broadcast([128, NT, 4])
            p.i('dve', 'tensor_tensor', out=oh[:], in0=gl, in1=gmb, op=ALU.is_equal, reads=['LG', 'rm_gmax'], writes=['rm_oh'])
            p.i('dve', 'tensor_tensor', out=eg[:], in0=gl, in1=gmb, op=ALU.subtract, reads=['LG', 'rm_gmax'], writes=['rm_eg'])
            p.i('act', 'activation', out=eg[:], in_=eg[:], func=AF.Exp, reads=['rm_eg'], writes=['rm_eg'])
            p.i('dve', 'tensor_reduce', out=gs_[:], in_=eg[:], axis=AX.X, op=ALU.add, reads=['rm_eg'], writes=['rm_gs'])
            p.i('dve', 'reciprocal', out=gs_[:], in_=gs_[:], reads=['rm_gs'], writes=['rm_gs'])
            p.i('dve', 'tensor_scalar', out=oh[:], in0=oh[:], scalar1=-1.0, scalar2=BIGR, op0=ALU.add, op1=ALU.mult, reads=['rm_oh'], writes=['rm_oh'])
            el = LG[:, :, 4:36].rearrange("p a (g e) -> p a g e", e=8)
            p.i('dve', 'tensor_tensor', out=msk_[:], in0=el, in1=oh[:].unsqueeze(3).to_broadcast([128, NT, 4, 8]), op=ALU.add,
                reads=['LG', 'rm_oh'], writes=['rm_msk'])
            mf = msk_[:].rearrange("p a g e -> p a (g e)")
            p.i('dve', 'tensor_reduce', out=m1[:], in_=mf, axis=AX.X, op=ALU.max, reads=['rm_msk'], writes=['rm_m1'])
            p.i('dve', 'tensor_tensor', out=k1[:], in0=mf, in1=m1[:].unsqueeze(2).to_broadcast([128, NT, 32]), op=ALU.is_equal,
                reads=['rm_msk', 'rm_m1'], writes=['rm_k1'])
            p.i('dve', 'scalar_tensor_tensor', out=ms2[:], in0=k1[:], scalar=-BIGR, in1=mf, op0=ALU.mult, op1=ALU.add,
                reads=['rm_k1', 'rm_msk'], writes=['rm_ms2'])
            p.i('dve', 'tensor_reduce', out=m2[:], in_=ms2[:], axis=AX.X, op=ALU.max, reads=['rm_ms2'], writes=['rm_m2'])
            p.i('dve', 'tensor_tensor', out=k2[:], in0=ms2[:], in1=m2[:].unsqueeze(2).to_broadcast([128, NT, 32]), op=ALU.is_equal,
                reads=['rm_ms2', 'rm_m2'], writes=['rm_k2'])
            p.i('dve', 'tensor_tensor', out=w1[:], in0=m2[:], in1=m1[:], op=ALU.subtract, reads=['rm_m1', 'rm_m2'], writes=['rm_w1'])
            p.i('act', 'activation', out=w1[:], in_=w1[:], func=AF.Exp, reads=['rm_w1'], writes=['rm_w1'])
            p.i('dve', 'tensor_scalar', out=w1[:], in0=w1[:], scalar1=1.0, scalar2=None, op0=ALU.add, reads=['rm_w1'], writes=['rm_w1'])
            p.i('dve', 'reciprocal', out=w1[:], in_=w1[:], reads=['rm_w1'], writes=['rm_w1'])
            p.i('dve', 'tensor_tensor', out=w1[:], in0=w1[:], in1=gs_[:], op=ALU.mult, reads=['rm_w1', 'rm_gs'], writes=['rm_w1'])
            p.i('dve', 'tensor_tensor', out=w2[:], in0=gs_[:], in1=w1[:], op=ALU.subtract, reads=['rm_w1', 'rm_gs'], writes=['rm_w2'])
            p.i('dve', 'tensor_tensor', out=k1[:], in0=k1[:], in1=w1[:].unsqueeze(2).to_broadcast([128, NT, 32]), op=ALU.mult,
                reads=['rm_k1', 'rm_w1'], writes=['rm_k1'])
            p.i('dve', 'tensor_tensor', out=k2[:], in0=k2[:], in1=w2[:].unsqueeze(2).to_broadcast([128, NT, 32]), op=ALU.mult,
                reads=['rm_k2', 'rm_w2'], writes=['rm_k2'])
            p.i('dve', 'tensor_tensor', out=comb[:], in0=k1[:], in1=k2[:], op=ALU.add, reads=['rm_k1', 'rm_k2'], writes=['rm_comb'])
            if 'comb' in dbg and 'comb' not in dbg_o:
                d_ = dout('comb', [128, NT, 32])
                p.dma('sp', d_[:, :, :], comb[:], reads=['rm_comb'], writes=['dbg_comb'])
            ctp = [p.ps([128, 128], F32, 'rm_ctp%d' % i) for i in range(2)]
            for tt in range(NT):
                i = tt % 2
                p.i('pe', 'matmul', ctp[i][0:32, :], lhsT=comb[:, tt, :], rhs=ident_f[:], start=True, stop=True,
                    reads=['rm_comb', 'ident_f'], writes=[('rm_ctp', i)])
                p.i('act', 'activation', out=combT[0:32, tt * 128:(tt + 1) * 128], in_=ctp[i][0:32, :], func=AF.Copy,
                    reads=[('rm_ctp', i)], writes=['combT'])

    def moe_ffn(l, b, blks, combT):
        with p.phase():
            Esel = p.sb([128, 32, 128], BF16, 'Esel')
            p.dma('sp', Esel[0:32], esel_d[:, :, :], writes=['Esel'])
            Wg = [p.sb([128, 8, 512], BF16, 'mWg%d' % i) for i in range(2)]
            Wu = [p.sb([128, 8, 512], BF16, 'mWu%d' % i) for i in range(2)]
            Wd = [p.sb([128, 4, D], BF16, 'mWd%d' % i) for i in range(2)]
            hid = p.sb([128, 4, 512], BF16, 'mhid')
            sg = [p.sb([128, 512], BF16, 'msg%d' % i) for i in range(2)]
            tu = [p.sb([128, 512], BF16, 'mtu%d' % i) for i in range(2)]
            cwb = p.sb([128, 512], F32, 'mcwb')
            gps = [p.ps([128, 512], F32, 'mgps%d' % i) for i in range(2)]
            ups = [p.ps([128, 512], F32, 'mups%d' % i) for i in range(2)]
            cwp = p.ps([128, 512], F32, 'mcwp')
            yps = [p.ps([128, 512], F32, 'myps%d' % i) for i in range(2)]
            ky = 0
            for e in range(32):
                wi = e % 2
                p.dma('sp', Wg[wi][:], mg_bf[l, e].rearrange("(c p) n -> p c n", p=128), reads=['mg_bf'], writes=[('mWg', wi)])
                p.dma('pool', Wu[wi][:], mu_bf[l, e].rearrange("(c p) n -> p c n", p=128), reads=['mu_bf'], writes=[('mWu', wi)])
                p.dma('sp', Wd[wi][:], md_bf[l, e].rearrange("(c p) n -> p c n", p=128), reads=['md_bf'], writes=[('mWd', wi)])
                for (t0, n) in blks:
                    j = b if t0 >= LC else nb
                    p.i('pe', 'matmul', cwp[:, 0:n], lhsT=Esel[0:32, e, :], rhs=combT[0:32, t0:t0 + n], start=True, stop=True,
                        reads=['Esel', 'combT'], writes=['mcwp'])
                    p.i('act', 'activation', out=cwb[:, 0:n], in_=cwp[:, 0:n], func=AF.Copy, reads=['mcwp'], writes=['mcwb'])
                    for hc in range(4):
                        i = hc % 2
                        for kc in range(8):
                            p.i('pe', 'matmul', gps[i][:, 0:n], lhsT=Wg[wi][:, kc, hc * 128:(hc + 1) * 128], rhs=G['HT'][:, kc, t0:t0 + n],
                                start=(kc == 0), stop=(kc == 7), reads=[('mWg', wi), 'HT'], writes=[('mgps', i)], sig=(kc == 7))
                        for kc in range(8):
                            p.i('pe', 'matmul', ups[i][:, 0:n], lhsT=Wu[wi][:, kc, hc * 128:(hc + 1) * 128], rhs=G['HT'][:, kc, t0:t0 + n],
                                start=(kc == 0), stop=(kc == 7), reads=[('mWu', wi), 'HT'], writes=[('mups', i)], sig=(kc == 7))
                        p.i('act', 'activation', out=sg[i][:, 0:n], in_=gps[i][:, 0:n], func=AF.Silu, reads=[('mgps', i)], writes=[('msg', i)])
                        p.i('dve', 'tensor_tensor', out=tu[i][:, 0:n], in0=ups[i][:, 0:n], in1=cwb[:, 0:n], op=ALU.mult,
                            reads=[('mups', i), 'mcwb'], writes=[('mtu', i)])
                        p.i('pool', 'tensor_tensor', out=hid[:, hc, 0:n], in0=sg[i][:, 0:n], in1=tu[i][:, 0:n], op=ALU.mult,
                            reads=[('msg', i), ('mtu', i)], writes=[('mhid', hc)])
                    for dc in range(8):
                        i = ky % 2; ky += 1
                        for hc in range(4):
                            p.i('pe', 'matmul', yps[i][:, 0:n], lhsT=Wd[wi][:, hc, dc * 128:(dc + 1) * 128], rhs=hid[:, hc, 0:n],
                                start=(hc == 0), stop=(hc == 3), reads=[('mWd', wi), ('mhid', hc)], writes=[('myps', i)], sig=(hc == 3))
                        p.i('dve', 'scalar_tensor_tensor', out=XT[:, dc, t0:t0 + n], in0=yps[i][:, 0:n], scalar=modT[l][:, 40 + dc, j:j + 1],
                            in1=XT[:, dc, t0:t0 + n], op0=ALU.mult, op1=ALU.add,
                            reads=[('myps', i), 'modT%d' % l, ('XT', dc)], writes=[('XT', dc)])

    def final_norm(b):
        with p.phase():
            rs = p.sb([128, T], F32, 'rs')
            rstd_all(rs, BLKS[1:])
            fw = p.sb([128, 8], F32, 'fnw'); p.dma('sp', fw[:], fnw[:, :], writes=['fnw'])
            xn = [p.sb([128, NL], F32, 'fxn%d' % i) for i in range(2)]
            for c in range(8):
                xb = xn[c % 2]
                p.i('dve' if c % 2 == 0 else 'pool', 'tensor_tensor', out=xb[:], in0=XT[:, c, LC:T], in1=rs[:, LC:T], op=ALU.mult,
                    reads=['XT', 'rs'], writes=[('fxn', c % 2)])
                p.i('act', 'activation', out=xb[:], in_=xb[:], func=AF.Identity, scale=fw[:, c:c + 1], reads=[('fxn', c % 2), 'fnw'], writes=[('fxn', c % 2)])
                p.dma('sp', outT[b, c * 128:(c + 1) * 128, :], xb[:], reads=[('fxn', c % 2)], writes=['outT'])

    def moe_prep(layers):
        with p.phase():
            stf = [p.sb([128, 4096], F32, 'mpf%d' % i) for i in range(3)]
            stb = [p.sb([128, 4096], BF16, 'mpb%d' % i) for i in range(3)]
            engs = ['dve', 'pool', 'act']
            k = 0
            def cast(src_t, dst_t, dkey, ncol):
                nonlocal k
                i = k % 3; k += 1
                fv = stf[i][:].rearrange("p (c n) -> p c n", n=ncol); bv = stb[i][:].rearrange("p (c n) -> p c n", n=ncol)
                p.dma('sp', fv, src_t.rearrange("(c p) n -> p c n", p=128), writes=[('mpf', i)])
                if engs[i] == 'act':
                    p.i('act', 'activation', out=stb[i][:], in_=stf[i][:], func=AF.Copy, reads=[('mpf', i)], writes=[('mpb', i)])
                else:
                    p.i(engs[i], 'tensor_copy', out=stb[i][:], in_=stf[i][:], reads=[('mpf', i)], writes=[('mpb', i)])
                p.dma('pool', dst_t.rearrange("(c p) n -> p c n", p=128), bv, reads=[('mpb', i)], writes=[dkey])
            for l in layers:
                for c2 in range(2):
                    cast(w_out[l, c2 * 512:(c2 + 1) * 512, :], wout_bf[l, c2 * 512:(c2 + 1) * 512, :], 'wout_bf', 1024)
                for e in range(32):
                    cast(mwg[l, e], mg_bf[l, e], 'mg_bf', 512)
                    cast(mwu[l, e], mu_bf[l, e], 'mu_bf', 512)
                    cast(mwd[l, e], md_bf[l, e], 'md_bf', 1024)

    def proj(l, b):
        with p.phase():
            W = p.sb([128, 8, 1536], BF16, 'Wp')
            rc = p.sb([128, NL], F32, 'ropeC'); rsn = p.sb([128, NL], F32, 'ropeS')
            p.dma('pool', rc[:], ropeC_d[:, :], writes=['ropeC'])
            p.dma('pool', rsn[:], ropeS_d[:, :], writes=['ropeS'])
            pps = [p.ps([128, 512], F32, 'pps%d' % i) for i in range(4)]
            ev = [p.sb([128, 512], BF16, 'pev%d' % i) for i in range(4)]
            t1 = p.sb([128, 512], F32, 'pt1'); t2 = p.sb([128, 512], F32, 'pt2')
            wv = win_bf[l].rearrange("(c p) n -> p c n", p=128)
            cnt = [0]

            def evac(pi, n, dst_ap):
                i = cnt[0] % 4; cnt[0] += 1
                if i % 2 == 0:
                    p.i('act', 'activation', out=ev[i][:, 0:n], in_=pps[pi][:, 0:n], func=AF.Copy, reads=[('pps', pi)], writes=[('pev', i)])
                else:
                    p.i('dve', 'tensor_copy', out=ev[i][:, 0:n], in_=pps[pi][:, 0:n], reads=[('pps', pi)], writes=[('pev', i)])
                p.dma('sp', dst_ap, ev[i][:, 0:n], reads=[('pev', i)], writes=['Pfm'])

            def mm(wcol, pi, t0, n):
                for c in range(8):
                    p.i('pe', 'matmul', pps[pi][:, 0:n], lhsT=W[:, c, wcol:wcol + 128], rhs=G['HT'][:, c, t0:t0 + n],
                        start=(c == 0), stop=(c == 7), reads=['Wp', 'HT'], writes=[('pps', pi)], sig=(c == 7))

            p.dma('sp', W[:], wv[:, :, 0:1536], writes=['Wp'])
            for (t0, n) in BLKS:
                lat = t0 >= LC
                for (ga, gs, go) in ((0, 2, 0), (1, 3, 1), (4, 5, 4)):
                    mm(ga * 128, 0, t0, n)
                    if lat:
                        mm(gs * 128, 1, t0, n)
                        l0 = t0 - LC
                        p.i('dve', 'tensor_tensor', out=t1[:, 0:n], in0=pps[0][:, 0:n], in1=rc[:, l0:l0 + n], op=ALU.mult,
                            reads=[('pps', 0), 'ropeC'], writes=['pt1'])
                        p.i('dve', 'tensor_tensor', out=t2[:, 0:n], in0=pps[1][:, 0:n], in1=rsn[:, l0:l0 + n], op=ALU.mult,
                            reads=[('pps', 1), 'ropeS'], writes=['pt2'])
                        i = cnt[0] % 4; cnt[0] += 1
                        p.i('pool', 'tensor_tensor', out=ev[i][:, 0:n], in0=t1[:, 0:n], in1=t2[:, 0:n], op=ALU.add,
                            reads=['pt1', 'pt2'], writes=[('pev', i)])
                        p.dma('sp', Pfm[b, go * 128:(go + 1) * 128, t0:t0 + n], ev[i][:, 0:n], reads=[('pev', i)], writes=['Pfm'])
                    else:
                        evac(0, n, Pfm[b, go * 128:(go + 1) * 128, t0:t0 + n])
                for gi, g in enumerate(range(6, 12)):
                    pi = 2 + gi % 2
                    mm(g * 128, pi, t0, n)
                    evac(pi, n, Pfm[b, g * 128:(g + 1) * 128, t0:t0 + n])
            p.dma('sp', W[:, :, 0:1024], wv[:, :, 1536:2560], writes=['Wp'])
            for (t0, n) in BLKS:
                for gi, g in enumerate(range(12, 20)):
                    pi = gi % 4
                    mm(gi * 128, pi, t0, n)
                    evac(pi, n, Pfm[b, g * 128:(g + 1) * 128, t0:t0 + n])
            p.dma('sp', W[:, :, 0:NTM], wv[:, :, 2560:2560 + NTM], writes=['Wp'])
            evt = [p.sb([128, NTM], BF16, 'pevt%d' % i) for i in range(2)]
            for tt in range(NT):
                for (c0, cn, pi) in ((0, 512, 0), (512, 384, 1)):
                    for c in range(8):
                        p.i('pe', 'matmul', pps[pi][:, 0:cn], lhsT=G['HT'][:, c, tt * 128:(tt + 1) * 128], rhs=W[:, c, c0:c0 + cn],
                            start=(c == 0), stop=(c == 7), reads=['Wp', 'HT'], writes=[('pps', pi)], sig=(c == 7))
                i = tt % 2
                p.i('act', 'activation', out=evt[i][:, 0:512], in_=pps[0][:, 0:512], func=AF.Copy, reads=[('pps', 0)], writes=[('pevt', i)])
                p.i('dve', 'tensor_copy', out=evt[i][:, 512:896], in_=pps[1][:, 0:384], reads=[('pps', 1)], writes=[('pevt', i)])
                p.dma('sp', Ptm[b, tt * 128:(tt + 1) * 128, :], evt[i][:], reads=[('pevt', i)], writes=['Ptm'])

    layers = list(range(nlayers))
    if 'moe' in stages or 'wout' in stages:
        moe_prep(layers)
    for b in range(nb):
        p.dma('sp', XT[:], xT_d[b].rearrange("(c p) t -> p c t", p=128), writes=['XT'])
        for l in layers:
            last = (l == 1)
            blks = BLKS[1:] if last else BLKS
            with p.phase():
                G['HT'] = p.sb([128, 8, T], BF16, 'HT')
                if 'norm1' in stages:
                    norm_mod(l, b, g1, 'g1_', 0)
                if 'proj' in stages:
                    proj(l, b)
                if 'HT' in dbg and 'HT' not in dbg_o:
                    d_ = dout('HT', [128, 8, T], BF16)
                    p.dma('sp', d_[:, :, :], G['HT'][:], reads=['HT'], writes=['dbg_HT'])
            with p.phase():
                G['MIXT'] = p.sb([128, 8, T], BF16, 'MIXT')
                if 'swa' in stages:
                    swa(l, b, not last)
                if 'na' in stages:
                    na_prep(l)
                    na(l, b, not last)
                if 'hgrn' in stages:
                    hgrn(l, b)
                if 's5' in stages:
                    s5mix(l, b)
                if 'MIXT' in dbg and 'MIXT' not in dbg_o:
                    d_ = dout('MIXT', [128, 8, T], BF16)
                    p.dma('sp', d_[:, :, :], G['MIXT'][:], reads=['MIXT'], writes=['dbg_MIXT'])
                if 'wout' in stages:
                    wout(l, b, blks)
            if 'x1' in dbg and 'x1' not in dbg_o:
                d_ = dout('x1', [128, 8, T])
                p.dma('sp', d_[:, :, :], XT[:], reads=['XT'], writes=['dbg_x1'])
            if 'moe' in stages:
                with p.phase():
                    G['HT'] = p.sb([128, 8, T], BF16, 'HT')
                    LG = p.sb([128, NT, 36], F32, 'LG'); combT = p.sb([128, T], BF16, 'combT')
                    norm2_router(l, b, LG)
                    if 'LG' in dbg and 'LG' not in dbg_o:
                        d_ = dout('LG', [128, NT, 36])
                        p.dma('sp', d_[:, :, :], LG[:], reads=['LG'], writes=['dbg_LG'])
                    router_math(LG, combT)
                    if 'nomoeffn' not in stages:
                        moe_ffn(l, b, blks, combT)
            if 'x2' in dbg and ('x2_%d' % l) not in dbg_o and b == 0:
                d_ = dout('x2_%d' % l, [128, 8, T])
                p.dma('sp', d_[:, :, :], XT[:], reads=['XT'], writes=['dbg_x2_%d' % l])
        if 'final' in stages:
            final_norm(b)
    if 'P' in dbg:
        d1 = dout('Pfm', [NFM * 128, T], BF16); d2 = dout('Ptm', [T, NTM], BF16)
        p.dma('sp', d1[:, :], Pfm[nb - 1], reads=['Pfm'], writes=['dbg_Pfm'])
        p.dma('sp', d2[:, :], Ptm[nb - 1], reads=['Ptm'], writes=['dbg_Ptm'])
    nc = p.finish(['dbg_' + k for k in dbg_o] + (['outT'] if 'final' in stages else []))
    print('ninst', p.ninst, 'nwait', p.nwait, 'cnt', p.cnt, 'duse', p.duse)
    return nc


ALL_STAGES = ('mod', 'norm1', 'proj', 'swa', 'na', 'hgrn', 's5', 'wout', 'moe', 'final')


def kernel(**inputs):
    inputs = {k: np.asarray(v) for k, v in inputs.items()}
    shared = host_shared(inputs)
    nc = build(nb=NB, stages=ALL_STAGES, dbg=(), nlayers=2)
    in_maps = []
    for core in range(8):
        m = host_prep(inputs, core, nb=NB)
        m.update(shared)
        in_maps.append(m)
    res = run_bass_kernel_spmd(nc, in_maps, core_ids=list(range(8)))
    outs = [np.asarray(r['outT']) for r in res.results]
    out = np.concatenate(outs, 0).transpose(0, 2, 1)
    return np.ascontiguousarray(out).astype(np.float32)
```

```python
import numpy as np
from contextlib import ExitStack
import concourse.bass as bass
import concourse.mybir as mybir
from concourse.bass_utils import run_bass_kernel_spmd

F32 = mybir.dt.float32
BF16 = mybir.dt.bfloat16
I32 = mybir.dt.int32
U32 = mybir.dt.uint32
AF = mybir.ActivationFunctionType
ALU = mybir.AluOpType
AX = mybir.AxisListType

ENG = ('pe', 'act', 'dve', 'pool', 'sp')
NDMA = 24


class P:
    def __init__(self):
        self.nc = bass.Bass("TRN2", target_bir_lowering=False)
        self.es = ExitStack()
        nc = self.nc
        self.ops = {e: [] for e in ENG}
        self.cnt = {e: 0 for e in ENG}
        self.sem = {e: self.es.enter_context(nc.semaphore("s_" + e)) for e in ENG}
        self.dsem = [self.es.enter_context(nc.semaphore("d%d" % i)) for i in range(NDMA)]
        self.duse = [0] * NDMA
        self.dnext = 0
        self.known = {e: {} for e in ENG}
        self.res = {}
        self.ntens = 0
        self.nwait = 0

    def sb(self, shape, dt=F32, name=None):
        self.ntens += 1
        return self.es.enter_context(self.nc.sbuf_tensor((name or "t") + "_s%d" % self.ntens, list(shape), dt))

    def ps(self, shape, dt=F32, name=None):
        self.ntens += 1
        return self.es.enter_context(self.nc.psum_tensor((name or "p") + "_p%d" % self.ntens, list(shape), dt))

    def dram(self, name, shape, dt=F32, kind="Internal"):
        return self.nc.dram_tensor(name, list(shape), dt, kind=kind).ap()

    def _st(self, name):
        if name not in self.res:
            self.res[name] = {'whole': [None, []], 'subs': {}}
        return self.res[name]

    def _involved(self, key):
        if isinstance(key, tuple):
            name, idx = key
        else:
            name, idx = key, None
        st = self._st(name)
        if idx is None:
            return st, idx, [st['whole']] + list(st['subs'].values())
        if idx not in st['subs']:
            st['subs'][idx] = [None, []]
        return st, idx, [st['whole'], st['subs'][idx]]

    def _deps(self, reads, writes):
        deps = []
        for k in reads:
            _, _, inv = self._involved(k)
            for s in inv:
                if s[0] is not None:
                    deps.append(s[0])
        for k in writes:
            _, _, inv = self._involved(k)
            for s in inv:
                if s[0] is not None:
                    deps.append(s[0])
                deps.extend(s[1])
        return deps

    def _commit(self, reads, writes, tok):
        for k in reads:
            st, idx, inv = self._involved(k)
            (st['whole'] if idx is None else st['subs'][idx])[1].append(tok)
        for k in writes:
            st, idx, inv = self._involved(k)
            if idx is None:
                st['subs'] = {}
                st['whole'] = [tok, []]
            else:
                st['subs'][idx] = [tok, []]

    def _emit_waits(self, e, deps):
        kn = self.known[e]
        need = {}
        for (sk, v) in deps:
            if e == 'pe' and sk == 'pe':
                continue
            if kn.get(sk, 0) >= v:
                continue
            if need.get(sk, 0) < v:
                need[sk] = v
        for sk, v in need.items():
            kn[sk] = v
            sem = self.sem[sk] if isinstance(sk, str) else self.dsem[sk]
            self.ops[e].append(('wait', sem, v))
            self.nwait += 1

    def op(self, e, fn, reads=(), writes=(), sig=True):
        deps = self._deps(reads, writes)
        self._emit_waits(e, deps)
        if sig:
            self.cnt[e] += 1
            tok = (e, self.cnt[e])
        else:
            tok = (e, self.cnt[e] + 1)
        self.ops[e].append(('op', fn, sig))
        self._commit(reads, writes, tok)

    def i(self, e, method, *args, reads=(), writes=(), sig=True, **kwargs):
        self.op(e, lambda eng: getattr(eng, method)(*args, **kwargs), reads=reads, writes=writes, sig=sig)

    def dma(self, q, out, in_, reads=(), writes=(), **kw):
        deps = self._deps(reads, writes)
        j = self.dnext
        self.dnext = (self.dnext + 1) % NDMA
        if self.duse[j] > 0:
            deps.append((j, 16 * self.duse[j]))
        self._emit_waits(q, deps)
        self.duse[j] += 1
        tok = (j, 16 * self.duse[j])
        self.ops[q].append(('dma', out, in_, self.dsem[j], kw))
        self._commit(reads, writes, tok)

    def barrier(self):
        for e in ENG:
            deps = [(f, self.cnt[f]) for f in ENG if self.cnt[f] > 0]
            deps += [(j, 16 * self.duse[j]) for j in range(NDMA) if self.duse[j] > 0]
            self._emit_waits(e, deps)

    def phase(self):
        return _Phase(self)

    def flush(self):
        nc = self.nc
        if not any(self.ops[e] for e in ENG):
            return
        engs = {'pe': 'tensor', 'act': 'scalar', 'dve': 'vector', 'pool': 'gpsimd', 'sp': 'sync'}
        with nc.Block() as block:
            for e in ENG:
                lst = self.ops[e]
                semE = self.sem[e]

                def body(eng, lst=lst, semE=semE):
                    for it in lst:
                        if it[0] == 'wait':
                            eng.wait_ge(it[1], it[2])
                        elif it[0] == 'op':
                            inst = it[1](eng)
                            if it[2]:
                                inst.then_inc(semE, 1)
                        else:
                            eng.dma_start(out=it[1], in_=it[2], **it[4]).then_inc(it[3], 16)
                getattr(block, engs[e])(body)
        self.ninst = getattr(self, 'ninst', 0) + sum(len(self.ops[e]) for e in ENG)
        self.ops = {e: [] for e in ENG}

    def finish(self, final_keys):
        deps = self._deps(final_keys, [])
        self._emit_waits('sp', deps)
        self.flush()
        self.es.close()
        return self.nc


class _Phase:
    def __init__(self, p):
        self.p = p

    def __enter__(self):
        self.saved = self.p.es
        self.p.es = ExitStack()
        return self

    def __exit__(self, *a):
        self.p.barrier()
        self.p.flush()
        self.p.es.close()
        self.p.es = self.saved
        return False


import math

NB = 4
T = 2304
NT = 18
LC = 256
NL = 2048
D = 1024
NFM = 20
NTM = 896
BLKS = [(0, 256), (256, 512), (768, 512), (1280, 512), (1792, 512)]


def win_perm():
    A, B, C, Dd = 0, 512, 1280, 2560
    def hd(base, h): return list(range(base + 64 * h, base + 64 * h + 64))
    def sw(cols):
        c = np.array(cols)
        idx = np.concatenate([np.arange(16, 32), np.arange(0, 16), np.arange(48, 64), np.arange(32, 48)])
        return list(c[idx])
    fm = []
    fm += hd(A, 0) + hd(A, 2)
    fm += hd(A, 1) + hd(A, 3)
    fm += sw(hd(A, 0)) + sw(hd(A, 2))
    fm += sw(hd(A, 1)) + sw(hd(A, 3))
    fm += hd(A + 256, 0) + hd(A + 256, 1)
    fm += sw(hd(A + 256, 0)) + sw(hd(A + 256, 1))
    fm += list(range(B, B + 256))
    fm += list(range(B + 256, B + 512))
    fm += list(range(C, C + 256))
    fm += list(range(C + 256, C + 512))
    fm += list(range(C + 512, C + 768))
    fm += list(range(C + 1024, C + 1280))
    fm += list(range(Dd, Dd + 256))
    assert len(fm) == NFM * 128
    tm = list(range(A + 384, A + 512)) + list(range(B + 512, B + 768)) + list(range(C + 768, C + 1024)) + list(range(Dd, Dd + 256))
    assert len(tm) == NTM
    return np.array(fm + tm)


def rope_tables():
    half = 32
    inv = 1.0 / (10000.0 ** (np.arange(0, half, 2, dtype=np.float32) / half))
    t = np.arange(NL)
    row = (t // 64).astype(np.float32); col = (t % 64).astype(np.float32)
    ar = row[:, None] * inv[None, :]; ac = col[:, None] * inv[None, :]
    C = np.zeros((64, NL), np.float32); S = np.zeros((64, NL), np.float32)
    C[0:16] = np.cos(ar).T; C[16:32] = np.cos(ar).T; C[32:48] = np.cos(ac).T; C[48:64] = np.cos(ac).T
    S[0:16] = -np.sin(ar).T; S[16:32] = np.sin(ar).T; S[32:48] = -np.sin(ac).T; S[48:64] = np.sin(ac).T
    return np.concatenate([C, C], 0), np.concatenate([S, S], 0)


def host_prep(inp, core, nb=NB):
    f = np.float32
    b0 = core * nb
    m = {}
    x = inp['x'][b0:b0 + nb]; ctx = inp['ctx'][b0:b0 + nb]
    xt = np.concatenate([ctx, x], axis=1)
    m['xT'] = np.ascontiguousarray(xt.transpose(0, 2, 1)).astype(f)
    cc = np.concatenate([inp['c'][b0:b0 + nb], inp['c_ctx'][None, :]], 0)
    m['cT'] = np.ascontiguousarray(cc.T.reshape(8, 128, nb + 1).transpose(1, 0, 2)).astype(f)
    return m


def host_shared(inp):
    f = np.float32
    m = {}
    m['mod_w'] = inp['mod_w'].astype(f)
    m['mod_b2'] = np.ascontiguousarray(inp['mod_b'].reshape(2, 48, 128).transpose(0, 2, 1)).astype(f)
    for k in ('norm1_w', 'norm2_w'):
        m[k + '2'] = np.ascontiguousarray(inp[k].reshape(2, 8, 128).transpose(0, 2, 1)).astype(f)
    m['fnw2'] = np.ascontiguousarray(inp['final_norm_w'].reshape(8, 128).T).astype(f)
    m['w_in2'] = np.ascontiguousarray(inp['w_in'][:, :, win_perm()]).astype(f)
    m['w_out'] = inp['w_out'].astype(f)
    C, S = rope_tables()
    m['ropeC'] = C; m['ropeS'] = S
    import ml_dtypes
    bf = ml_dtypes.bfloat16
    i = np.arange(128)[:, None]; j = np.arange(128)[None, :]
    BIG = -240000.0
    mk = np.zeros((128, 384), f)
    mk[:, 0:128] = np.where(i <= j, 0.0, BIG)
    mk[:, 256:384] = np.where(j <= i, 0.0, BIG)
    m['swa_mask'] = mk.astype(bf)
    m['ident_bf'] = np.eye(128, dtype=f).astype(bf)
    m['ident_f'] = np.eye(128, dtype=f)
    m['swa_sink'] = inp['swa_sink'].astype(f)
    rp = np.zeros((2, 4, 15, 127), f); rp[:, :, :, 48:79] = inp['na_rpb']
    same = (i // 32) == (j // 32)
    m['hg_mask'] = np.stack([(same & (i <= j)), (same & (i >= j))]).astype(f).astype(bf)
    bo = np.zeros((128, 128), f); bo[:64, :64] = 1; bo[64:, 64:] = 1
    m['blockones'] = bo.astype(bf)
    m['hgrn_lb2'] = np.ascontiguousarray(inp['hgrn_lb'].reshape(2, 2, 2, 128).transpose(3, 0, 1, 2)).astype(f)
    G_, P_, H_ = 16, 64, 16
    def pdup(a):
        t = a.transpose(0, 3, 1, 2)
        return np.ascontiguousarray(np.concatenate([t, t], 1)).astype(f)
    m['s5_lre_p'] = pdup(inp['s5_lam_re']); m['s5_lim_p'] = pdup(inp['s5_lam_im'])
    m['s5_ldt_p'] = np.ascontiguousarray(np.broadcast_to(inp['s5_log_dt'][:, None, :, :], (2, 128, 2, 16))).astype(f)
    m['s5_lre_r'] = np.ascontiguousarray(inp['s5_lam_re'].reshape(2, 2, 1024)).astype(f)
    m['s5_lim_r'] = np.ascontiguousarray(inp['s5_lam_im'].reshape(2, 2, 1024)).astype(f)
    m['s5_ldt_r'] = np.ascontiguousarray(np.repeat(inp['s5_log_dt'], 64, axis=-1)).astype(f)
    def bemb(a):
        o = np.zeros((2, 2, 128, 16, 64), f)
        for g in range(16):
            o[:, :, (g % 8) * 16:(g % 8) * 16 + 16, g, :] = a[:, :, g].transpose(0, 1, 3, 2)
        return o
    m['s5_Br_emb'] = bemb(inp['s5_b_re']); m['s5_Bi_emb'] = bemb(inp['s5_b_im'])
    def cdup(a):
        t = a.transpose(0, 1, 4, 2, 3)
        return np.ascontiguousarray(np.concatenate([t, t], 2)).astype(f)
    m['s5_Cr2'] = cdup(inp['s5_c_re']); m['s5_Ci2'] = cdup(inp['s5_c_im'])
    m['s5_d2'] = np.ascontiguousarray(inp['s5_d'].reshape(2, 2, 128).transpose(0, 2, 1)).astype(f)
    m['s5_glu_w'] = inp['s5_glu_w'].astype(f)
    m['s5_glu_b2'] = np.ascontiguousarray(inp['s5_glu_b'].reshape(2, 2, 128).transpose(0, 2, 1)).astype(f)
    m['svec'] = np.ascontiguousarray(np.broadcast_to(np.arange(1, 129, dtype=f)[None, :], (128, 128)))
    m['Jrev128'] = np.ascontiguousarray(np.eye(128, dtype=f)[::-1]).astype(bf)
    msw = np.zeros((128, 128), f)
    for mm_ in range(64):
        msw[mm_ + 64, mm_] = -1.0
        msw[mm_, mm_ + 64] = 1.0
    m['MswT'] = msw
    es = np.zeros((32, 32, 128), f)
    for e_ in range(32):
        es[e_, e_, :] = 1.0
    m['esel'] = es.astype(bf)
    m['moe_rw'] = np.ascontiguousarray(np.concatenate([inp['moe_group_w'], inp['moe_expert_w']], -1)).astype(f)
    m['moe_rb'] = np.ascontiguousarray(np.concatenate([inp['moe_group_b'], inp['moe_expert_b']], -1)).astype(f)
    m['moe_w_gate'] = inp['moe_w_gate'].astype(f); m['moe_w_up'] = inp['moe_w_up'].astype(f); m['moe_w_down'] = inp['moe_w_down'].astype(f)
    m['hgrn_nw2'] = np.ascontiguousarray(np.concatenate([inp['hgrn_norm_w'], inp['hgrn_norm_w']], 1).T).astype(f)
    m['rpb_pad'] = rp
    m['Jrev'] = np.ascontiguousarray(np.eye(64, dtype=f)[::-1])
    kc = np.arange(64)[:, None]; qc = np.arange(64)[None, :]
    ws = np.clip(qc - 8, 0, 48)
    m['na_colmask'] = np.where((kc >= ws) & (kc < ws + 16), 0.0, BIG).astype(f)
    return m


def build(nb=NB, stages=('mod', 'norm1', 'proj'), dbg=(), nlayers=1):
    p = P(); nc = p.nc
    di = {}
    def din(name, shape, dt=F32):
        di[name] = p.dram(name, shape, dt, 'ExternalInput'); return di[name]
    xT_d = din('xT', [nb, D, T]); cT_d = din('cT', [128, 8, nb + 1])
    mod_w = din('mod_w', [2, D, 6 * D]); mod_b2 = din('mod_b2', [2, 128, 48])
    n1w = din('norm1_w2', [2, 128, 8]); n2w = din('norm2_w2', [2, 128, 8]); fnw = din('fnw2', [128, 8])
    w_in2 = din('w_in2', [2, D, NFM * 128 + NTM]); w_out = din('w_out', [2, D, D])
    ropeC_d = din('ropeC', [128, NL]); ropeS_d = din('ropeS', [128, NL])
    swa_mask_d = din('swa_mask', [128, 384], BF16); ident_bf_d = din('ident_bf', [128, 128], BF16)
    ident_f_d = din('ident_f', [128, 128]); swa_sink_d = din('swa_sink', [2, 4])
    rpb_pad_d = din('rpb_pad', [2, 4, 15, 127]); Jrev_d = din('Jrev', [64, 64]); na_cm_d = din('na_colmask', [64, 64])
    hg_mask_d = din('hg_mask', [2, 128, 128], BF16); blockones_d = din('blockones', [128, 128], BF16)
    hgrn_lb_d = din('hgrn_lb2', [128, 2, 2, 2]); hgrn_nw_d = din('hgrn_nw2', [128, 2])
    s5 = {}
    for nm, shp, dt_ in (('s5_lre_p', [2, 128, 2, 16], F32), ('s5_lim_p', [2, 128, 2, 16], F32), ('s5_ldt_p', [2, 128, 2, 16], F32),
                         ('s5_lre_r', [2, 2, 1024], F32), ('s5_lim_r', [2, 2, 1024], F32), ('s5_ldt_r', [2, 2, 1024], F32),
                         ('s5_Br_emb', [2, 2, 128, 16, 64], F32), ('s5_Bi_emb', [2, 2, 128, 16, 64], F32),
                         ('s5_Cr2', [2, 2, 128, 16, 16], F32), ('s5_Ci2', [2, 2, 128, 16, 16], F32),
                         ('s5_d2', [2, 128, 2], F32), ('s5_glu_w', [2, 256, 256], F32), ('s5_glu_b2', [2, 128, 2], F32),
                         ('svec', [128, 128], F32), ('Jrev128', [128, 128], BF16), ('MswT', [128, 128], F32)):
        s5[nm] = din(nm, shp, dt_)
    esel_d = din('esel', [32, 32, 128], BF16); moe_rw = din('moe_rw', [2, D, 36]); moe_rb = din('moe_rb', [2, 36])
    mwg = din('moe_w_gate', [2, 32, D, 512]); mwu = din('moe_w_up', [2, 32, D, 512]); mwd = din('moe_w_down', [2, 32, 512, D])
    mg_bf = p.dram('mg_bf', [2, 32, D, 512], BF16); mu_bf = p.dram('mu_bf', [2, 32, D, 512], BF16); md_bf = p.dram('md_bf', [2, 32, 512, D], BF16)
    wout_bf = p.dram('wout_bf', [2, D, D], BF16)
    outT = p.dram('outT', [nb, D, NL], F32, 'ExternalOutput')
    dbg_o = {}
    def dout(name, shape, dt=F32):
        dbg_o[name] = p.dram('dbg_' + name, shape, dt, 'ExternalOutput'); return dbg_o[name]
    win_bf = p.dram('win_bf', [2, D, NFM * 128 + NTM], BF16)
    Pfm = p.dram('Pfm', [nb, NFM * 128, T], BF16)
    Ptm = p.dram('Ptm', [nb, T, NTM], BF16)
    NJ = nb + 1

    modT = [p.sb([128, 48, NJ], F32, 'modT%d' % l) for l in range(2)]
    g1 = [p.sb([128, 8, NJ], F32, 'g1_%d' % l) for l in range(2)]
    g2 = [p.sb([128, 8, NJ], F32, 'g2_%d' % l) for l in range(2)]
    ones_f = p.sb([128, 128], F32, 'ones_f')
    p.i('dve', 'memset', ones_f[:], 1.0, writes=['ones_f'])
    ones_b = p.sb([128, 128], BF16, 'ones_b')
    p.i('dve', 'memset', ones_b[:], 1.0, writes=['ones_b'])
    ident_b = p.sb([128, 128], BF16, 'ident_b'); ident_f = p.sb([128, 128], F32, 'ident_f')
    p.dma('sp', ident_b[:], ident_bf_d[:, :], writes=['ident_b'])
    p.dma('sp', ident_f[:], ident_f_d[:, :], writes=['ident_f'])

    with p.phase():
        c_sb = p.sb([128, 8, NJ]); sc = p.sb([128, 8, NJ])
        mb = p.sb([128, 2, 48]); nw1 = p.sb([128, 2, 8]); nw2 = p.sb([128, 2, 8])
        p.dma('sp', c_sb[:], cT_d[:, :, :], writes=['c_sb'])
        p.dma('sp', mb[:], mod_b2.rearrange("l p n -> p l n"), writes=['mb'])
        p.dma('sp', nw1[:], n1w.rearrange("l p n -> p l n"), writes=['nw1'])
        p.dma('sp', nw2[:], n2w.rearrange("l p n -> p l n"), writes=['nw2'])
        p.i('act', 'activation', out=sc[:], in_=c_sb[:], func=AF.Silu, reads=['c_sb'], writes=['sc'])
        mwb = [p.sb([128, 8, 512], F32, 'mw%d' % i) for i in range(2)]
        mps = p.ps([128, 48, 8], F32, 'mps')
        for l in range(2):
            mwv = mod_w[l].rearrange("(c p) n -> p c n", p=128)
            for g in range(12):
                mw = mwb[g % 2]
                p.dma('sp' if g % 2 == 0 else 'pool', mw[:], mwv[:, :, g * 512:(g + 1) * 512], writes=[('mw', g % 2)])
                for j in range(4):
                    for kc in range(8):
                        p.i('pe', 'matmul', mps[:, g * 4 + j, 0:NJ], lhsT=mw[:, kc, j * 128:(j + 1) * 128], rhs=sc[:, kc, :],
                            start=(kc == 0), stop=(kc == 7), reads=[('mw', g % 2), 'sc'], writes=['mps'], sig=(kc == 7))
            p.i('dve', 'tensor_tensor', out=modT[l][:], in0=mps[:, :, 0:NJ],
                in1=mb[:, l, :].unsqueeze(2).to_broadcast([128, 48, NJ]), op=ALU.add,
                reads=['mps', 'mb'], writes=['modT%d' % l])
            for (gg, nw, mi, nm) in ((g1, nw1, 1, 'g1_'), (g2, nw2, 4, 'g2_')):
                p.i('dve', 'scalar_tensor_tensor', out=gg[l][:], in0=modT[l][:, mi * 8:(mi + 1) * 8, :], scalar=1.0,
                    in1=nw[:, l, :].unsqueeze(2).to_broadcast([128, 8, NJ]), op0=ALU.add, op1=ALU.mult,
                    reads=['modT%d' % l, 'nw1', 'nw2'], writes=[nm + str(l)])
        if 'mod' in dbg:
            d_ = dout('modT0', [128, 48, NJ])
            p.dma('sp', d_[:, :, :], modT[0][:], reads=['modT0'], writes=['dbg_modT0'])

    if 'proj' in stages:
        with p.phase():
            st = [p.sb([128, NFM * 128 + NTM], F32, 'wst%d' % i) for i in range(2)]
            sb_ = [p.sb([128, NFM * 128 + NTM], BF16, 'wsb%d' % i) for i in range(2)]
            k = 0
            for l in range(nlayers):
                for c in range(8):
                    i = k % 2; k += 1
                    p.dma('sp', st[i][:], w_in2[l, c * 128:(c + 1) * 128, :], writes=[('wst', i)])
                    p.i('dve' if c % 2 == 0 else 'pool', 'tensor_copy', out=sb_[i][:], in_=st[i][:],
                        reads=[('wst', i)], writes=[('wsb', i)])
                    p.dma('pool', win_bf[l, c * 128:(c + 1) * 128, :], sb_[i][:], reads=[('wsb', i)], writes=['win_bf'])

    XT = p.sb([128, 8, T], F32, 'XT')
    G = {}

    GTds = [p.sb([128, 4, 15, 64], BF16, 'GTd0')] * 2

    def na_prep(l):
        GTd = GTds[l]
        with p.phase():
            Hk = p.sb([64, 60, 64], F32, 'naH'); Jr = p.sb([64, 64], F32, 'naJ'); cm = p.sb([64, 64], F32, 'nacm')
            for hh in range(4):
                src = bass.AP(tensor=rpb_pad_d.tensor, offset=(l * 4 + hh) * 15 * 127, ap=[[1, 64], [127, 15], [1, 64]])
                p.dma('sp', Hk[:, hh * 15:(hh + 1) * 15, :], src, writes=['naH'])
            p.dma('sp', Jr[:], Jrev_d[:, :], writes=['naJ'])
            p.dma('sp', cm[:], na_cm_d[:, :], writes=['nacm'])
            tp = [p.ps([64, 8, 64], F32, 'natp%d' % i) for i in range(2)]
            k = 0
            for h in range(4):
                for d0 in (0, 8):
                    nd = min(8, 15 - d0)
                    ti = k % 2; k += 1
                    for j in range(nd):
                        dr = d0 + j
                        p.i('pe', 'matmul', tp[ti][:, j, :], lhsT=Hk[:, h * 15 + dr, :], rhs=Jr[:], start=True, stop=True,
                            reads=['naH', 'naJ'], writes=[('natp', ti)], sig=(j == nd - 1))
                    for j in range(nd):
                        dr = d0 + j
                        p.i('dve', 'scalar_tensor_tensor', out=GTd[(h % 2) * 64:(h % 2) * 64 + 64, h, 14 - dr, :], in0=tp[ti][:, j, :], scalar=8.0, in1=cm[:],
                            op0=ALU.mult, op1=ALU.add, reads=[('natp', ti), 'nacm'], writes=['GTd'])

    def na(l, b, need_ctx):
        rs_ = [min(max(qr - 4, 0), 24) for qr in range(32)]
        rng = []
        for kr in range(32):
            qs = [qr for qr in range(32) if rs_[qr] <= kr <= rs_[qr] + 7]
            assert qs == list(range(qs[0], qs[-1] + 1))
            rng.append((qs[0], qs[-1]))
        GTd = GTds[l]
        with p.phase():
            qg = [p.sb([128, T], BF16, 'naq%d' % i) for i in range(2)]
            kg = [p.sb([128, T], BF16, 'nak%d' % i) for i in range(2)]
            V = p.sb([128, 2, 256], BF16, 'nav')
            Vr = p.sb([128, 36, 256], BF16, 'navr')
            p.dma('sp', qg[0][:], Pfm[b, 768:896, :], reads=['Pfm'], writes=['naq0'])
            p.dma('pool', qg[1][:], Pfm[b, 896:1024, :], reads=['Pfm'], writes=['naq1'])
            p.dma('sp', kg[0][:], Pfm[b, 1024:1152, :], reads=['Pfm'], writes=['nak0'])
            p.dma('pool', kg[1][:], Pfm[b, 1152:1280, :], reads=['Pfm'], writes=['nak1'])
            p.dma('sp', V[:], Ptm[b, 0:256, :].rearrange("(n p) c -> p n c", p=128)[:, :, 128:384], reads=['Ptm'], writes=['nav'])
            p.dma('sp', Vr[0:64], Ptm[b].rearrange("(n p) c -> p n c", p=64)[:, :, 128:384], reads=['Ptm'], writes=['nav'])
            O = p.ps([64, 1024], F32, 'naO'); Dn = p.ps([64, 1024], F32, 'naD')
            st = [p.ps([128, 512], F32, 'nast%d' % i) for i in range(2)]
            pt = [p.sb([128, 512], BF16, 'napt%d' % i) for i in range(3)]
            rd = p.sb([64, 1024], F32, 'nard')
            cnt = [0]
            GTf = GTd[:].rearrange('p h d c -> p h (d c)')
            for h in range(4):
                qT = qg[h // 2]; kT = kg[h // 2]; base = (h % 2) * 64
                qsets = [(LC + 1024 * qh, 1024, True, qh) for qh in range(2)]
                if need_ctx:
                    qsets.append((0, LC, False, None))
                for (tq0, nqs, lat, qh) in qsets:
                    started = [False] * ((nqs + 511) // 512)

                    def pv(ptb, pti, vl, a, bnd, pb):
                        c = a
                        while c < bnd:
                            e_ = min(bnd, (c // 512 + 1) * 512)
                            bk = c // 512
                            stt = not started[bk]
                            if stt:
                                assert c % 512 == 0 and e_ - c == min(512, nqs - c)
                                started[bk] = True
                            K = vl.shape[0]
                            p.i('pe', 'matmul', O[:, c:e_], lhsT=vl, rhs=ptb[pb:pb + K, c - a:e_ - a],
                                start=stt, stop=False, reads=['nav', ('napt', pti)], writes=['naO'], sig=False, skip_group_check=True)
                            p.i('pe', 'matmul', Dn[:, c:e_], lhsT=ones_b[pb:pb + K, 0:64], rhs=ptb[pb:pb + K, c - a:e_ - a],
                                start=stt, stop=False, reads=['ones_b', ('napt', pti)], writes=['naD'], sig=True, skip_group_check=True)
                            c = e_

                    items = []
                    for ct in range(2):
                        for c0 in range(0, nqs, 512):
                            n = min(512, nqs - c0)
                            si = cnt[0] % 2; pi = cnt[0] % 3; cnt[0] += 1

                            def A_(si=si, pi=pi, ct=ct, c0=c0, n=n):
                                p.i('pe', 'matmul', st[si][:, 0:n], lhsT=kT[base:base + 64, ct * 128:(ct + 1) * 128],
                                    rhs=qT[base:base + 64, tq0 + c0:tq0 + c0 + n], start=True, stop=True,
                                    reads=['nak%d' % (h // 2), 'naq%d' % (h // 2)], writes=[('nast', si)])
                                p.i('act', 'activation', out=pt[pi][:, 0:n], in_=st[si][:, 0:n], func=AF.Exp, scale=0.125,
                                    reads=[('nast', si)], writes=[('napt', pi)])

                            def B_(pi=pi, ct=ct, c0=c0, n=n):
                                pv(pt[pi], pi, V[:, ct, h * 64:(h + 1) * 64], c0, c0 + n, 0)
                            items.append((A_, B_))
                    import os
                    NADBG = os.environ.get('NADBG', '')
                    if lat and 'nolat' not in NADBG:
                        for kr in range(0, 32, 2 if 'even' in NADBG else 1):
                            qlo, qhi = rng[kr]
                            for (ra, rb) in ((16 * qh, 16 * qh + 7), (16 * qh + 8, 16 * qh + 15)):
                                r0 = max(qlo, ra); r1 = min(qhi, rb)
                                if r0 > r1:
                                    continue
                                n = (r1 - r0 + 1) * 64
                                pb = 0
                                ktok = LC + kr * 64
                                qa = LC + r0 * 64
                                d0 = r0 - kr + 7
                                si = cnt[0] % 2; pi = cnt[0] % 3; cnt[0] += 1
                                a = (r0 - 16 * qh) * 64
                                nr_ = r1 - r0 + 1

                                def A_(si=si, pi=pi, n=n, pb=pb, ktok=ktok, qa=qa, d0=d0, nr_=nr_):
                                    p.i('pe', 'matmul', st[si][pb:pb + 64, 0:n], lhsT=kT[base:base + 64, ktok:ktok + 64],
                                        rhs=qT[base:base + 64, qa:qa + n], start=True, stop=False,
                                        reads=['nak%d' % (h // 2), 'naq%d' % (h // 2)], writes=[('nast', si)], sig=False)
                                    p.i('pe', 'matmul', st[si][pb:pb + 64, 0:n], lhsT=ident_b[base:base + 64, base:base + 64],
                                        rhs=GTf[base:base + 64, h, d0 * 64:(d0 + nr_) * 64], start=False, stop=True,
                                        reads=['ident_b', 'GTd'], writes=[('nast', si)])
                                    p.i('act', 'activation', out=pt[pi][pb:pb + 64, 0:n], in_=st[si][pb:pb + 64, 0:n], func=AF.Exp, scale=0.125,
                                        reads=[('nast', si)], writes=[('napt', pi)])

                                def B_(pi=pi, kr=kr, a=a, n=n, pb=pb):
                                    pv(pt[pi], pi, Vr[0:64, 4 + kr, h * 64:(h + 1) * 64], a, a + n, pb)
                                items.append((A_, B_))
                    items[0][0]()
                    for ii in range(len(items)):
                        if ii + 1 < len(items):
                            items[ii + 1][0]()
                        items[ii][1]()
                    p.i('dve', 'reciprocal', out=rd[:, 0:nqs], in_=Dn[:, 0:nqs], reads=['naD'], writes=['nard'])
                    ob = (h % 2) * 64
                    p.i('dve', 'tensor_tensor', out=G['MIXT'][ob:ob + 64, 2 + h // 2, tq0:tq0 + nqs], in0=O[:, 0:nqs], in1=rd[:, 0:nqs], op=ALU.mult,
                        reads=['naO', 'nard'], writes=[('MIXT', 2 + h // 2)])

    def hgrn(l, b):
        NCH = T // 32
        orders = [list(range(NCH)), list(range(7, -1, -1)) + list(range(NCH - 1, 7, -1))]
        import os
        HGDBG = os.environ.get('HGDBG', '')
        PL = 'dve' if 'nopool' in HGDBG else 'pool'
        with p.phase():
            msk = p.sb([128, T // 2], F32, 'hgmsk')
            p.i(PL, 'memset', msk[:], 1.0, writes=['hgmsk'])
            p.i(PL, 'memset', msk[:].rearrange("p (c k) -> p c k", k=32)[:, :, 0:1], 0.0, writes=['hgmsk'])
            hm = p.sb([128, 2, 128], BF16, 'hgm'); bo = p.sb([128, 128], BF16, 'hgbo')
            p.dma('sp', hm[:], hg_mask_d.rearrange("d s t -> s d t"), writes=['hgm'])
            p.dma('sp', bo[:], blockones_d[:, :], writes=['hgbo'])
            lbr = p.sb([128, 2, 2, 2], F32, 'hglbr'); lbv = p.sb([128, 2, 2], F32, 'hglb'); oml = p.sb([128, 2, 2], F32, 'hgoml')
            nw = p.sb([128, 2], F32, 'hgnw')
            p.dma('sp', lbr[:], hgrn_lb_d[:, :, :, :], writes=['hglbr'])
            p.dma('sp', nw[:], hgrn_nw_d[:, :], writes=['hgnw'])
            if l == 0:
                p.i('dve', 'memset', lbv[:], 0.0, writes=['hglb'])
            else:
                p.i('dve', 'tensor_tensor', out=lbv[:], in0=lbr[:, :, 1, :], in1=lbr[:, :, 0, :], op=ALU.subtract, reads=['hglbr'], writes=['hglb'])
                p.i('act', 'activation', out=lbv[:], in_=lbv[:], func=AF.Sigmoid, reads=['hglb'], writes=['hglb'])
            p.i('dve', 'tensor_scalar', out=oml[:], in0=lbv[:], scalar1=-1.0, scalar2=1.0, op0=ALU.mult, op1=ALU.add, reads=['hglb'], writes=['hgoml'])
            HN = T // 2
            tA = p.sb([128, HN], F32, 'hgA'); tB = p.sb([128, HN], F32, 'hgB'); tC = p.sb([128, HN], F32, 'hgC'); tD = p.sb([128, HN], F32, 'hgD')
            zb = p.sb([128, HN], BF16, 'hgz')
            sq_ = p.sb([128, T], BF16, 'hgsq')
            qt = [p.sb([128, T], BF16, 'hgqt%d' % d) for d in range(2)]
            kt = [p.sb([128, T], BF16, 'hgkt%d' % d) for d in range(2)]
            Sall = [p.sb([128, NCH, 64], BF16, 'hgS%d' % d) for d in range(2)]
            KhT = p.sb([128, T], BF16, 'hgKhT'); Khtm = p.sb([128, NT, 128], BF16, 'hgKhtm')
            Vc = p.sb([128, NT, 128], BF16, 'hgV')
            Dc = p.sb([128, NCH], F32, 'hgDc')
            Srun = [p.sb([128, 64], F32, 'hgSr%d' % i) for i in range(2)]
            gsb = p.sb([128, 512], BF16, 'hgg')
            tpa = p.ps([128, 128], F32, 'hgtpa'); tpb = p.ps([128, 128], F32, 'hgtpb'); tps = [tpa[:], tpb[:]]
            Ups = [p.ps([128, 512], F32, 'hgU%d' % i) for i in range(2)]
            Ops = [p.ps([128, 512], F32, 'hgO%d' % i) for i in range(2)]
            Aps = tps
            SSp = p.ps([128, 512], F32, 'hgSS')
            attn = [p.sb([128, 128], BF16, 'hgat%d' % i) for i in range(3)]
            n1 = p.sb([128, 512], F32, 'hgn1'); n2 = p.sb([128, 512], F32, 'hgn2'); n3 = p.sb([128, 512], BF16, 'hgn3')
            for ct in range(2):
                for hf in range(2):
                    t0 = hf * HN
                    p.dma('sp', zb[:], Pfm[b, (10 + ct) * 128:(11 + ct) * 128, t0:t0 + HN], reads=['Pfm'], writes=['hgz'])
                    p.i('act', 'activation', out=sq_[:, t0:t0 + HN], in_=zb[:], func=AF.Silu, reads=['hgz'], writes=['hgsq'])
                p.dma('pool', Vc[:], Ptm[b].rearrange("(n p) c -> p n c", p=128)[:, :, 384 + ct * 128:512 + ct * 128], reads=['Ptm'], writes=['hgV'])
                for d in range(2):
                    lb_ap = lbv[:, d, ct:ct + 1]; oml_ap = oml[:, d, ct:ct + 1]
                    for hf in range(2):
                        t0 = hf * HN; c0 = t0 // 32; ncq = HN // 32
                        p.dma('sp', zb[:], Pfm[b, (12 + 2 * d + ct) * 128:(13 + 2 * d + ct) * 128, t0:t0 + HN], reads=['Pfm'], writes=['hgz'])
                        p.i('act', 'activation', out=tA[:], in_=zb[:], func=AF.Sigmoid, reads=['hgz'], writes=['hgA'])
                        p.i('dve', 'tensor_scalar', out=tA[:], in0=tA[:], scalar1=oml_ap, scalar2=lb_ap, op0=ALU.mult, op1=ALU.add,
                            reads=['hgA', 'hglb', 'hgoml'], writes=['hgA'])
                        p.i('act', 'activation', out=tB[:], in_=tA[:], func=AF.Ln, reads=['hgA'], writes=['hgB'])
                        p.i('dve', 'tensor_tensor_scan', out=tC[:], data0=msk[:, 0:HN], data1=tB[:], initial=0.0, op0=ALU.mult, op1=ALU.add,
                            reads=['hgmsk', 'hgB'], writes=['hgC'])
                        p.i(PL, 'tensor_scalar', out=tA[:], in0=tA[:], scalar1=-1.0, scalar2=1.0, op0=ALU.mult, op1=ALU.add,
                            reads=['hgA'], writes=['hgA'])
                        C3 = tC[:].rearrange("p (c k) -> p c k", k=32)
                        totb = C3[:, :, 31:32].to_broadcast([128, ncq, 32])
                        p.i('act', 'activation', out=Dc[:, c0:c0 + ncq], in_=C3[:, :, 31], func=AF.Exp, reads=['hgC'], writes=['hgDc'])
                        B3 = tB[:].rearrange("p (c k) -> p c k", k=32); D3 = tD[:].rearrange("p (c k) -> p c k", k=32)
                        if d == 0:
                            p.i('dve', 'tensor_tensor', out=B3, in0=totb, in1=C3, op=ALU.subtract, reads=['hgC'], writes=['hgB'])
                            b_t, bl_t, bk, blk = tC, tB, 'hgC', 'hgB'
                        else:
                            p.i('dve', 'tensor_tensor', out=tB[:], in0=tC[:], in1=tB[:], op=ALU.subtract, reads=['hgC', 'hgB'], writes=['hgB'])
                            p.i('dve', 'tensor_tensor', out=D3, in0=totb, in1=B3, op=ALU.subtract, reads=['hgC', 'hgB'], writes=['hgD'])
                            b_t, bl_t, bk, blk = tD, tB, 'hgD', 'hgB'
                        p.i('act', 'activation', out=bl_t[:], in_=bl_t[:], func=AF.Exp, reads=[blk], writes=[blk])
                        p.i(PL, 'tensor_tensor', out=KhT[:, t0:t0 + HN], in0=tA[:], in1=bl_t[:], op=ALU.mult, reads=['hgA', blk], writes=['hgKhT'])
                        o_t, ok = (tD, 'hgD') if d == 0 else (tC, 'hgC')
                        p.i('act', 'activation', out=o_t[:], in_=b_t[:], func=AF.Exp, scale=-1.0, reads=[bk], writes=[ok])
                        p.i('dve', 'tensor_tensor', out=kt[d][:, t0:t0 + HN], in0=tA[:], in1=o_t[:], op=ALU.mult, reads=['hgA', ok], writes=['hgkt%d' % d])
                        p.i('act', 'activation', out=b_t[:], in_=b_t[:], func=AF.Exp, reads=[bk], writes=[bk])
                        p.i('dve', 'tensor_tensor', out=qt[d][:, t0:t0 + HN], in0=sq_[:, t0:t0 + HN], in1=b_t[:], op=ALU.mult, reads=['hgsq', bk], writes=['hgqt%d' % d])
                    for tt in range(0 if 'notr' in HGDBG else NT):
                        ti = tt % 2
                        p.i('pe', 'matmul', tps[ti], lhsT=KhT[:, tt * 128:(tt + 1) * 128], rhs=ident_b[:], start=True, stop=True,
                            reads=['hgKhT', 'ident_b'], writes=[('hgtp', ti)])
                        if tt % 2 == 0:
                            p.i('act', 'activation', out=Khtm[:, tt, :], in_=tps[ti], func=AF.Copy, reads=[('hgtp', ti)], writes=['hgKhtm'])
                        else:
                            p.i('dve', 'tensor_copy', out=Khtm[:, tt, :], in_=tps[ti], reads=[('hgtp', ti)], writes=['hgKhtm'])
                    order = orders[d]
                    p.i('dve', 'memset', Srun[0][:], 0.0, writes=[('hgSr', 0)])
                    p.i(PL, 'memset', Sall[d][:, order[0], :], 0.0, writes=['hgS%d' % d])
                    import os
                    HGDBG = os.environ.get('HGDBG', '')
                    for idx in range(0 if 'nostate' in HGDBG else NCH - 1):
                        c = order[idx]; tile_ = c // 4; j = c % 4
                        bank = (idx // 8) % 2; slot = idx % 8
                        for hl in range(2):
                            p.i('pe', 'matmul', Ups[bank][hl * 64:(hl + 1) * 64, slot * 64:(slot + 1) * 64], lhsT=Khtm[32 * j:32 * j + 32, tile_, hl * 64:(hl + 1) * 64],
                                rhs=Vc[32 * j:32 * j + 32, tile_, hl * 64:(hl + 1) * 64], start=True, stop=True, tile_position=(32 * j, 64 * hl),
                                reads=['hgKhtm', 'hgV'], writes=[('hgU', bank)], sig=(hl == 1))
                        so = Srun[idx % 2]; sn = Srun[(idx + 1) % 2]
                        p.i('dve', 'scalar_tensor_tensor', out=sn[:], in0=so[:], scalar=Dc[:, c:c + 1], in1=Ups[bank][:, slot * 64:(slot + 1) * 64], op0=ALU.mult, op1=ALU.add,
                            reads=[('hgSr', idx % 2), 'hgDc', ('hgU', bank)], writes=[('hgSr', (idx + 1) % 2)])
                        p.i('act', 'activation', out=Sall[d][:, order[idx + 1], :], in_=sn[:], func=AF.Copy,
                            reads=[('hgSr', (idx + 1) % 2)], writes=['hgS%d' % d])
                acnt = 0
                for gi, (t0, n) in enumerate([] if 'nopass2' in HGDBG else BLKS):
                    Op = Ops[gi % 2]; ok_ = ('hgO', gi % 2)
                    p.dma('sp', gsb[:, 0:n], Pfm[b, (16 + ct) * 128:(17 + ct) * 128, t0:t0 + n], reads=['Pfm'], writes=['hgg'])
                    for tl in range(n // 128):
                        tt = (t0 // 128) + tl
                        cs = slice(tl * 128, (tl + 1) * 128); ts_ = slice(tt * 128, (tt + 1) * 128)
                        for hl in range(2):
                            hb = hl * 64
                            for d in range(2):
                                ai = acnt % 2; ati = acnt % 3; acnt += 1
                                p.i('pe', 'matmul', Aps[ai], lhsT=kt[d][hb:hb + 64, ts_], rhs=qt[d][hb:hb + 64, ts_], start=True, stop=True,
                                    reads=['hgkt%d' % d, 'hgqt%d' % d], writes=[('hgtp', ai)])
                                p.i('dve', 'tensor_tensor', out=attn[ati][:], in0=Aps[ai], in1=hm[:, d, :], op=ALU.mult,
                                    reads=[('hgtp', ai), 'hgm'], writes=[('hgat', ati)])
                                p.i('pe', 'matmul', Op[hb:hb + 64, cs], lhsT=Vc[:, tt, hb:hb + 64], rhs=attn[ati][:], start=(d == 0), stop=False,
                                    reads=['hgV', ('hgat', ati)], writes=[ok_], sig=False, skip_group_check=True)
                                for j in range(4):
                                    c = tt * 4 + j
                                    p.i('pe', 'matmul', Op[hb:hb + 64, tl * 128 + 32 * j:tl * 128 + 32 * j + 32], lhsT=Sall[d][hb:hb + 64, c, :],
                                        rhs=qt[d][hb:hb + 64, c * 32:(c + 1) * 32], start=False, stop=(d == 1),
                                        reads=['hgS%d' % d, 'hgqt%d' % d], writes=[ok_], sig=(j == 3), skip_group_check=True)
                    p.i('act', 'activation', out=n3[:, 0:n], in_=Op[:, 0:n], func=AF.Square, reads=[ok_], writes=['hgn3'])
                    p.i('pe', 'matmul', SSp[:, 0:n], lhsT=bo[:], rhs=n3[:, 0:n], start=True, stop=True, reads=['hgbo', 'hgn3'], writes=['hgSS'])
                    p.i('dve', 'tensor_scalar', out=n1[:, 0:n], in0=SSp[:, 0:n], scalar1=1.0 / 64, scalar2=1e-6, op0=ALU.mult, op1=ALU.add,
                        reads=['hgSS'], writes=['hgn1'])
                    p.i('act', 'activation', out=n1[:, 0:n], in_=n1[:, 0:n], func=AF.Sqrt, reads=['hgn1'], writes=['hgn1'])
                    p.i('dve', 'reciprocal', out=n1[:, 0:n], in_=n1[:, 0:n], reads=['hgn1'], writes=['hgn1'])
                    p.i('act', 'activation', out=n2[:, 0:n], in_=gsb[:, 0:n], func=AF.Silu, reads=['hgg'], writes=['hgn2'])
                    p.i('dve', 'scalar_tensor_tensor', out=n1[:, 0:n], in0=n1[:, 0:n], scalar=nw[:, l:l + 1], in1=n2[:, 0:n], op0=ALU.mult, op1=ALU.mult,
                        reads=['hgn1', 'hgn2', 'hgnw'], writes=['hgn1'])
                    p.i('dve', 'tensor_tensor', out=G['MIXT'][:, 4 + ct, t0:t0 + n], in0=Op[:, 0:n], in1=n1[:, 0:n], op=ALU.mult,
                        reads=[ok_, 'hgn1'], writes=[('MIXT', 4 + ct)])

    TWO_PI = 6.283185307179586
    PI = 3.141592653589793

    def sincos(src, n, sin_out, cos_out, tmpf, tmpi, key_src, keys, mpi, eng='dve'):
        kf, ki, ks, kc = keys
        for (off, out_ap, ko) in ((0.0, sin_out, ks), (PI / 2, cos_out, kc)):
            p.i('dve', 'tensor_scalar', out=tmpf, in0=src, scalar1=off, scalar2=1.0 / TWO_PI, op0=ALU.add, op1=ALU.mult, reads=[key_src], writes=[kf])
            p.i('dve', 'tensor_copy', out=tmpi, in_=tmpf, reads=[kf], writes=[ki])
            p.i('dve', 'tensor_copy', out=tmpf, in_=tmpi, reads=[ki], writes=[kf])
            p.i('dve', 'scalar_tensor_tensor', out=tmpf, in0=tmpf, scalar=-TWO_PI, in1=src, op0=ALU.mult, op1=ALU.add, reads=[kf, key_src], writes=[kf])
            p.i('dve', 'tensor_scalar', out=tmpf, in0=tmpf, scalar1=off + PI, scalar2=None, op0=ALU.add, reads=[kf], writes=[kf])
            p.i('act', 'activation', out=out_ap, in_=tmpf, func=AF.Sin, bias=mpi[:], reads=[kf, 's5mpi'], writes=[ko])

    def s5mix(l, b):
        with p.phase():
            mpi = p.sb([128, 1], F32, 's5mpi')
            p.i('dve', 'memset', mpi[:], -PI, writes=['s5mpi'])
            Yacc = p.sb([128, 2, T], F32, 's5Y')
            d2 = p.sb([128, 2], F32, 's5d'); gb = p.sb([128, 2], F32, 's5gb')
            p.dma('sp', d2[:], s5['s5_d2'][l], writes=['s5d']); p.dma('sp', gb[:], s5['s5_glu_b2'][l], writes=['s5gb'])
            J128 = p.sb([128, 128], BF16, 's5J'); Msw = p.sb([128, 128], F32, 's5Msw'); sv = p.sb([128, 128], F32, 's5sv')
            p.dma('sp', J128[:], s5['Jrev128'][:, :], writes=['s5J']); p.dma('sp', Msw[:], s5['MswT'][:, :], writes=['s5Msw'])
            p.dma('sp', sv[:], s5['svec'][:, :], writes=['s5sv'])
            with p.phase():
                ub = p.sb([128, T], BF16, 's5ub')
                for ct in range(2):
                    p.dma('sp', ub[:], Pfm[b, (18 + ct) * 128:(19 + ct) * 128, :], reads=['Pfm'], writes=['s5ub'])
                    p.i('dve', 'tensor_scalar', out=Yacc[:, ct, :], in0=ub[:], scalar1=d2[:, ct:ct + 1], scalar2=None, op0=ALU.mult,
                        reads=['s5ub', 's5d'], writes=[('s5Y', ct)])
            cosT = p.sb([128, 16, 128], F32, 's5cos'); sinT = p.sb([128, 16, 128], F32, 's5sin')
            Bw1 = p.sb([128, 16, 128], BF16, 's5Bw1'); Bw2 = p.sb([128, 16, 128], BF16, 's5Bw2')
            C1 = p.sb([128, 16, 16], BF16, 's5C1'); C2 = p.sb([128, 16, 16], BF16, 's5C2')
            rho = p.sb([128, 16], F32, 's5rho')
            for d in range(2):
                Rm = ident_b if d == 0 else J128
                rkey = 'ident_b' if d == 0 else 's5J'
                with p.phase():
                    lre = p.sb([128, 16], F32, 'q_lre'); lim = p.sb([128, 16], F32, 'q_lim'); ldt = p.sb([128, 16], F32, 'q_ldt')
                    p.dma('sp', lre[:], s5['s5_lre_p'][l, :, d, :], writes=['q_lre']); p.dma('sp', lim[:], s5['s5_lim_p'][l, :, d, :], writes=['q_lim'])
                    p.dma('sp', ldt[:], s5['s5_ldt_p'][l, :, d, :], writes=['q_ldt'])
                    p.i('act', 'activation', out=ldt[:], in_=ldt[:], func=AF.Exp, reads=['q_ldt'], writes=['q_ldt'])
                    p.i('dve', 'tensor_tensor', out=lre[:], in0=lre[:], in1=ldt[:], op=ALU.mult, reads=['q_lre', 'q_ldt'], writes=['q_lre'])
                    p.i('act', 'activation', out=rho[:], in_=lre[:], func=AF.Exp, reads=['q_lre'], writes=['s5rho'])
                    p.i('dve', 'tensor_tensor', out=lim[:], in0=lim[:], in1=ldt[:], op=ALU.mult, reads=['q_lim', 'q_ldt'], writes=['q_lim'])
                    ang = p.sb([128, 16, 128], F32, 'q_ang'); tf = p.sb([128, 16, 128], F32, 'q_tf'); ti_ = p.sb([128, 16, 128], I32, 'q_ti')
                    p.i('dve', 'tensor_tensor', out=ang[:], in0=lim[:].unsqueeze(2).to_broadcast([128, 16, 128]),
                        in1=sv[:].unsqueeze(1).to_broadcast([128, 16, 128]), op=ALU.mult, reads=['q_lim', 's5sv'], writes=['q_ang'])
                    sincos(ang[:], None, sinT[:], cosT[:], tf[:], ti_[:], 'q_ang', ('q_tf', 'q_ti', 's5sin', 's5cos'), mpi)
                with p.phase():
                    R = lambda nm: p.sb([128, 1024], F32, nm)
                    rl, ri, rd_ = R('r_lre'), R('r_lim'), R('r_ldt')
                    p.dma('sp', rl[:], s5['s5_lre_r'][l, d].partition_broadcast(128), writes=['r_lre'])
                    p.dma('sp', ri[:], s5['s5_lim_r'][l, d].partition_broadcast(128), writes=['r_lim'])
                    p.dma('sp', rd_[:], s5['s5_ldt_r'][l, d].partition_broadcast(128), writes=['r_ldt'])
                    p.i('act', 'activation', out=rd_[:], in_=rd_[:], func=AF.Exp, reads=['r_ldt'], writes=['r_ldt'])
                    mag, th = R('r_mag'), R('r_th')
                    p.i('dve', 'tensor_tensor', out=mag[:], in0=rl[:], in1=rd_[:], op=ALU.mult, reads=['r_lre', 'r_ldt'], writes=['r_mag'])
                    p.i('act', 'activation', out=mag[:], in_=mag[:], func=AF.Exp, reads=['r_mag'], writes=['r_mag'])
                    p.i('dve', 'tensor_tensor', out=th[:], in0=ri[:], in1=rd_[:], op=ALU.mult, reads=['r_lim', 'r_ldt'], writes=['r_th'])
                    sn, cs = R('r_sn'), R('r_cs'); tf2 = R('r_tf'); ti2 = p.sb([128, 1024], I32, 'r_ti')
                    sincos(th[:], None, sn[:], cs[:], tf2[:], ti2[:], 'r_th', ('r_tf', 'r_ti', 'r_sn', 'r_cs'), mpi)
                    p.i('dve', 'tensor_tensor', out=cs[:], in0=cs[:], in1=mag[:], op=ALU.mult, reads=['r_cs', 'r_mag'], writes=['r_cs'])
                    p.i('dve', 'tensor_scalar', out=cs[:], in0=cs[:], scalar1=-1.0, scalar2=None, op0=ALU.add, reads=['r_cs'], writes=['r_cs'])
                    p.i('dve', 'tensor_tensor', out=sn[:], in0=sn[:], in1=mag[:], op=ALU.mult, reads=['r_sn', 'r_mag'], writes=['r_sn'])
                    p.i('dve', 'tensor_tensor', out=mag[:], in0=rl[:], in1=rl[:], op=ALU.mult, reads=['r_lre'], writes=['r_mag'])
                    p.i('dve', 'tensor_tensor', out=th[:], in0=ri[:], in1=ri[:], op=ALU.mult, reads=['r_lim'], writes=['r_th'])
                    p.i('dve', 'tensor_tensor', out=mag[:], in0=mag[:], in1=th[:], op=ALU.add, reads=['r_mag', 'r_th'], writes=['r_mag'])
                    p.i('dve', 'reciprocal', out=mag[:], in_=mag[:], reads=['r_mag'], writes=['r_mag'])
                    p.i('dve', 'tensor_tensor', out=th[:], in0=cs[:], in1=rl[:], op=ALU.mult, reads=['r_cs', 'r_lre'], writes=['r_th'])
                    p.i('dve', 'tensor_tensor', out=tf2[:], in0=sn[:], in1=ri[:], op=ALU.mult, reads=['r_sn', 'r_lim'], writes=['r_tf'])
                    p.i('dve', 'tensor_tensor', out=th[:], in0=th[:], in1=tf2[:], op=ALU.add, reads=['r_th', 'r_tf'], writes=['r_th'])
                    p.i('dve', 'tensor_tensor', out=th[:], in0=th[:], in1=mag[:], op=ALU.mult, reads=['r_th', 'r_mag'], writes=['r_th'])
                    p.i('dve', 'tensor_tensor', out=tf2[:], in0=sn[:], in1=rl[:], op=ALU.mult, reads=['r_sn', 'r_lre'], writes=['r_tf'])
                    p.i('dve', 'tensor_tensor', out=rd_[:], in0=cs[:], in1=ri[:], op=ALU.mult, reads=['r_cs', 'r_lim'], writes=['r_ldt'])
                    p.i('dve', 'tensor_tensor', out=tf2[:], in0=tf2[:], in1=rd_[:], op=ALU.subtract, reads=['r_tf', 'r_ldt'], writes=['r_tf'])
                    p.i('dve', 'tensor_tensor', out=tf2[:], in0=tf2[:], in1=mag[:], op=ALU.mult, reads=['r_tf', 'r_mag'], writes=['r_tf'])
                    p.dma('sp', rl[:], s5['s5_Br_emb'][l, d].rearrange("p g q -> p (g q)"), writes=['r_lre'])
                    p.dma('sp', ri[:], s5['s5_Bi_emb'][l, d].rearrange("p g q -> p (g q)"), writes=['r_lim'])
                    p.i('dve', 'tensor_tensor', out=cs[:], in0=th[:], in1=rl[:], op=ALU.mult, reads=['r_th', 'r_lre'], writes=['r_cs'])
                    p.i('dve', 'tensor_tensor', out=mag[:], in0=tf2[:], in1=ri[:], op=ALU.mult, reads=['r_tf', 'r_lim'], writes=['r_mag'])
                    p.i('dve', 'tensor_tensor', out=cs[:], in0=cs[:], in1=mag[:], op=ALU.subtract, reads=['r_cs', 'r_mag'], writes=['r_cs'])
                    p.i('dve', 'tensor_tensor', out=sn[:], in0=th[:], in1=ri[:], op=ALU.mult, reads=['r_th', 'r_lim'], writes=['r_sn'])
                    p.i('dve', 'tensor_tensor', out=mag[:], in0=tf2[:], in1=rl[:], op=ALU.mult, reads=['r_tf', 'r_lre'], writes=['r_mag'])
                    p.i('dve', 'tensor_tensor', out=sn[:], in0=sn[:], in1=mag[:], op=ALU.add, reads=['r_sn', 'r_mag'], writes=['r_sn'])
                    cs3 = cs[:].rearrange("p (g q) -> p g q", q=64); sn3 = sn[:].rearrange("p (g q) -> p g q", q=64)
                    p.i('dve', 'tensor_copy', out=Bw1[:, :, 0:64], in_=cs3, reads=['r_cs'], writes=['s5Bw1'])
                    p.i('dve', 'tensor_copy', out=Bw1[:, :, 64:128], in_=sn3, reads=['r_sn'], writes=['s5Bw1'])
                    p.i('dve', 'tensor_copy', out=Bw2[:, :, 0:64], in_=sn3, reads=['r_sn'], writes=['s5Bw2'])
                    p.i('dve', 'tensor_scalar', out=Bw2[:, :, 64:128], in0=cs3, scalar1=-1.0, scalar2=None, op0=ALU.mult, reads=['r_cs'], writes=['s5Bw2'])
                with p.phase():
                    cr = p.sb([128, 16, 16], F32, 'r_cr'); ci = p.sb([128, 16, 16], F32, 'r_ci')
                    p.dma('sp', cr[:], s5['s5_Cr2'][l, d], writes=['r_cr']); p.dma('sp', ci[:], s5['s5_Ci2'][l, d], writes=['r_ci'])
                    p.i('dve', 'tensor_copy', out=C1[0:64], in_=cr[0:64], reads=['r_cr'], writes=['s5C1'])
                    p.i('dve', 'tensor_scalar', out=C1[64:128], in0=ci[64:128], scalar1=-1.0, scalar2=None, op0=ALU.mult, reads=['r_ci'], writes=['s5C1'])
                    p.i('dve', 'tensor_scalar', out=C2[0:64], in0=ci[0:64], scalar1=-1.0, scalar2=None, op0=ALU.mult, reads=['r_ci'], writes=['s5C2'])
                    p.i('dve', 'tensor_scalar', out=C2[64:128], in0=cr[64:128], scalar1=-1.0, scalar2=None, op0=ALU.mult, reads=['r_cr'], writes=['s5C2'])
                with p.phase():
                    uTs = p.sb([128, 2, 128], BF16, 'm_uT')
                    utl = [p.sb([128, 256], BF16, 's5U%d' % i) for i in range(2)]
                    t2 = p.sb([128, 1024], BF16, 'm_t2')
                    inp_ = p.sb([128, 16, 128], F32, 'm_inp'); xt = p.sb([128, 16, 128], F32, 'm_xt')
                    P1 = p.sb([128, 16, 128], BF16, 'm_P1'); P2 = p.sb([128, 16, 128], BF16, 'm_P2')
                    carry = p.sb([128, 16], F32, 'm_carry'); pl1 = p.sb([128, 16], F32, 'm_pl1'); pl2 = p.sb([128, 16], F32, 'm_pl2')
                    Ytm = p.sb([128, 256], BF16, 'm_Ytm')
                    p.i('dve', 'memset', carry[:], 0.0, writes=['m_carry'])
                    ups = [p.ps([128, 128], F32, 'm_ups%d' % i) for i in range(2)]
                    bs = [p.ps([128, 1024], F32, 'm_bs%d' % i) for i in range(2)]
                    cps = p.ps([128, 16], F32, 'm_cps'); yps = p.ps([128, 256], F32, 'm_yps')
                    order = list(range(NT)) if d == 0 else [1, 0] + list(range(NT - 1, 1, -1))
                    inp2 = p.sb([128, 16, 128], F32, 'm_inpB'); uTs2 = p.sb([128, 2, 128], BF16, 'm_uTB')
                    inps = [inp_, inp2]; uTl = [uTs, uTs2]

                    def stage_in(tt, bf_):
                        ib = inps[bf_]; ub_ = uTl[bf_]
                        p.dma('sp', utl[bf_][:], Ptm[b, tt * 128:(tt + 1) * 128, 640:896], reads=['Ptm'], writes=[('s5U', bf_)])
                        for ct in range(2):
                            p.i('pe', 'matmul', ups[ct][:], lhsT=utl[bf_][:, ct * 128:(ct + 1) * 128], rhs=Rm[:], start=True, stop=True,
                                reads=[('s5U', bf_), rkey], writes=[('m_ups', ct)])
                            p.i('act', 'activation', out=ub_[:, ct, :], in_=ups[ct][:], func=AF.Copy, reads=[('m_ups', ct)], writes=[('m_uT%d' % bf_, ct)])
                        for gh in range(2):
                            for gl in range(8):
                                g = gh * 8 + gl
                                p.i('pe', 'matmul', bs[0][:, gl * 128:(gl + 1) * 128], lhsT=Bw1[:, g, :], rhs=ub_[:, gh, :], start=True, stop=True,
                                    reads=['s5Bw1', ('m_uT%d' % bf_, gh)], writes=[('m_bs', 0)], sig=(gl == 7))
                            for gl in range(8):
                                g = gh * 8 + gl
                                p.i('pe', 'matmul', bs[1][:, gl * 128:(gl + 1) * 128], lhsT=Bw2[:, g, :], rhs=ub_[:, gh, :], start=True, stop=True,
                                    reads=['s5Bw2', ('m_uT%d' % bf_, gh)], writes=[('m_bs', 1)], sig=(gl == 7))
                            gs = slice(gh * 8, gh * 8 + 8)
                            c3 = cosT[:, gs, :].rearrange("p g s -> p (g s)"); s3 = sinT[:, gs, :].rearrange("p g s -> p (g s)")
                            ibv = ib[:, gs, :].rearrange("p g s -> p (g s)")
                            p.i('dve', 'tensor_tensor', out=ibv, in0=bs[0][:], in1=c3, op=ALU.mult, reads=[('m_bs', 0), 's5cos'], writes=[('m_inp%d' % bf_, gh)])
                            p.i('dve', 'tensor_tensor', out=t2[:], in0=bs[1][:], in1=s3, op=ALU.mult, reads=[('m_bs', 1), 's5sin'], writes=['m_t2'])
                            p.i('pool', 'tensor_tensor', out=ibv, in0=ibv, in1=t2[:], op=ALU.add,
                                reads=['m_t2', ('m_inp%d' % bf_, gh)], writes=[('m_inp%d' % bf_, gh)])

                    stage_in(order[0], 0)
                    for k_, tt in enumerate(order):
                        bf_ = k_ % 2
                        ib = inps[bf_]
                        for g in range(16):
                            p.i('dve', 'tensor_tensor_scan', out=xt[:, g, :], data0=rho[:, g:g + 1].to_broadcast([128, 128]), data1=ib[:, g, :],
                                initial=carry[:, g:g + 1], op0=ALU.mult, op1=ALU.add,
                                reads=['s5rho', ('m_inp%d' % bf_, g // 8), 'm_carry'], writes=[('m_xt', g)])
                        p.i('dve', 'tensor_tensor', out=pl1[:], in0=xt[:, :, 127], in1=cosT[:, :, 127], op=ALU.mult, reads=['m_xt', 's5cos'], writes=['m_pl1'])
                        p.i('dve', 'tensor_tensor', out=pl2[:], in0=xt[:, :, 127], in1=sinT[:, :, 127], op=ALU.mult, reads=['m_xt', 's5sin'], writes=['m_pl2'])
                        p.i('pe', 'matmul', cps[:], lhsT=ident_f[:], rhs=pl1[:], start=True, stop=False, reads=['ident_f', 'm_pl1'], writes=['m_cps'], sig=False)
                        p.i('pe', 'matmul', cps[:], lhsT=Msw[:], rhs=pl2[:], start=False, stop=True, reads=['s5Msw', 'm_pl2'], writes=['m_cps'])
                        p.i('pool', 'tensor_tensor', out=P1[:], in0=xt[:], in1=cosT[:], op=ALU.mult, reads=['m_xt', 's5cos'], writes=['m_P1'])
                        p.i('dve', 'tensor_tensor', out=P2[:], in0=xt[:], in1=sinT[:], op=ALU.mult, reads=['m_xt', 's5sin'], writes=['m_P2'])
                        if k_ + 1 < len(order):
                            stage_in(order[k_ + 1], 1 - bf_)
                        p.i('dve', 'tensor_copy', out=carry[:], in_=cps[:], reads=['m_cps'], writes=['m_carry'])
                        for g in range(16):
                            p.i('pe', 'matmul', yps[:, g * 16:(g + 1) * 16], lhsT=P1[:, g, :], rhs=C1[:, g, :], start=True, stop=False,
                                reads=['m_P1', 's5C1'], writes=['m_yps'], sig=False)
                            p.i('pe', 'matmul', yps[:, g * 16:(g + 1) * 16], lhsT=P2[:, g, :], rhs=C2[:, g, :], start=False, stop=True,
                                reads=['m_P2', 's5C2'], writes=['m_yps'], sig=(g == 15))
                        p.i('act', 'activation', out=Ytm[:], in_=yps[:], func=AF.Copy, reads=['m_yps'], writes=['m_Ytm'])
                        for ct in range(2):
                            p.i('pe', 'matmul', ups[ct][:], lhsT=Ytm[:, ct * 128:(ct + 1) * 128], rhs=Rm[:], start=True, stop=True,
                                reads=['m_Ytm', rkey], writes=[('m_ups', ct)])
                            p.i('dve', 'tensor_tensor', out=Yacc[:, ct, tt * 128:(tt + 1) * 128], in0=ups[ct][:], in1=Yacc[:, ct, tt * 128:(tt + 1) * 128],
                                op=ALU.add, reads=[('m_ups', ct), ('s5Y', ct)], writes=[('s5Y', ct)])
            with p.phase():
                gw_f = p.sb([128, 2, 256], F32, 'g_wf'); gw = p.sb([128, 2, 256], BF16, 'g_w')
                p.dma('sp', gw_f[:], s5['s5_glu_w'][l].rearrange("(c p) n -> p c n", p=128), writes=['g_wf'])
                p.i('dve', 'tensor_copy', out=gw[:], in_=gw_f[:], reads=['g_wf'], writes=['g_w'])
                zT = p.sb([128, 2, T], BF16, 'g_z'); w1 = p.sb([128, T], F32, 'g_w1'); w2 = p.sb([128, T], F32, 'g_w2')
                for ct in range(2):
                    y = Yacc[:, ct, :]
                    p.i('dve', 'tensor_tensor', out=w1[:], in0=y, in1=y, op=ALU.mult, reads=[('s5Y', ct)], writes=['g_w1'])
                    p.i('dve', 'tensor_scalar', out=w1[:], in0=w1[:], scalar1=0.044715, scalar2=1.0, op0=ALU.mult, op1=ALU.add, reads=['g_w1'], writes=['g_w1'])
                    p.i('dve', 'tensor_tensor', out=w1[:], in0=w1[:], in1=y, op=ALU.mult, reads=['g_w1', ('s5Y', ct)], writes=['g_w1'])
                    p.i('act', 'activation', out=w2[:], in_=w1[:], func=AF.Sigmoid, scale=1.5957691216057308, reads=['g_w1'], writes=['g_w2'])
                    p.i('dve', 'tensor_tensor', out=zT[:, ct, :], in0=w2[:], in1=y, op=ALU.mult, reads=['g_w2', ('s5Y', ct)], writes=[('g_z', ct)])
                gps = [p.ps([128, 512], F32, 'g_ps%d' % i) for i in range(2)]
                sg_ = [p.sb([128, 512], F32, 'g_sg%d' % i) for i in range(2)]
                k = 0
                for oc in range(2):
                    for (t0, n) in BLKS:
                        i = k % 2; k += 1
                        for kc in range(2):
                            p.i('pe', 'matmul', gps[i][:, 0:n], lhsT=gw[:, kc, oc * 128:(oc + 1) * 128], rhs=zT[:, kc, t0:t0 + n], start=(kc == 0), stop=(kc == 1),
                                reads=['g_w', 'g_z'], writes=[('g_ps', i)], sig=(kc == 1))
                        p.i('act', 'activation', out=sg_[i][:, 0:n], in_=gps[i][:, 0:n], func=AF.Sigmoid, bias=gb[:, oc:oc + 1],
                            reads=[('g_ps', i), 's5gb'], writes=[('g_sg', i)])
                        p.i('dve', 'tensor_tensor', out=G['MIXT'][:, 6 + oc, t0:t0 + n], in0=sg_[i][:, 0:n], in1=zT[:, oc, t0:t0 + n], op=ALU.mult,
                            reads=[('g_sg', i), 'g_z'], writes=[('MIXT', 6 + oc)])

    def swa(l, b, need_ctx):
        with p.phase():
            qg = [p.sb([128, T], BF16, 'swq%d' % i) for i in range(2)]
            kT = p.sb([128, T], BF16, 'swk'); V = p.sb([128, NT, 128], BF16, 'swv')
            mk = p.sb([128, 384], BF16, 'swmask'); esk = p.sb([128, 4], F32, 'esk')
            p.dma('sp', qg[0][:], Pfm[b, 0:128, :], reads=['Pfm'], writes=['swq0'])
            p.dma('pool', qg[1][:], Pfm[b, 128:256, :], reads=['Pfm'], writes=['swq1'])
            p.dma('sp', kT[:], Pfm[b, 512:640, :], reads=['Pfm'], writes=['swk'])
            p.dma('pool', V[:], Ptm[b].rearrange("(n p) c -> p n c", p=128)[:, :, 0:128], reads=['Ptm'], writes=['swv'])
            p.dma('sp', mk[:], swa_mask_d[:, :], writes=['swmask'])
            p.dma('sp', esk[:], swa_sink_d[l].partition_broadcast(128), writes=['esk'])
            p.i('act', 'activation', out=esk[:], in_=esk[:], func=AF.Exp, reads=['esk'], writes=['esk'])
            O = p.ps([64, 1024], F32, 'swO'); Dn = p.ps([64, 1024], F32, 'swD')
            st = [p.ps([128, 512], F32, 'swst%d' % i) for i in range(2)]
            pt = [p.sb([128, 512], BF16, 'swpt%d' % i) for i in range(3)]
            rd = p.sb([64, 1024], F32, 'swrd')
            cnt = [0]
            for h in range(4):
                qT = qg[h % 2]; base = (h // 2) * 64; kh = h // 2
                qsets = [(LC + 1024 * qh, 1024, True, qh) for qh in range(2)]
                if need_ctx:
                    qsets.append((0, LC, False, None))
                for (tq0, nqs, lat, qh) in qsets:
                    started = [False] * ((nqs + 511) // 512)

                    def pv(ptb, pti, ktile, a, bnd):
                        c = a
                        while c < bnd:
                            e_ = min(bnd, (c // 512 + 1) * 512)
                            bk = c // 512
                            stt = not started[bk]
                            if stt:
                                assert c % 512 == 0 and e_ - c == min(512, nqs - c)
                                started[bk] = True
                            p.i('pe', 'matmul', O[:, c:e_], lhsT=V[:, ktile, kh * 64:(kh + 1) * 64], rhs=ptb[:, c - a:e_ - a],
                                start=stt, stop=False, reads=['swv', ('swpt', pti)], writes=['swO'], sig=False, skip_group_check=True)
                            p.i('pe', 'matmul', Dn[:, c:e_], lhsT=ones_b[:, 0:64], rhs=ptb[:, c - a:e_ - a],
                                start=stt, stop=False, reads=['ones_b', ('swpt', pti)], writes=['swD'], sig=True, skip_group_check=True)
                            c = e_

                    items = []
                    for ct in range(2):
                        for c0 in range(0, nqs, 512):
                            n = min(512, nqs - c0)
                            si = cnt[0] % 2; pi = cnt[0] % 3; cnt[0] += 1

                            def A_(si=si, pi=pi, ct=ct, c0=c0, n=n):
                                p.i('pe', 'matmul', st[si][:, 0:n], lhsT=kT[base:base + 64, ct * 128:(ct + 1) * 128],
                                    rhs=qT[base:base + 64, tq0 + c0:tq0 + c0 + n], start=True, stop=True,
                                    reads=['swk', 'swq%d' % (h % 2)], writes=[('swst', si)])
                                p.i('act', 'activation', out=pt[pi][:, 0:n], in_=st[si][:, 0:n], func=AF.Exp, scale=0.125,
                                    reads=[('swst', si)], writes=[('swpt', pi)])

                            def B_(pi=pi, ct=ct, c0=c0, n=n):
                                pv(pt[pi], pi, ct, c0, c0 + n)
                            items.append((A_, B_))
                    if lat:
                        for kt in range(16):
                            lo = max(kt - 1, 8 * qh); hi = min(kt + 1, 8 * qh + 7)
                            if lo > hi:
                                continue
                            n = (hi - lo + 1) * 128
                            m0 = (lo - (kt - 1)) * 128
                            qa = LC + lo * 128
                            si = cnt[0] % 2; pi = cnt[0] % 3; cnt[0] += 1
                            a = (lo - 8 * qh) * 128

                            def A_(si=si, pi=pi, kt=kt, n=n, m0=m0, qa=qa):
                                p.i('pe', 'matmul', st[si][:, 0:n], lhsT=kT[base:base + 64, LC + kt * 128:LC + (kt + 1) * 128],
                                    rhs=qT[base:base + 64, qa:qa + n], start=True, stop=False,
                                    reads=['swk', 'swq%d' % (h % 2)], writes=[('swst', si)], sig=False)
                                p.i('pe', 'matmul', st[si][:, 0:n], lhsT=ident_b[:], rhs=mk[:, m0:m0 + n], start=False, stop=True,
                                    reads=['ident_b', 'swmask'], writes=[('swst', si)])
                                p.i('act', 'activation', out=pt[pi][:, 0:n], in_=st[si][:, 0:n], func=AF.Exp, scale=0.125,
                                    reads=[('swst', si)], writes=[('swpt', pi)])

                            def B_(pi=pi, kt=kt, a=a, n=n):
                                pv(pt[pi], pi, 2 + kt, a, a + n)
                            items.append((A_, B_))
                    items[0][0]()
                    for ii in range(len(items)):
                        if ii + 1 < len(items):
                            items[ii + 1][0]()
                        items[ii][1]()
                    p.i('dve', 'tensor_scalar', out=rd[:, 0:nqs], in0=Dn[:, 0:nqs], scalar1=esk[0:64, h:h + 1], scalar2=None, op0=ALU.add,
                        reads=['swD', 'esk'], writes=['swrd'])
                    p.i('dve', 'reciprocal', out=rd[:, 0:nqs], in_=rd[:, 0:nqs], reads=['swrd'], writes=['swrd'])
                    pb = (h % 2) * 64
                    p.i('dve', 'tensor_tensor', out=G['MIXT'][pb:pb + 64, h // 2, tq0:tq0 + nqs], in0=O[:, 0:nqs], in1=rd[:, 0:nqs], op=ALU.mult,
                        reads=['swO', 'swrd'], writes=[('MIXT', h // 2)])

    def norm_mod(l, b, gsb, gname, shift_idx):
        with p.phase():
            sq = p.sb([128, 8, 512], F32, 'sq')
            rs = p.sb([128, T], F32, 'rs')
            tmp = p.sb([128, 512], F32, 'ntmp')
            pss = [p.ps([128, 512], F32, 'nps%d' % i) for i in range(2)]
            for bi, (t0, n) in enumerate(BLKS):
                ps_ = pss[bi % 2]
                p.i('act', 'activation', out=sq[:, :, 0:n], in_=XT[:, :, t0:t0 + n], func=AF.Square, reads=['XT'], writes=['sq'])
                for c in range(8):
                    p.i('pe', 'matmul', ps_[:, 0:n], lhsT=ones_f[:], rhs=sq[:, c, 0:n], start=(c == 0), stop=(c == 7),
                        reads=['sq', 'ones_f'], writes=[('nps', bi % 2)], sig=(c == 7))
                p.i('dve', 'tensor_scalar', out=tmp[:, 0:n], in0=ps_[:, 0:n], scalar1=1.0 / D, scalar2=1e-6, op0=ALU.mult, op1=ALU.add,
                    reads=[('nps', bi % 2)], writes=['ntmp'])
                p.i('act', 'activation', out=tmp[:, 0:n], in_=tmp[:, 0:n], func=AF.Sqrt, reads=['ntmp'], writes=['ntmp'])
                p.i('dve', 'reciprocal', out=rs[:, t0:t0 + n], in_=tmp[:, 0:n], reads=['ntmp'], writes=[('rs', bi)])
            xn = [p.sb([128, T], F32, 'xn%d' % i) for i in range(2)]
            for c in range(8):
                xb = xn[c % 2]
                p.i('dve' if c % 2 == 0 else 'pool', 'tensor_tensor', out=xb[:], in0=XT[:, c, :], in1=rs[:], op=ALU.mult,
                    reads=['XT', 'rs'], writes=[('xn', c % 2)])
                for (t0, n, j) in ((0, LC, nb), (LC, NL, b)):
                    p.i('act', 'activation', out=G['HT'][:, c, t0:t0 + n], in_=xb[:, t0:t0 + n], func=AF.Identity,
                        scale=gsb[l][:, c, j:j + 1], bias=modT[l][:, shift_idx * 8 + c, j:j + 1],
                        reads=[('xn', c % 2), 'modT%d' % l, gname + str(l)], writes=[('HT', c)])

    def rstd_all(rs, blks):
        sq = p.sb([128, 8, 512], F32, 'sq')
        tmp = p.sb([128, 512], F32, 'ntmp')
        pss = [p.ps([128, 512], F32, 'nps%d' % i) for i in range(2)]
        for bi, (t0, n) in enumerate(blks):
            ps_ = pss[bi % 2]
            p.i('act', 'activation', out=sq[:, :, 0:n], in_=XT[:, :, t0:t0 + n], func=AF.Square, reads=['XT'], writes=['sq'])
            for c in range(8):
                p.i('pe', 'matmul', ps_[:, 0:n], lhsT=ones_f[:], rhs=sq[:, c, 0:n], start=(c == 0), stop=(c == 7),
                    reads=['sq', 'ones_f'], writes=[('nps', bi % 2)], sig=(c == 7))
            p.i('dve', 'tensor_scalar', out=tmp[:, 0:n], in0=ps_[:, 0:n], scalar1=1.0 / D, scalar2=1e-6, op0=ALU.mult, op1=ALU.add,
                reads=[('nps', bi % 2)], writes=['ntmp'])
            p.i('act', 'activation', out=tmp[:, 0:n], in_=tmp[:, 0:n], func=AF.Sqrt, reads=['ntmp'], writes=['ntmp'])
            p.i('dve', 'reciprocal', out=rs[:, t0:t0 + n], in_=tmp[:, 0:n], reads=['ntmp'], writes=[('rs', bi)])

    def wout(l, b, blks):
        with p.phase():
            Wo = p.sb([128, 8, D], BF16, 'Wo')
            p.dma('sp', Wo[:], wout_bf[l].rearrange("(c p) n -> p c n", p=128), reads=['wout_bf'], writes=['Wo'])
            ops_ = [p.ps([128, 512], F32, 'wops%d' % i) for i in range(2)]
            k = 0
            for (t0, n) in blks:
                j = b if t0 >= LC else nb
                for dc in range(8):
                    i = k % 2; k += 1
                    for kc in range(8):
                        p.i('pe', 'matmul', ops_[i][:, 0:n], lhsT=Wo[:, kc, dc * 128:(dc + 1) * 128], rhs=G['MIXT'][:, kc, t0:t0 + n],
                            start=(kc == 0), stop=(kc == 7), reads=['Wo', 'MIXT'], writes=[('wops', i)], sig=(kc == 7))
                    p.i('dve', 'scalar_tensor_tensor', out=XT[:, dc, t0:t0 + n], in0=ops_[i][:, 0:n], scalar=modT[l][:, 16 + dc, j:j + 1],
                        in1=XT[:, dc, t0:t0 + n], op0=ALU.mult, op1=ALU.add,
                        reads=[('wops', i), 'modT%d' % l, ('XT', dc)], writes=[('XT', dc)])

    def norm2_router(l, b, LG):
        with p.phase():
            rs = p.sb([128, T], F32, 'rs')
            rstd_all(rs, BLKS)
            Wr = p.sb([128, 8, 36], F32, 'Wr'); rb = p.sb([128, 36], F32, 'rb')
            p.dma('sp', Wr[:], moe_rw[l].rearrange("(c p) n -> p c n", p=128), writes=['Wr'])
            p.dma('sp', rb[:], moe_rb[l].partition_broadcast(128), writes=['rb'])
            xn = [p.sb([128, 8, 512], F32, 'xn%d' % i) for i in range(2)]
            lps = [p.ps([128, 512], F32, 'lgps%d' % i) for i in range(2)]
            for bi, (t0, n) in enumerate(BLKS):
                xb = xn[bi % 2]; j = b if t0 >= LC else nb
                p.i('dve', 'tensor_tensor', out=xb[:, :, 0:n], in0=XT[:, :, t0:t0 + n], in1=rs[:, t0:t0 + n].unsqueeze(1).to_broadcast([128, 8, n]),
                    op=ALU.mult, reads=['XT', 'rs'], writes=[('xn', bi % 2)])
                for c in range(8):
                    p.i('act', 'activation', out=xb[:, c, 0:n], in_=xb[:, c, 0:n], func=AF.Identity,
                        scale=g2[l][:, c, j:j + 1], bias=modT[l][:, 24 + c, j:j + 1],
                        reads=[('xn', bi % 2), 'modT%d' % l, 'g2_%d' % l], writes=[('xn', bi % 2)])
                p.i('pool', 'tensor_copy', out=G['HT'][:, :, t0:t0 + n], in_=xb[:, :, 0:n], reads=[('xn', bi % 2)], writes=['HT'])
                lp = lps[bi % 2]
                nt_ = n // 128
                for tl in range(nt_):
                    for c in range(8):
                        p.i('pe', 'matmul', lp[:, tl * 36:(tl + 1) * 36], lhsT=xb[:, c, tl * 128:(tl + 1) * 128], rhs=Wr[:, c, :],
                            start=(c == 0), stop=(c == 7), reads=[('xn', bi % 2), 'Wr'], writes=[('lgps', bi % 2)], sig=(c == 7))
                a0 = t0 // 128
                p.i('dve', 'tensor_tensor', out=LG[:, a0:a0 + nt_, :], in0=lp[:, 0:nt_ * 36].rearrange("p (a e) -> p a e", e=36),
                    in1=rb[:].unsqueeze(1).to_broadcast([128, nt_, 36]), op=ALU.add, reads=[('lgps', bi % 2), 'rb'], writes=['LG'])

    def router_math(LG, combT):
        BIGR = 1.0e4
        with p.phase():
            A_ = lambda shp, nm: p.sb(shp, F32, nm)
            gmax = A_([128, NT], 'rm_gmax'); oh = A_([128, NT, 4], 'rm_oh'); eg = A_([128, NT, 4], 'rm_eg'); gs_ = A_([128, NT], 'rm_gs')
            msk_ = A_([128, NT, 4, 8], 'rm_msk'); m8 = A_([128, NT, 8], 'rm_m8'); k1 = A_([128, NT, 32], 'rm_k1'); k2 = A_([128, NT, 32], 'rm_k2')
            m1 = A_([128, NT], 'rm_m1'); m2 = A_([128, NT], 'rm_m2'); w1 = A_([128, NT], 'rm_w1'); w2 = A_([128, NT], 'rm_w2')
            comb = A_([128, NT, 32], 'rm_comb'); ms2 = A_([128, NT, 32], 'rm_ms2')
            gl = LG[:, :, 0:4]
            p.i('dve', 'tensor_reduce', out=gmax[:], in_=gl, axis=AX.X, op=ALU.max, reads=['LG'], writes=['rm_gmax'])
            gmb = gmax[:].unsqueeze(2).to_broadcast([128, NT, 4])
            p.i('dve', 'tensor_tensor', out=oh[:], in0=gl, in1=gmb, op=ALU.is_equal, reads=['LG', 'rm_gmax'], writes=['rm_oh'])
            p.i('dve', 'tensor_tensor', out=eg[:], in0=gl, in1=gmb, op=ALU.subtract, reads=['LG', 'rm_gmax'], writes=['rm_eg'])
            p.i('act', 'activation', out=eg[:], in_=eg[:], func=AF.Exp, reads=['rm_eg'], writes=['rm_eg'])
            p.i('dve', 'tensor_reduce', out=gs_[:], in_=eg[:], axis=AX.X, op=ALU.add, reads=['rm_eg'], writes=['rm_gs'])
            p.i('dve', 'reciprocal', out=gs_[:], in_=gs_[:], reads=['rm_gs'], writes=['rm_gs'])
            p.i('dve', 'tensor_scalar', out=oh[:], in0=oh[:], scalar1=-1.0, scalar2=BIGR, op0=ALU.add, op1=ALU.mult, reads=['rm_oh'], writes=['rm_oh'])
            el = LG[:, :, 4:36].rearrange("p a (g e) -> p a g e", e=8)
            p.i('dve', 'tensor_tensor', out=msk_[:], in0=el, in1=oh[:].unsqueeze(3).to_broadcast([128, NT, 4, 8]), op=ALU.add,
                reads=['LG', 'rm_oh'], writes=['rm_msk'])
            mf = msk_[:].rearrange("p a g e -> p a (g e)")
            p.i('dve', 'tensor_reduce', out=m1[:], in_=mf, axis=AX.X, op=ALU.max, reads=['rm_msk'], writes=['rm_m1'])
            p.i('dve', 'tensor_tensor', out=k1[:], in0=mf, in1=m1[:].unsqueeze(2).to_broadcast([128, NT, 32]), op=ALU.is_equal,
                reads=['rm_msk', 'rm_m1'], writes=['rm_k1'])
            p.i('dve', 'scalar_tensor_tensor', out=ms2[:], in0=k1[:], scalar=-BIGR, in1=mf, op0=ALU.mult, op1=ALU.add,
                reads=['rm_k1', 'rm_msk'], writes=['rm_ms2'])
            p.i('dve', 'tensor_reduce', out=m2[:], in_=ms2[:], axis=AX.X, op=ALU.max, reads=['rm_ms2'], writes=['rm_m2'])
            p.i('dve', 'tensor_tensor', out=k2[:], in0=ms2[:], in1=m2[:].unsqueeze(2).to_broadcast([128, NT, 32]), op=ALU.is_equal,
                reads=['rm_ms2', 'rm_m2'], writes=['rm_k2'])
            p.i('dve', 'tensor_tensor', out=w1[:], in0=m2[:], in1=m1[:], op=ALU.subtract, reads=['rm_m1', 'rm_m2'], writes=['rm_w1'])
            p.i('act', 'activation', out=w1[:], in_=w1[:], func=AF.Exp, reads=['rm_w1'], writes=['rm_w1'])
            p.i('dve', 'tensor_scalar', out=w1[:], in0=w1[:], scalar1=1.0, scalar2=None, op0=ALU.add, reads=['rm_w1'], writes=['rm_w1'])
            p.i('dve', 'reciprocal', out=w1[:], in_=w1[:], reads=['rm_w1'], writes=['rm_w1'])
            p.i('dve', 'tensor_tensor', out=w1[:], in0=w1[:], in1=gs_[:], op=ALU.mult, reads=['rm_w1', 'rm_gs'], writes=['rm_w1'])
            p.i('dve', 'tensor_tensor', out=w2[:], in0=gs_[:], in1=w1[:], op=ALU.subtract, reads=['rm_w1', 'rm_gs'], writes=['rm_w2'])
            p.i('dve', 'tensor_tensor', out=k1[:], in0=k1[:], in1=w1[:].unsqueeze(2).to_broadcast([128, NT, 32]), op=ALU.mult,
                reads=['rm_k1', 'rm_w1'], writes=['rm_k1'])
            p.i('dve', 'tensor_tensor', out=k2[:], in0=k2[:], in1=w2[:].unsqueeze(2).to_broadcast([128, NT, 32]), op=ALU.mult,
                reads=['rm_k2', 'rm_w2'], writes=['rm_k2'])
            p.i('dve', 'tensor_tensor', out=comb[:], in0=k1[:], in1=k2[:], op=ALU.add, reads=['rm_k1', 'rm_k2'], writes=['rm_comb'])
            if 'comb' in dbg and 'comb' not in dbg_o:
                d_ = dout('comb', [128, NT, 32])
                p.dma('sp', d_[:, :, :], comb[:], reads=['rm_comb'], writes=['dbg_comb'])
            ctp = [p.ps([128, 128], F32, 'rm_ctp%d' % i) for i in range(2)]
            for tt in range(NT):
                i = tt % 2
                p.i('pe', 'matmul', ctp[i][0:32, :], lhsT=comb[:, tt, :], rhs=ident_f[:], start=True, stop=True,
                    reads=['rm_comb', 'ident_f'], writes=[('rm_ctp', i)])
                p.i('act', 'activation', out=combT[0:32, tt * 128:(tt + 1) * 128], in_=ctp[i][0:32, :], func=AF.Copy,
                    reads=[('rm_ctp', i)], writes=['combT'])

    def moe_ffn(l, b, blks, combT):
        with p.phase():
            Esel = p.sb([128, 32, 128], BF16, 'Esel')
            p.dma('sp', Esel[0:32], esel_d[:, :, :], writes=['Esel'])
            Wg = [p.sb([128, 8, 512], BF16, 'mWg%d' % i) for i in range(2)]
            Wu = [p.sb([128, 8, 512], BF16, 'mWu%d' % i) for i in range(2)]
            Wd = [p.sb([128, 4, D], BF16, 'mWd%d' % i) for i in range(2)]
            hid = p.sb([128, 4, 512], BF16, 'mhid')
            sg = [p.sb([128, 512], BF16, 'msg%d' % i) for i in range(2)]
            tu = [p.sb([128, 512], BF16, 'mtu%d' % i) for i in range(2)]
            cwb = p.sb([128, 512], F32, 'mcwb')
            gps = [p.ps([128, 512], F32, 'mgps%d' % i) for i in range(2)]
            ups = [p.ps([128, 512], F32, 'mups%d' % i) for i in range(2)]
            cwp = p.ps([128, 512], F32, 'mcwp')
            yps = [p.ps([128, 512], F32, 'myps%d' % i) for i in range(2)]
            hid2 = p.sb([128, 4, 512], BF16, 'mhidB')
            hids = [hid, hid2]
            seq = [(e, t0, n) for e in range(32) for (t0, n) in blks]
            loaded = set()
            kyc = [0]

            def gu(e, t0, n, hb):
                wi = e % 2
                if e not in loaded:
                    loaded.add(e)
                    p.dma('sp', Wg[wi][:], mg_bf[l, e].rearrange("(c p) n -> p c n", p=128), reads=['mg_bf'], writes=[('mWg', wi)])
                    p.dma('sp', Wu[wi][:], mu_bf[l, e].rearrange("(c p) n -> p c n", p=128), reads=['mu_bf'], writes=[('mWu', wi)])
                    p.dma('sp', Wd[wi][:], md_bf[l, e].rearrange("(c p) n -> p c n", p=128), reads=['md_bf'], writes=[('mWd', wi)])
                hd = hids[hb]
                p.i('pe', 'matmul', cwp[:, 0:n], lhsT=Esel[0:32, e, :], rhs=combT[0:32, t0:t0 + n], start=True, stop=True,
                    reads=['Esel', 'combT'], writes=['mcwp'])
                p.i('act', 'activation', out=cwb[:, 0:n], in_=cwp[:, 0:n], func=AF.Copy, reads=['mcwp'], writes=['mcwb'])
                for hc in range(4):
                    i = hc % 2
                    for kc in range(8):
                        p.i('pe', 'matmul', gps[i][:, 0:n], lhsT=Wg[wi][:, kc, hc * 128:(hc + 1) * 128], rhs=G['HT'][:, kc, t0:t0 + n],
                            start=(kc == 0), stop=(kc == 7), reads=[('mWg', wi), 'HT'], writes=[('mgps', i)], sig=(kc == 7))
                    for kc in range(8):
                        p.i('pe', 'matmul', ups[i][:, 0:n], lhsT=Wu[wi][:, kc, hc * 128:(hc + 1) * 128], rhs=G['HT'][:, kc, t0:t0 + n],
                            start=(kc == 0), stop=(kc == 7), reads=[('mWu', wi), 'HT'], writes=[('mups', i)], sig=(kc == 7))
                    p.i('act', 'activation', out=sg[i][:, 0:n], in_=gps[i][:, 0:n], func=AF.Silu, reads=[('mgps', i)], writes=[('msg', i)])
                    p.i('dve', 'tensor_tensor', out=tu[i][:, 0:n], in0=ups[i][:, 0:n], in1=cwb[:, 0:n], op=ALU.mult,
                        reads=[('mups', i), 'mcwb'], writes=[('mtu', i)])
                    p.i('pool', 'tensor_tensor', out=hd[:, hc, 0:n], in0=sg[i][:, 0:n], in1=tu[i][:, 0:n], op=ALU.mult,
                        reads=[('msg', i), ('mtu', i)], writes=[('mhid%d' % hb, hc)])

            def dn(e, t0, n, hb):
                wi = e % 2; hd = hids[hb]
                j = b if t0 >= LC else nb
                for dc in range(8):
                    i = kyc[0] % 2; kyc[0] += 1
                    for hc in range(4):
                        p.i('pe', 'matmul', yps[i][:, 0:n], lhsT=Wd[wi][:, hc, dc * 128:(dc + 1) * 128], rhs=hd[:, hc, 0:n],
                            start=(hc == 0), stop=(hc == 3), reads=[('mWd', wi), ('mhid%d' % hb, hc)], writes=[('myps', i)], sig=(hc == 3))
                    p.i('dve', 'scalar_tensor_tensor', out=XT[:, dc, t0:t0 + n], in0=yps[i][:, 0:n], scalar=modT[l][:, 40 + dc, j:j + 1],
                        in1=XT[:, dc, t0:t0 + n], op0=ALU.mult, op1=ALU.add,
                        reads=[('myps', i), 'modT%d' % l, ('XT', dc)], writes=[('XT', dc)])

            gu(*seq[0], 0)
            for i_ in range(len(seq)):
                if i_ + 1 < len(seq):
                    gu(*seq[i_ + 1], (i_ + 1) % 2)
                dn(*seq[i_], i_ % 2)

    def final_norm(b):
        with p.phase():
            rs = p.sb([128, T], F32, 'rs')
            rstd_all(rs, BLKS[1:])
            fw = p.sb([128, 8], F32, 'fnw'); p.dma('sp', fw[:], fnw[:, :], writes=['fnw'])
            xn = [p.sb([128, NL], F32, 'fxn%d' % i) for i in range(2)]
            for c in range(8):
                xb = xn[c % 2]
                p.i('dve' if c % 2 == 0 else 'pool', 'tensor_tensor', out=xb[:], in0=XT[:, c, LC:T], in1=rs[:, LC:T], op=ALU.mult,
                    reads=['XT', 'rs'], writes=[('fxn', c % 2)])
                p.i('act', 'activation', out=xb[:], in_=xb[:], func=AF.Identity, scale=fw[:, c:c + 1], reads=[('fxn', c % 2), 'fnw'], writes=[('fxn', c % 2)])
                p.dma('sp', outT[b, c * 128:(c + 1) * 128, :], xb[:], reads=[('fxn', c % 2)], writes=['outT'])

    def moe_prep(layers):
        with p.phase():
            stf = [p.sb([128, 4096], F32, 'mpf%d' % i) for i in range(3)]
            stb = [p.sb([128, 4096], BF16, 'mpb%d' % i) for i in range(3)]
            engs = ['dve', 'pool', 'act']
            k = 0
            def cast(src_t, dst_t, dkey, ncol):
                nonlocal k
                i = k % 3; k += 1
                fv = stf[i][:].rearrange("p (c n) -> p c n", n=ncol); bv = stb[i][:].rearrange("p (c n) -> p c n", n=ncol)
                p.dma('sp', fv, src_t.rearrange("(c p) n -> p c n", p=128), writes=[('mpf', i)])
                if engs[i] == 'act':
                    p.i('act', 'activation', out=stb[i][:], in_=stf[i][:], func=AF.Copy, reads=[('mpf', i)], writes=[('mpb', i)])
                else:
                    p.i(engs[i], 'tensor_copy', out=stb[i][:], in_=stf[i][:], reads=[('mpf', i)], writes=[('mpb', i)])
                p.dma('pool', dst_t.rearrange("(c p) n -> p c n", p=128), bv, reads=[('mpb', i)], writes=[dkey])
            for l in layers:
                for c2 in range(2):
                    cast(w_out[l, c2 * 512:(c2 + 1) * 512, :], wout_bf[l, c2 * 512:(c2 + 1) * 512, :], 'wout_bf', 1024)
                for e in range(32):
                    cast(mwg[l, e], mg_bf[l, e], 'mg_bf', 512)
                    cast(mwu[l, e], mu_bf[l, e], 'mu_bf', 512)
                    cast(mwd[l, e], md_bf[l, e], 'md_bf', 1024)

    def proj(l, b):
        with p.phase():
            W = p.sb([128, 8, 1536], BF16, 'Wp')
            rc = p.sb([128, NL], F32, 'ropeC'); rsn = p.sb([128, NL], F32, 'ropeS')
            p.dma('pool', rc[:], ropeC_d[:, :], writes=['ropeC'])
            p.dma('pool', rsn[:], ropeS_d[:, :], writes=['ropeS'])
            pps = [p.ps([128, 512], F32, 'pps%d' % i) for i in range(4)]
            ev = [p.sb([128, 512], BF16, 'pev%d' % i) for i in range(4)]
            t1 = p.sb([128, 512], F32, 'pt1'); t2 = p.sb([128, 512], F32, 'pt2')
            wv = win_bf[l].rearrange("(c p) n -> p c n", p=128)
            cnt = [0]

            def evac(pi, n, dst_ap):
                i = cnt[0] % 4; cnt[0] += 1
                if i % 2 == 0:
                    p.i('act', 'activation', out=ev[i][:, 0:n], in_=pps[pi][:, 0:n], func=AF.Copy, reads=[('pps', pi)], writes=[('pev', i)])
                else:
                    p.i('dve', 'tensor_copy', out=ev[i][:, 0:n], in_=pps[pi][:, 0:n], reads=[('pps', pi)], writes=[('pev', i)])
                p.dma('sp', dst_ap, ev[i][:, 0:n], reads=[('pev', i)], writes=['Pfm'])

            def mm(wcol, pi, t0, n):
                for c in range(8):
                    p.i('pe', 'matmul', pps[pi][:, 0:n], lhsT=W[:, c, wcol:wcol + 128], rhs=G['HT'][:, c, t0:t0 + n],
                        start=(c == 0), stop=(c == 7), reads=['Wp', 'HT'], writes=[('pps', pi)], sig=(c == 7))

            p.dma('sp', W[:], wv[:, :, 0:1536], writes=['Wp'])
            for (t0, n) in BLKS:
                lat = t0 >= LC
                for (ga, gs, go) in ((0, 2, 0), (1, 3, 1), (4, 5, 4)):
                    mm(ga * 128, 0, t0, n)
                    if lat:
                        mm(gs * 128, 1, t0, n)
                        l0 = t0 - LC
                        p.i('dve', 'tensor_tensor', out=t1[:, 0:n], in0=pps[0][:, 0:n], in1=rc[:, l0:l0 + n], op=ALU.mult,
                            reads=[('pps', 0), 'ropeC'], writes=['pt1'])
                        p.i('dve', 'tensor_tensor', out=t2[:, 0:n], in0=pps[1][:, 0:n], in1=rsn[:, l0:l0 + n], op=ALU.mult,
                            reads=[('pps', 1), 'ropeS'], writes=['pt2'])
                        i = cnt[0] % 4; cnt[0] += 1
                        p.i('pool', 'tensor_tensor', out=ev[i][:, 0:n], in0=t1[:, 0:n], in1=t2[:, 0:n], op=ALU.add,
                            reads=['pt1', 'pt2'], writes=[('pev', i)])
                        p.dma('sp', Pfm[b, go * 128:(go + 1) * 128, t0:t0 + n], ev[i][:, 0:n], reads=[('pev', i)], writes=['Pfm'])
                    else:
                        evac(0, n, Pfm[b, go * 128:(go + 1) * 128, t0:t0 + n])
                for gi, g in enumerate(range(6, 12)):
                    pi = 2 + gi % 2
                    mm(g * 128, pi, t0, n)
                    evac(pi, n, Pfm[b, g * 128:(g + 1) * 128, t0:t0 + n])
            p.dma('sp', W[:, :, 0:1024], wv[:, :, 1536:2560], writes=['Wp'])
            for (t0, n) in BLKS:
                for gi, g in enumerate(range(12, 20)):
                    pi = gi % 4
                    mm(gi * 128, pi, t0, n)
                    evac(pi, n, Pfm[b, g * 128:(g + 1) * 128, t0:t0 + n])
            p.dma('sp', W[:, :, 0:NTM], wv[:, :, 2560:2560 + NTM], writes=['Wp'])
            evt = [p.sb([128, NTM], BF16, 'pevt%d' % i) for i in range(2)]
            for tt in range(NT):
                for (c0, cn, pi) in ((0, 512, 0), (512, 384, 1)):
                    for c in range(8):
                        p.i('pe', 'matmul', pps[pi][:, 0:cn], lhsT=G['HT'][:, c, tt * 128:(tt + 1) * 128], rhs=W[:, c, c0:c0 + cn],
                            start=(c == 0), stop=(c == 7), reads=['Wp', 'HT'], writes=[('pps', pi)], sig=(c == 7))
                i = tt % 2
                p.i('act', 'activation', out=evt[i][:, 0:512], in_=pps[0][:, 0:512], func=AF.Copy, reads=[('pps', 0)], writes=[('pevt', i)])
                p.i('dve', 'tensor_copy', out=evt[i][:, 512:896], in_=pps[1][:, 0:384], reads=[('pps', 1)], writes=[('pevt', i)])
                p.dma('sp', Ptm[b, tt * 128:(tt + 1) * 128, :], evt[i][:], reads=[('pevt', i)], writes=['Ptm'])

    layers = list(range(nlayers))
    if 'moe' in stages or 'wout' in stages:
        moe_prep(layers)
    for b in range(nb):
        p.dma('sp', XT[:], xT_d[b].rearrange("(c p) t -> p c t", p=128), writes=['XT'])
        for l in layers:
            last = (l == 1)
            blks = BLKS[1:] if last else BLKS
            with p.phase():
                G['HT'] = p.sb([128, 8, T], BF16, 'HT')
                if 'norm1' in stages:
                    norm_mod(l, b, g1, 'g1_', 0)
                if 'proj' in stages:
                    proj(l, b)
                if 'HT' in dbg and 'HT' not in dbg_o:
                    d_ = dout('HT', [128, 8, T], BF16)
                    p.dma('sp', d_[:, :, :], G['HT'][:], reads=['HT'], writes=['dbg_HT'])
            with p.phase():
                G['MIXT'] = p.sb([128, 8, T], BF16, 'MIXT')
                if 'swa' in stages:
                    swa(l, b, not last)
                if 'na' in stages:
                    na_prep(l)
                    na(l, b, not last)
                if 'hgrn' in stages:
                    hgrn(l, b)
                if 's5' in stages:
                    s5mix(l, b)
                if 'MIXT' in dbg and 'MIXT' not in dbg_o:
                    d_ = dout('MIXT', [128, 8, T], BF16)
                    p.dma('sp', d_[:, :, :], G['MIXT'][:], reads=['MIXT'], writes=['dbg_MIXT'])
                if 'wout' in stages:
                    wout(l, b, blks)
            if 'x1' in dbg and 'x1' not in dbg_o:
                d_ = dout('x1', [128, 8, T])
                p.dma('sp', d_[:, :, :], XT[:], reads=['XT'], writes=['dbg_x1'])
            if 'moe' in stages:
                with p.phase():
                    G['HT'] = p.sb([128, 8, T], BF16, 'HT')
                    LG = p.sb([128, NT, 36], F32, 'LG'); combT = p.sb([128, T], BF16, 'combT')
                    norm2_router(l, b, LG)
                    if 'LG' in dbg and 'LG' not in dbg_o:
                        d_ = dout('LG', [128, NT, 36])
                        p.dma('sp', d_[:, :, :], LG[:], reads=['LG'], writes=['dbg_LG'])
                    router_math(LG, combT)
                    if 'nomoeffn' not in stages:
                        moe_ffn(l, b, blks, combT)
            if 'x2' in dbg and ('x2_%d' % l) not in dbg_o and b == 0:
                d_ = dout('x2_%d' % l, [128, 8, T])
                p.dma('sp', d_[:, :, :], XT[:], reads=['XT'], writes=['dbg_x2_%d' % l])
        if 'final' in stages:
            final_norm(b)
    if 'P' in dbg:
        d1 = dout('Pfm', [NFM * 128, T], BF16); d2 = dout('Ptm', [T, NTM], BF16)
        p.dma('sp', d1[:, :], Pfm[nb - 1], reads=['Pfm'], writes=['dbg_Pfm'])
        p.dma('sp', d2[:, :], Ptm[nb - 1], reads=['Ptm'], writes=['dbg_Ptm'])
    nc = p.finish(['dbg_' + k for k in dbg_o] + (['outT'] if 'final' in stages else []))
    print('ninst', p.ninst, 'nwait', p.nwait, 'cnt', p.cnt, 'duse', p.duse)
    return nc


ALL_STAGES = ('mod', 'norm1', 'proj', 'swa', 'na', 'hgrn', 's5', 'wout', 'moe', 'final')


def kernel(**inputs):
    inputs = {k: np.asarray(v) for k, v in inputs.items()}
    shared = host_shared(inputs)
    nc = build(nb=NB, stages=ALL_STAGES, dbg=(), nlayers=2)
    in_maps = []
    for core in range(8):
        m = host_prep(inputs, core, nb=NB)
        m.update(shared)
        in_maps.append(m)
    res = run_bass_kernel_spmd(nc, in_maps, core_ids=list(range(8)))
    outs = [np.asarray(r['outT']) for r in res.results]
    out = np.concatenate(outs, 0).transpose(0, 2, 1)
    return np.ascontiguousarray(out).astype(np.float32)
```
